# Optimizing a Trainium2 kernel written in Bass

```python
import jax
import jax.numpy as jnp
from jax import lax
import numpy as np


D_MODEL = 1024
BATCH = 32
SEQ = 2048
DEPTH = 1

CHUNK = 64
Q_BLOCK = 128
A_HEADS = 8
A_HEAD_DIM = 64
A_KV_RANK = 128
IDX_HEADS = 8
IDX_DIM = 64
IDX_TOPK = 256
IDX_SCALE = (IDX_HEADS * IDX_DIM) ** -0.5
B_HEADS = 8
B_HEAD_DIM = 64
B_LEFT_CHUNKS = 8
B_BAND = (B_LEFT_CHUNKS + 1) * CHUNK
REL_CLIP = 128
N_EXPERTS = 32
TOP_K = 4
D_FF = 1024
SWIGLU_LIMIT = 7.0
SWIGLU_ALPHA = 1.702
EXPERT_BLOCK = 256
DN_ALPHA = (2.0 * DEPTH) ** 0.25
DN_BETA = (8.0 * DEPTH) ** -0.25
LN_EPS = 1e-5

A_WIDTH = A_HEADS * A_HEAD_DIM
B_WIDTH = B_HEADS * B_HEAD_DIM
SPLITS = [A_WIDTH, A_KV_RANK, IDX_HEADS * IDX_DIM, IDX_DIM, IDX_HEADS,
          B_WIDTH, B_WIDTH, B_WIDTH, D_MODEL, D_MODEL]
IN_WIDTH = sum(SPLITS)

kernel_name = 'streaming_hybrid_dsa_chunkband_moe_block'


def layer_norm(x, g, b):
    xf = x.astype(jnp.float32)
    mu = jnp.mean(xf, axis=-1, keepdims=True)
    var = jnp.mean(jnp.square(xf - mu), axis=-1, keepdims=True)
    return ((xf - mu) * lax.rsqrt(var + LN_EPS)).astype(x.dtype) * g + b


def rms_norm(x, g):
    xf = x.astype(jnp.float32)
    ms = jnp.mean(jnp.square(xf), axis=-1, keepdims=True)
    return (xf * lax.rsqrt(ms + LN_EPS)).astype(x.dtype) * g


def alibi_slopes(n_heads):
    return jnp.array([2.0 ** (-8.0 * (h + 1) / n_heads) for h in range(n_heads)], jnp.float32)


def sparse_indexed_attention(q, c_kv, q_idx, k_idx, w_idx, w_uk, w_uv):
    bsz, seq = q.shape[0], q.shape[1]
    topk = min(IDX_TOPK, seq // 4)
    nb = seq // Q_BLOCK
    scale = A_HEAD_DIM ** -0.5
    q_lat = jnp.einsum('bshd,hdr->bshr', q, w_uk)
    key_chunk = jnp.arange(seq) // CHUNK
    slopes = alibi_slopes(A_HEADS)

    def to_blocks(a):
        return a.reshape((bsz, nb, Q_BLOCK) + a.shape[2:]).swapaxes(0, 1)

    def block_fn(args):
        blk, qb, qib, wb = args
        qpos = blk * Q_BLOCK + jnp.arange(Q_BLOCK)
        qchunk = qpos // CHUNK
        admissible = key_chunk[None, :] <= qchunk[:, None]
        logits = jax.nn.relu(jnp.einsum('bthd,bsd->bths', qib, k_idx).astype(jnp.float32))
        iscore = jnp.einsum('bth,bths->bts', wb.astype(jnp.float32) * IDX_SCALE, logits)
        iscore = jnp.where(admissible[None], iscore, -jnp.inf)
        _, sel = lax.top_k(iscore, topk)
        kv_sel = jax.vmap(lambda c, i: c[i])(c_kv, sel)
        scores = jnp.einsum('bthr,btkr->bthk', qb, kv_sel).astype(jnp.float32) * scale
        dist = jnp.abs(qpos[None, :, None] - sel)
        scores = scores - slopes[None, None, :, None] * dist[:, :, None, :]
        valid = (sel // CHUNK) <= qchunk[None, :, None]
        scores = jnp.where(valid[:, :, None, :], scores, -jnp.inf)
        p = jax.nn.softmax(scores, axis=-1).astype(c_kv.dtype)
        return jnp.einsum('bthk,btkr->bthr', p, kv_sel)

    o_lat = lax.map(block_fn, (jnp.arange(nb), to_blocks(q_lat), to_blocks(q_idx), to_blocks(w_idx)))
    o_lat = o_lat.swapaxes(0, 1).reshape(bsz, seq, A_HEADS, A_KV_RANK)
    o = jnp.einsum('bshr,hrd->bshd', o_lat, w_uv)
    return o.reshape(bsz, seq, A_WIDTH)


def chunk_band_attention(q, k, v, rel_bias):
    bsz, seq = q.shape[0], q.shape[1]
    nc = seq // CHUNK
    pad = B_LEFT_CHUNKS * CHUNK
    scale = B_HEAD_DIM ** -0.5
    k_pad = jnp.pad(k, ((0, 0), (pad, 0), (0, 0), (0, 0)))
    v_pad = jnp.pad(v, ((0, 0), (pad, 0), (0, 0), (0, 0)))
    rel = jnp.arange(CHUNK)[:, None] - jnp.arange(B_BAND)[None, :] + pad
    bias = rel_bias[:, jnp.clip(rel, -REL_CLIP, REL_CLIP) + REL_CLIP].astype(jnp.float32)
    q_chunks = q.reshape(bsz, nc, CHUNK, B_HEADS, B_HEAD_DIM).swapaxes(0, 1)

    def chunk_fn(args):
        c, qc = args
        kb = lax.dynamic_slice_in_dim(k_pad, c * CHUNK, B_BAND, axis=1)
        vb = lax.dynamic_slice_in_dim(v_pad, c * CHUNK, B_BAND, axis=1)
        s = jnp.einsum('bqhd,bkhd->bhqk', qc, kb).astype(jnp.float32) * scale + bias[None]
        key_valid = (c * CHUNK - pad + jnp.arange(B_BAND)) >= 0
        s = jnp.where(key_valid[None, None, None, :], s, -jnp.inf)
        p = jax.nn.softmax(s, axis=-1).astype(vb.dtype)
        return jnp.einsum('bhqk,bkhd->bqhd', p, vb)

    o = lax.map(chunk_fn, (jnp.arange(nc), q_chunks))
    return o.swapaxes(0, 1).reshape(bsz, seq, B_WIDTH)


def moe_ffn(x, w_router, b_router, w_gate, b_gate, w_up, b_up, w_down, b_down):
    bsz, seq, d = x.shape
    n = bsz * seq
    xf = x.reshape(n, d)
    logits = (xf @ w_router + b_router).astype(jnp.float32)
    top_val, top_idx = lax.top_k(logits, TOP_K)
    gates = jax.nn.softmax(top_val, axis=-1)
    n_assign = n * TOP_K
    expert_flat = top_idx.reshape(-1)
    token_flat = jnp.repeat(jnp.arange(n, dtype=jnp.int32), TOP_K)
    gate_flat = gates.reshape(-1)
    order = jnp.argsort(expert_flat)
    e_s = expert_flat[order]
    t_s = token_flat[order]
    g_s = gate_flat[order]
    counts = jnp.bincount(expert_flat, length=N_EXPERTS).astype(jnp.int32)
    offsets = jnp.cumsum(counts) - counts
    padded = (counts + EXPERT_BLOCK - 1) // EXPERT_BLOCK * EXPERT_BLOCK
    padded_ends = jnp.cumsum(padded)
    padded_starts = padded_ends - padded
    dest = padded_starts[e_s] + jnp.arange(n_assign, dtype=jnp.int32) - offsets[e_s]
    nblk = -(-n_assign // EXPERT_BLOCK) + N_EXPERTS
    cap = nblk * EXPERT_BLOCK
    tok_buf = jnp.zeros((cap,), jnp.int32).at[dest].set(t_s)
    gate_buf = jnp.zeros((cap,), jnp.float32).at[dest].set(g_s)
    blk_start = jnp.arange(nblk, dtype=jnp.int32) * EXPERT_BLOCK
    blk_expert = jnp.minimum(jnp.searchsorted(padded_ends, blk_start, side='right'), N_EXPERTS - 1)

    def block_fn(args):
        e, tok, g = args
        xb = xf[tok]
        hg = jnp.minimum(xb @ w_gate[e] + b_gate[e], SWIGLU_LIMIT)
        hu = jnp.clip(xb @ w_up[e] + b_up[e], -SWIGLU_LIMIT, SWIGLU_LIMIT)
        h = (hu + 1.0) * (hg * jax.nn.sigmoid(SWIGLU_ALPHA * hg))
        y = h @ w_down[e] + b_down[e]
        return y * g[:, None].astype(y.dtype)

    y_buf = lax.map(block_fn, (blk_expert, tok_buf.reshape(nblk, EXPERT_BLOCK),
                               gate_buf.reshape(nblk, EXPERT_BLOCK)))
    y = jax.ops.segment_sum(y_buf.reshape(cap, d), tok_buf, num_segments=n)
    return y.reshape(bsz, seq, d)


def setup_inputs(seed: int = 0) -> dict:
    key = jax.random.key(seed)
    ks = jax.random.split(key, 24)
    f32 = jnp.float32
    nrm = lambda k, shape, s: jax.random.normal(k, shape, f32) * s
    bounds = np.cumsum([0] + SPLITS)
    col_scale = np.ones((IN_WIDTH,), np.float32)
    col_scale[bounds[7]:bounds[8]] = DN_BETA
    w_in = nrm(ks[1], (DEPTH, D_MODEL, IN_WIDTH), D_MODEL ** -0.5) * jnp.asarray(col_scale)
    return {
        'x': jax.random.normal(ks[0], (BATCH, SEQ, D_MODEL), f32),
        'w_in': w_in,
        'kv_norm_g': 1.0 + nrm(ks[2], (DEPTH, A_KV_RANK), 0.02),
        'idx_k_norm_g': 1.0 + nrm(ks[3], (DEPTH, IDX_DIM), 0.02),
        'idx_k_norm_b': nrm(ks[4], (DEPTH, IDX_DIM), 0.02),
        'w_uk': nrm(ks[5], (DEPTH, A_HEADS, A_HEAD_DIM, A_KV_RANK), A_KV_RANK ** -0.5),
        'w_uv': nrm(ks[6], (DEPTH, A_HEADS, A_KV_RANK, A_HEAD_DIM), DN_BETA * A_KV_RANK ** -0.5),
        'rel_bias': nrm(ks[7], (DEPTH, B_HEADS, 2 * REL_CLIP + 1), 0.1),
        'w_branch_a': nrm(ks[8], (DEPTH, A_WIDTH, D_MODEL), DN_BETA * A_WIDTH ** -0.5),
        'w_branch_b': nrm(ks[9], (DEPTH, B_WIDTH, D_MODEL), DN_BETA * B_WIDTH ** -0.5),
        'w_out': nrm(ks[10], (DEPTH, D_MODEL, D_MODEL), DN_BETA * D_MODEL ** -0.5),
        'ln1_g': 1.0 + nrm(ks[11], (DEPTH, D_MODEL), 0.02),
        'ln1_b': nrm(ks[12], (DEPTH, D_MODEL), 0.02),
        'w_router': nrm(ks[13], (DEPTH, D_MODEL, N_EXPERTS), D_MODEL ** -0.5),
        'b_router': nrm(ks[14], (DEPTH, N_EXPERTS), 0.01),
        'w_gate': nrm(ks[15], (DEPTH, N_EXPERTS, D_MODEL, D_FF), DN_BETA * D_MODEL ** -0.5),
        'b_gate': nrm(ks[16], (DEPTH, N_EXPERTS, D_FF), 0.01),
        'w_up': nrm(ks[17], (DEPTH, N_EXPERTS, D_MODEL, D_FF), DN_BETA * D_MODEL ** -0.5),
        'b_up': nrm(ks[18], (DEPTH, N_EXPERTS, D_FF), 0.01),
        'w_down': nrm(ks[19], (DEPTH, N_EXPERTS, D_FF, D_MODEL), DN_BETA * D_FF ** -0.5),
        'b_down': nrm(ks[20], (DEPTH, N_EXPERTS, D_MODEL), 0.01),
        'ln2_g': 1.0 + nrm(ks[21], (DEPTH, D_MODEL), 0.02),
        'ln2_b': nrm(ks[22], (DEPTH, D_MODEL), 0.02),
    }


def reference(x, w_in, kv_norm_g, idx_k_norm_g, idx_k_norm_b, w_uk, w_uv, rel_bias,
              w_branch_a, w_branch_b, w_out, ln1_g, ln1_b, w_router, b_router,
              w_gate, b_gate, w_up, b_up, w_down, b_down, ln2_g, ln2_b):
    bsz, seq, _ = x.shape
    split_at = np.cumsum(SPLITS)[:-1].tolist()
    for l in range(DEPTH):
        proj = jnp.einsum('bsd,de->bse', x, w_in[l])
        (qa, ckv, qi, ki, wi, qb, kb, vb, ga, gb) = jnp.split(proj, split_at, axis=-1)
        qa = qa.reshape(bsz, seq, A_HEADS, A_HEAD_DIM)
        ckv = rms_norm(ckv, kv_norm_g[l])
        qi = qi.reshape(bsz, seq, IDX_HEADS, IDX_DIM)
        ki = layer_norm(ki, idx_k_norm_g[l], idx_k_norm_b[l])
        y_a = sparse_indexed_attention(qa, ckv, qi, ki, wi, w_uk[l], w_uv[l])
        y_b = chunk_band_attention(qb.reshape(bsz, seq, B_HEADS, B_HEAD_DIM),
                                   kb.reshape(bsz, seq, B_HEADS, B_HEAD_DIM),
                                   vb.reshape(bsz, seq, B_HEADS, B_HEAD_DIM), rel_bias[l])
        merged = (jax.nn.sigmoid(ga) * (y_a @ w_branch_a[l])
                  + jax.nn.sigmoid(gb) * (y_b @ w_branch_b[l]))
        x = layer_norm(DN_ALPHA * x + merged @ w_out[l], ln1_g[l], ln1_b[l])
        ffn = moe_ffn(x, w_router[l], b_router[l], w_gate[l], b_gate[l], w_up[l], b_up[l],
                      w_down[l], b_down[l])
        x = layer_norm(DN_ALPHA * x + ffn, ln2_g[l], ln2_b[l])
    return x
```

```python
import numpy as np
import ml_dtypes
import concourse.bass as bass
import concourse.mybir as mybir
from concourse.bass_utils import run_bass_kernel_spmd

F32 = mybir.dt.float32
BF16 = mybir.dt.bfloat16
ALU = mybir.AluOpType
AF = mybir.ActivationFunctionType
AX = mybir.AxisListType

D = 1024
SEQ = 2048
NCORES = 8
NEXP = 32
DFF = 1024
INW = 4808
ALPHA = 2.0 ** 0.25
EPS = 1e-5
IDX_SCALE = 512.0 ** -0.5
NEG = -30000.0
TOPK = 256
CAP = 1280
NSLOT = NEXP * CAP
BIGM = 262144.0
U32 = mybir.dt.uint32


class Buf:
    __slots__ = ("w", "r")

    def __init__(self):
        self.w = {}
        self.r = {}


class Op:
    __slots__ = ("eng", "fn", "deps", "dma", "sem", "val", "signals", "cnt")

    def __init__(self, eng, fn, dma):
        self.eng = eng
        self.fn = fn
        self.dma = dma
        self.deps = []
        self.sem = None
        self.val = 0
        self.signals = False
        self.cnt = 0


class Prog:
    ENG = ("pe", "act", "dve", "pool", "sp")

    def __init__(self, nc, ndma=48):
        self.nc = nc
        self.ops = {e: [] for e in self.ENG}
        self.ndma = ndma
        self.dma_uses = [0] * ndma
        self.dma_last = [None] * ndma
        self.dma_n = 0
        self.live_dma = []

    def _add(self, op, reads, writes):
        deps = {}
        for b in reads:
            for o in b.w.values():
                deps[o] = True
        for b in writes:
            for o in b.w.values():
                deps.setdefault(o, False)
            for o in b.r.values():
                deps.setdefault(o, False)
        for o, raw in deps.items():
            if o is not op:
                op.deps.append((o, raw))
        key = op if op.dma else op.eng
        for b in reads:
            b.r[key] = op
        for b in writes:
            b.w = {key: op}
            b.r = {}
        self.ops[op.eng].append(op)
        return op

    def op(self, eng, fn, reads=(), writes=()):
        return self._add(Op(eng, fn, False), reads, writes)

    def dma(self, q, out, in_, reads=(), writes=()):
        op = Op(q, lambda h: h.dma_start(out=out, in_=in_), True)
        slot = self.dma_n % self.ndma
        self.dma_n += 1
        prev = self.dma_last[slot]
        if prev is not None:
            op.deps.append((prev, True))
        self.dma_uses[slot] += 1
        self.dma_last[slot] = op
        op.sem = slot
        op.val = 16 * self.dma_uses[slot]
        self.live_dma.append(op)
        return self._add(op, reads, writes)

    def idma(self, out, out_off, in_, in_off, reads=(), writes=(), bc=None):
        def fn(h):
            if getattr(self, "_bcreg", None) is None:
                self._bcreg = h.alloc_register("bcreg")
                h.reg_mov(self._bcreg, bc)
                self._bcval = bc
            assert self._bcval == bc
            return h.indirect_dma_start(out=out, out_offset=out_off, in_=in_, in_offset=in_off,
                                        bounds_check=self._bcreg, oob_is_err=False)
        op = Op("pool", fn, True)
        slot = self.dma_n % self.ndma
        self.dma_n += 1
        prev = self.dma_last[slot]
        if prev is not None:
            op.deps.append((prev, True))
        self.dma_uses[slot] += 1
        self.dma_last[slot] = op
        op.sem = slot
        op.val = 16 * self.dma_uses[slot]
        self.live_dma.append(op)
        return self._add(op, reads, writes)

    def barrier(self):
        last = {e: (self.ops[e][-1] if self.ops[e] else None) for e in self.ENG}
        live = self.live_dma
        self.live_dma = []
        for e in self.ENG:
            b = Op(e, None, False)
            for e2 in self.ENG:
                if e2 != e and last[e2] is not None and not last[e2].dma and last[e2].fn is not None:
                    b.deps.append((last[e2], True))
            for d in live:
                b.deps.append((d, True))
            self.ops[e].append(b)

    @staticmethod
    def _skip(op, d, raw):
        if d.dma or op.dma:
            return False
        if d.eng != op.eng:
            return False
        if op.eng == "pe":
            return True
        return not raw

    def emit(self):
        nc = self.nc
        for e in self.ENG:
            for op in self.ops[e]:
                for d, raw in op.deps:
                    if not self._skip(op, d, raw) and not d.dma:
                        d.signals = True
        for e in self.ENG:
            c = 0
            for op in self.ops[e]:
                if op.signals and not op.dma:
                    c += 1
                    op.cnt = c
        import contextlib
        with contextlib.ExitStack() as st:
            esem = {e: st.enter_context(nc.semaphore("s_" + e)) for e in self.ENG}
            dsem = [st.enter_context(nc.semaphore("d%d" % i)) for i in range(self.ndma)]
            block = st.enter_context(nc.Block())

            def run(e, h):
                waited = {}
                for op in self.ops[e]:
                    for d, raw in op.deps:
                        if self._skip(op, d, raw):
                            continue
                        if d.dma:
                            key, sem, val = ("d", d.sem), dsem[d.sem], d.val
                        else:
                            key, sem, val = ("e", d.eng), esem[d.eng], d.cnt
                        if waited.get(key, 0) >= val:
                            continue
                        waited[key] = val
                        h.wait_ge(sem, val)
                    if op.fn is None:
                        continue
                    ins = op.fn(h)
                    if op.dma:
                        ins.then_inc(dsem[op.sem], 16)
                    elif op.signals:
                        ins.then_inc(esem[e], 1)

            @block.tensor
            def _(h):
                run("pe", h)

            @block.scalar
            def _(h):
                run("act", h)

            @block.vector
            def _(h):
                run("dve", h)

            @block.gpsimd
            def _(h):
                run("pool", h)

            @block.sync
            def _(h):
                run("sp", h)


class Arena:
    def __init__(self, nc, lo, hi):
        self.nc, self.lo, self.hi, self.n = nc, lo, hi, 0

    def t(self, name, shape, dt):
        esz = 4 if dt == F32 else 2
        per = int(np.prod(shape[1:])) * esz
        per = (per + 63) // 64 * 64
        assert self.lo + per <= self.hi, (name, self.lo, per, self.hi)
        h = self.nc.alloc_sbuf_tensor_at("%s_%d" % (name, self.lo), list(shape), dt, offset=self.lo)
        self.lo += per
        return h


class Rot:
    def __init__(self, arena, name, shape, dt, n):
        self.items = [(arena.t("%s%d" % (name, i), shape, dt), Buf()) for i in range(n)]
        self.i = 0

    def next(self):
        it = self.items[self.i % len(self.items)]
        self.i += 1
        return it


def build(nseq=4, debug=False, stop_after=None, mixers="AB"):
    nc = bass.Bass("TRN2", target_bir_lowering=False)
    P = Prog(nc)
    TOK = nseq * SEQ
    NT = TOK // 128
    dk = "ExternalOutput" if debug else "Internal"

    def din(name, shape, dt=F32):
        return nc.dram_tensor(name, list(shape), dt, kind="ExternalInput").ap()

    def dscr(name, shape, dt):
        return nc.dram_tensor(name, list(shape), dt, kind=dk).ap()

    xT_d = din("xT", [D, TOK])
    x_d = din("x", [TOK, D])
    win_d = din("w_in", [D, INW])
    kvg_d = din("kvg_bc", [128, 128])
    ikg_d = din("ikg_bc", [128, 64])
    ikb_d = din("ikb_bc", [128, 64])
    wuk_d = din("w_uk2", [128, 4, 128])
    wuv_d = din("w_uv", [8, 128, 64])
    relb_d = din("relb", [8, 128, 640])
    maskb_d = din("maskB", [128, 640])
    negdh_d = din("negDh", [128, 2048], BF16)
    negdl_d = din("negDl", [128, 2048], BF16)
    slopei_d = din("slopeI", [128, 8, 128], BF16)
    wa_d = din("w_branch_a", [512, D])
    wb_d = din("w_branch_b", [512, D])
    wo_d = din("w_out", [D, D])
    ln1g_d = din("ln1g_bc", [128, D])
    ln1b_d = din("ln1b_bc", [128, D])
    wr_d = din("w_router", [D, NEXP])
    br_d = din("br_bc", [128, NEXP])
    wg_d = din("w_gate", [NEXP, D, DFF])
    wu_d = din("w_up", [NEXP, D, DFF])
    wd_d = din("w_down", [NEXP, DFF, D])
    bg_d = din("b_gate_p", [128, NEXP, 8])
    bu_d = din("b_up_p", [128, NEXP, 8])
    bd_d = din("b_down", [NEXP, D])
    ln2g_d = din("ln2g_bc", [128, D])
    ln2b_d = din("ln2b_bc", [128, D])
    identb_d = din("ident_bf", [128, 128], BF16)
    identf_d = din("ident_f", [128, 128])
    ustr_d = din("ustrict", [128, 128], BF16)
    onesb_d = din("ones_bf", [128, 128], BF16)
    ebase_d = din("ebase", [128, NEXP])
    pow2_d = din("pow2", [128, 21])
    out_d = nc.dram_tensor("out", [TOK, D], F32, kind="ExternalOutput").ap()

    featT = dscr("featT", [32, 128, TOK], BF16)
    ckvtm_s = dscr("ckv_tm", [TOK, 128], BF16)
    ckvT_s = dscr("ckvT", [128, TOK], BF16)
    kidxT_s = dscr("kidxT", [64, TOK], BF16)
    widx_s = dscr("widx", [TOK, 8], F32)
    vb_s = dscr("vB", [TOK, 512], BF16)
    yaT_s = dscr("yaT", [4, 128, TOK], BF16)
    ybT_s = dscr("ybT", [4, 128, TOK], BF16)
    x1_s = dscr("x1", [TOK, D], F32)
    x1T_s = dscr("x1T", [8, 128, TOK], BF16)
    gates_s = dscr("gates", [TOK, NEXP], F32)
    route_s = dscr("route", [TOK, 8], F32)
    xd_s = dscr("xd", [NSLOT, D], BF16)
    yd_h = [dscr("yd0", [NSLOT, 512], F32), dscr("yd1", [NSLOT, 512], F32)]

    sb = {}

    def tb(name, t0, t1):
        return [sb.setdefault((name, t), Buf()) for t in range(t0, t1)]

    psf = [(nc.alloc_psum_tensor("psf%d" % i, [128, 512], F32), Buf()) for i in range(6)]
    psb = [(nc.alloc_psum_tensor("psb%d" % i, [128, 1024], BF16), Buf()) for i in range(2)]
    psi = [0, 0]

    def PSF():
        psi[0] += 1
        return psf[psi[0] % 3]

    accA, accB, accC = psf[4], psf[5], psf[3]

    def PSB():
        psi[1] += 1
        return psb[psi[1] % 2]

    SBMAX = 224 * 1024
    C = Arena(nc, 16640, 60 * 1024)
    ident_b = C.t("identb", [128, 128], BF16)
    ident_f = C.t("identf", [128, 128], F32)
    B_const = Buf()
    P.dma("sp", ident_b[:], identb_d, writes=[B_const])
    P.dma("sp", ident_f[:], identf_d, writes=[B_const])
    eps_t = C.t("eps", [128, 1], F32)
    P.op("dve", lambda h: h.memset(eps_t[:], EPS), writes=[B_const])
    A0 = C.lo

    A = Arena(nc, A0, SBMAX)
    win = A.t("win", [128, 8, INW], BF16)
    B_w = Buf()
    for kc in range(8):
        P.dma("pool", win[:, kc, :], win_d[kc * 128:(kc + 1) * 128, :], writes=[B_w])
    kvg = A.t("kvg", [128, 128], F32)
    ikg = A.t("ikg", [128, 64], F32)
    ikb = A.t("ikb", [128, 64], F32)
    P.dma("sp", kvg[:], kvg_d, writes=[B_const])
    P.dma("sp", ikg[:], ikg_d, writes=[B_const])
    P.dma("sp", ikb[:], ikb_d, writes=[B_const])

    xTg_r = Rot(A, "xTg", [128, 8, 512], BF16, 2)
    stg_r = Rot(A, "stg", [128, 512], BF16, 4)
    ckvb_r = Rot(A, "ckvb", [128, 128], BF16, 2)
    ckvTs_r = Rot(A, "ckvTs", [128, 128], BF16, 2)
    knf_r = Rot(A, "knf", [128, 64], F32, 2)
    knb_r = Rot(A, "knb", [128, 64], BF16, 2)
    kTs_r = Rot(A, "kTs", [64, 128], BF16, 2)
    ws_r = Rot(A, "ws", [128, 8], F32, 2)
    vbs_r = Rot(A, "vbs", [128, 512], BF16, 2)
    junk_r = Rot(A, "junk", [128, 128], F32, 2)
    st_r = Rot(A, "st", [128, 16], F32, 4)

    xT_v = xT_d.rearrange("(kc p) t -> p kc t", p=128)
    FM = [0, 128, 256, 384, 640, 768, 896, 1024, 1224, 1352, 1480, 1608, 1736, 1864, 1992, 2120] + \
         [2760 + 128 * i for i in range(16)]

    def phaseA(seq):
        for g in range(seq * 4, seq * 4 + 4):
            t0 = g * 4
            xTg, Bx = xTg_r.next()
            P.dma("pool", xTg[:], xT_v[:, :, g * 512:(g + 1) * 512], writes=[Bx])
            for ci, c0 in enumerate(FM):
                ps, Bp = PSF()
                for kc in range(8):
                    P.op("pe", lambda h, ps=ps, kc=kc, c0=c0, xTg=xTg: h.matmul(
                        ps[:, :], lhsT=win[:, kc, c0:c0 + 128], rhs=xTg[:, kc, :], start=(kc == 0), stop=(kc == 7)),
                        reads=[B_w, Bx], writes=[Bp])
                stg, Bs = stg_r.next()
                if ci >= 16:
                    P.op("act", lambda h, stg=stg, ps=ps: h.activation(out=stg[:], in_=ps[:, :], func=AF.Sigmoid),
                         reads=[Bp], writes=[Bs])
                elif ci % 2 == 0:
                    P.op("act", lambda h, stg=stg, ps=ps: h.copy(out=stg[:], in_=ps[:, :]), reads=[Bp], writes=[Bs])
                else:
                    P.op("dve", lambda h, stg=stg, ps=ps: h.tensor_copy(out=stg[:], in_=ps[:, :]), reads=[Bp], writes=[Bs])
                P.dma("sp", featT[ci, :, g * 512:(g + 1) * 512], stg[:], reads=[Bs], writes=tb(("f", ci), t0, t0 + 4))
            for tt in range(4):
                t = t0 + tt
                tok = slice(t * 128, (t + 1) * 128)
                psA, BpA = PSF()
                for kc in range(8):
                    P.op("pe", lambda h, ps=psA, kc=kc, tt=tt, xTg=xTg: h.matmul(
                        ps[:, 0:128], lhsT=xTg[:, kc, tt * 128:(tt + 1) * 128], rhs=win[:, kc, 512:640],
                        start=(kc == 0), stop=(kc == 7)), reads=[B_w, Bx], writes=[BpA])
                for kc in range(8):
                    P.op("pe", lambda h, ps=psA, kc=kc, tt=tt, xTg=xTg: h.matmul(
                        ps[:, 128:200], lhsT=xTg[:, kc, tt * 128:(tt + 1) * 128], rhs=win[:, kc, 1152:1224],
                        start=(kc == 0), stop=(kc == 7)), reads=[B_w, Bx], writes=[BpA])
                psV, BpV = PSF()
                for kc in range(8):
                    P.op("pe", lambda h, ps=psV, kc=kc, tt=tt, xTg=xTg: h.matmul(
                        ps[:, :], lhsT=xTg[:, kc, tt * 128:(tt + 1) * 128], rhs=win[:, kc, 2248:2760],
                        start=(kc == 0), stop=(kc == 7)), reads=[B_w, Bx], writes=[BpV])
                vbs, Bv = vbs_r.next()
                P.op("act", lambda h, vbs=vbs, ps=psV: h.copy(out=vbs[:], in_=ps[:, :]), reads=[BpV], writes=[Bv])
                P.dma("sp", vb_s[tok, :], vbs[:], reads=[Bv], writes=tb("vb", t, t + 1))
                st, Bst = st_r.next()
                junk, Bj = junk_r.next()
                P.op("act", lambda h, junk=junk, ps=psA, st=st: h.activation(
                    out=junk[:, 0:128], in_=ps[:, 0:128], func=AF.Square, accum_out=st[:, 0:1]),
                    reads=[BpA], writes=[Bj, Bst])
                P.op("act", lambda h, st=st: h.activation(out=st[:, 1:2], in_=st[:, 0:1], func=AF.Sqrt,
                                                          scale=1.0 / 128.0, bias=eps_t[:, 0:1]),
                     reads=[Bst, B_const], writes=[Bst])
                P.op("dve", lambda h, st=st: h.reciprocal(out=st[:, 2:3], in_=st[:, 1:2]), reads=[Bst], writes=[Bst])
                ckvb, Bc = ckvb_r.next()
                P.op("dve", lambda h, ckvb=ckvb, ps=psA, st=st: h.scalar_tensor_tensor(
                    out=ckvb[:], in0=ps[:, 0:128], scalar=st[:, 2:3], in1=kvg[:], op0=ALU.mult, op1=ALU.mult),
                    reads=[BpA, Bst, B_const], writes=[Bc])
                P.dma("sp", ckvtm_s[tok, :], ckvb[:], reads=[Bc], writes=tb("ckvtm", t, t + 1))
                pT, BpT = PSB()
                P.op("pe", lambda h, pT=pT, ckvb=ckvb: h.transpose(out=pT[:, 0:128], in_=ckvb[:], identity=ident_b[:]),
                     reads=[Bc, B_const], writes=[BpT])
                cts, Bct = ckvTs_r.next()
                P.op("act", lambda h, cts=cts, pT=pT: h.copy(out=cts[:], in_=pT[:, 0:128]), reads=[BpT], writes=[Bct])
                P.dma("sp", ckvT_s[:, tok], cts[:], reads=[Bct], writes=tb("ckvT", t, t + 1))
                P.op("dve", lambda h, st=st, ps=psA: h.bn_stats(out=st[:, 4:10], in_=ps[:, 128:192]),
                     reads=[BpA], writes=[Bst])
                P.op("dve", lambda h, st=st: h.bn_aggr(out=st[:, 10:12], in_=st[:, 4:10]), reads=[Bst], writes=[Bst])
                P.op("act", lambda h, st=st: h.activation(out=st[:, 12:13], in_=st[:, 11:12], func=AF.Sqrt,
                                                          scale=1.0, bias=eps_t[:, 0:1]),
                     reads=[Bst, B_const], writes=[Bst])
                P.op("dve", lambda h, st=st: h.reciprocal(out=st[:, 13:14], in_=st[:, 12:13]), reads=[Bst], writes=[Bst])
                knf, Bkf = knf_r.next()
                P.op("dve", lambda h, knf=knf, ps=psA, st=st: h.tensor_scalar(
                    out=knf[:], in0=ps[:, 128:192], scalar1=st[:, 10:11], scalar2=st[:, 13:14],
                    op0=ALU.subtract, op1=ALU.mult), reads=[BpA, Bst], writes=[Bkf])
                P.op("dve", lambda h, knf=knf: h.tensor_tensor(out=knf[:], in0=knf[:], in1=ikg[:], op=ALU.mult),
                     reads=[Bkf, B_const], writes=[Bkf])
                knb, Bkb = knb_r.next()
                P.op("dve", lambda h, knf=knf, knb=knb: h.tensor_tensor(out=knb[:], in0=knf[:], in1=ikb[:], op=ALU.add),
                     reads=[Bkf, B_const], writes=[Bkb])
                pT2, BpT2 = PSB()
                P.op("pe", lambda h, pT2=pT2, knb=knb: h.transpose(out=pT2[0:64, 0:128], in_=knb[:], identity=ident_b[:]),
                     reads=[Bkb, B_const], writes=[BpT2])
                kts, Bkt = kTs_r.next()
                P.op("act", lambda h, kts=kts, pT2=pT2: h.copy(out=kts[:], in_=pT2[0:64, 0:128]), reads=[BpT2], writes=[Bkt])
                P.dma("sp", kidxT_s[:, tok], kts[:], reads=[Bkt], writes=tb("kidxT", t, t + 1))
                ws, Bws = ws_r.next()
                P.op("dve", lambda h, ws=ws, ps=psA: h.tensor_scalar(
                    out=ws[:], in0=ps[:, 192:200], scalar1=IDX_SCALE, scalar2=None, op0=ALU.mult),
                    reads=[BpA], writes=[Bws])
                P.dma("sp", widx_s[tok, :], ws[:], reads=[Bws], writes=tb("widx", t, t + 1))

    for seq in range(nseq):
        phaseA(seq)
    P.barrier()
    if stop_after == "A":
        P.emit()
        return nc

    S2 = Arena(nc, A0, SBMAX)
    B_c2 = Buf()
    wuk = S2.t("wuk", [128, 4, 128], BF16)
    wuv = S2.t("wuv", [128, 8, 64], BF16)
    negDh = S2.t("negDh", [128, 2048], BF16)
    negDl = S2.t("negDl", [128, 2048], BF16)
    slopeI = S2.t("slopeI", [128, 8, 128], BF16)
    biasB = S2.t("biasB", [128, 8, 640], F32)
    maskB = S2.t("maskB", [128, 640], F32)
    P.dma("pool", wuk[:], wuk_d, writes=[B_c2])
    P.dma("pool", wuv[:], wuv_d.rearrange("h r d -> r h d"), writes=[B_c2])
    P.dma("sp", negDh[:], negdh_d, writes=[B_c2])
    P.dma("sp", negDl[:], negdl_d, writes=[B_c2])
    P.dma("sp", slopeI[:], slopei_d, writes=[B_c2])
    P.dma("sp", maskB[:], maskb_d, writes=[B_c2])
    for h in range(8):
        P.dma("sp", biasB[:, h, :], relb_d[h], writes=[B_c2])
    for h in range(8):
        P.op("dve", lambda hh, h=h: hh.tensor_tensor(out=biasB[:, h, :], in0=biasB[:, h, :], in1=maskB[:], op=ALU.add),
             reads=[B_c2], writes=[B_c2])

    kidx2_r = Rot(S2, "kidx2", [128, 2048], BF16, 1)
    ckvT_r = Rot(S2, "ckvTr", [128, 2048], BF16, 1)
    ckvtm_r = Rot(S2, "ckvtmr", [128, 16, 128], BF16, 1)
    isc2 = [S2.t("isc%d" % i, [128, 2048], F32) for i in range(2)]
    B_isc2 = [[Buf() for _ in range(4)] for _ in range(2)]
    negm2 = [S2.t("negm%d" % i, [128, 2048], BF16) for i in range(2)]
    B_negm2 = [Buf(), Buf()]
    bs_r = Rot(S2, "bs", [128, 8], F32, 2)
    dl_r = Rot(S2, "dl", [128, 24], F32, 2)
    cn_r = Rot(S2, "cn", [128, 24], F32, 2)
    jk_r = Rot(S2, "jk", [128, 2048], BF16, 2)
    pow2 = S2.t("pow2", [128, 24], F32)
    P.dma("sp", pow2[:, 0:21], pow2_d, writes=[B_c2])
    qi_r = Rot(S2, "qi", [128, 4, 128], BF16, 2)
    qa_r = Rot(S2, "qa", [128, 4, 128], BF16, 2)
    wt_r = Rot(S2, "wt", [128, 8], F32, 2)
    rl_r = Rot(S2, "rl", [128, 512], F32, 2)
    m8_r = Rot(S2, "m8", [128, 8], F32, 2)
    qlat_r = Rot(S2, "qlat", [128, 128], BF16, 2)
    sm_r = Rot(S2, "sm", [128, 2048], F32, 2)
    pb_r = Rot(S2, "pb", [128, 2048], BF16, 2)
    pt_r = Rot(S2, "pt", [128, 2048], BF16, 2)
    st2_r = Rot(S2, "st2", [128, 4], F32, 8)
    mx_r = Rot(S2, "mx", [128, 4], F32, 4)
    rc_r = Rot(S2, "rc", [128, 8], F32, 2)
    olat_r = Rot(S2, "olat", [128, 1024], BF16, 1)
    olatT_r = Rot(S2, "olatT", [128, 1024], BF16, 1)
    yas_r = Rot(S2, "yas", [64, 1024], BF16, 2)
    qb_r = Rot(S2, "qb", [128, 4, 128], BF16, 2)
    kb_r = Rot(S2, "kb", [128, 4, 640], BF16, 2)
    vb_r = Rot(S2, "vb", [128, 5, 512], BF16, 2)
    sB_r = Rot(S2, "sB", [128, 640], F32, 2)
    pB_r = Rot(S2, "pB", [128, 640], BF16, 2)
    ptB_r = Rot(S2, "ptB", [128, 640], BF16, 2)
    yb_r = Rot(S2, "yb", [128, 512], BF16, 1)
    ybs_r = Rot(S2, "ybs", [128, 512], BF16, 2)
    SLOPES = [2.0 ** (-(h + 1)) for h in range(8)]
    yaT_v = yaT_s.rearrange("j (hp d) t -> d (j hp) t", hp=2)
    cpy = [0]

    def evac(out, in_, reads, writes):
        cpy[0] += 1
        if cpy[0] % 2:
            P.op("act", lambda h: h.copy(out=out, in_=in_), reads=reads, writes=writes)
        else:
            P.op("dve", lambda h: h.tensor_copy(out=out, in_=in_), reads=reads, writes=writes)

    NBIS = 20

    def indexer(seq, t, kidx2, Bk):
        par = t % 2
        isc, B_isc = isc2[par], B_isc2[par]
        tg = seq * 16 + t
        tok = slice(tg * 128, (tg + 1) * 128)
        S = (t + 1) * 128
        chunks = [(c * 512, min(512, S - c * 512)) for c in range((S + 511) // 512)]
        qi, Bqi = qi_r.next()
        wt, Bwt = wt_r.next()
        P.dma("sp", qi[:], featT[4:8, :, tok].rearrange("c p t -> p c t"), reads=[b for c in range(4, 8) for b in tb(("f", c), tg, tg + 1)], writes=[Bqi])
        P.dma("sp", wt[:], widx_s[tok, :], reads=tb("widx", tg, tg + 1), writes=[Bwt])
        for h in range(8):
            hp, j = h % 2, h // 2
            for ci, (c0, cs) in enumerate(chunks):
                ps, Bp = PSF()
                P.op("pe", lambda hh, ps=ps, cs=cs, c0=c0, hp=hp, j=j, qi=qi: hh.matmul(
                    ps[:, 0:cs], lhsT=qi[hp * 64:(hp + 1) * 64, j, :], rhs=kidx2[hp * 64:(hp + 1) * 64, c0:c0 + cs],
                    start=True, stop=True), reads=[Bqi, Bk], writes=[Bp])
                rl, Brl = rl_r.next()
                P.op("act", lambda hh, rl=rl, ps=ps, cs=cs: hh.activation(out=rl[:, 0:cs], in_=ps[:, 0:cs], func=AF.Relu),
                     reads=[Bp], writes=[Brl])
                if h == 0:
                    P.op("dve", lambda hh, rl=rl, cs=cs, c0=c0, wt=wt: hh.tensor_scalar(
                        out=isc[:, c0:c0 + cs], in0=rl[:, 0:cs], scalar1=wt[:, 0:1], scalar2=None, op0=ALU.mult),
                        reads=[Brl, Bwt], writes=[B_isc[ci]])
                else:
                    P.op("dve", lambda hh, rl=rl, cs=cs, c0=c0, wt=wt, h=h: hh.scalar_tensor_tensor(
                        out=isc[:, c0:c0 + cs], in0=rl[:, 0:cs], scalar=wt[:, h:h + 1], in1=isc[:, c0:c0 + cs],
                        op0=ALU.mult, op1=ALU.add), reads=[Brl, Bwt, B_isc[ci]], writes=[B_isc[ci]])
                yield
        P.op("dve", lambda hh: hh.memset(isc[0:64, t * 128 + 64:(t + 1) * 128], -1e30),
             reads=B_isc[:len(chunks)], writes=B_isc[:len(chunks)])

    def thresh_gen(t):
        par = t % 2
        isc, B_isc, negm, B_negm = isc2[par], B_isc2[par], negm2[par], B_negm2[par]
        S = (t + 1) * 128
        nch = (S + 511) // 512
        Bi = B_isc[:nch]
        if t < 2:
            P.op("dve", lambda hh: hh.tensor_scalar(
                out=negm[:, 0:S], in0=isc[:, 0:S], scalar1=-1e29, scalar2=NEG, op0=ALU.is_lt, op1=ALU.mult),
                reads=Bi, writes=[B_negm])
            return
        bs, Bbs = bs_r.next()
        dl, Bdl = dl_r.next()
        cn, Bcn = cn_r.next()
        P.op("dve", lambda hh: hh.tensor_reduce(out=bs[:, 0:1], in_=isc[:, 0:S], axis=AX.X, op=ALU.max), reads=Bi, writes=[Bbs])
        yield
        P.op("dve", lambda hh: hh.tensor_reduce(out=bs[:, 1:2], in_=isc[:, 0:S - 128], axis=AX.X, op=ALU.min), reads=Bi, writes=[Bbs])
        yield
        P.op("dve", lambda hh: hh.tensor_tensor(out=bs[:, 2:3], in0=bs[:, 0:1], in1=bs[:, 1:2], op=ALU.subtract), reads=[Bbs], writes=[Bbs])
        P.op("dve", lambda hh: hh.tensor_tensor(out=bs[:, 3:4], in0=bs[:, 0:1], in1=bs[:, 1:2], op=ALU.add), reads=[Bbs], writes=[Bbs])
        P.op("dve", lambda hh: hh.tensor_scalar(out=bs[:, 4:5], in0=bs[:, 3:4], scalar1=-0.5, scalar2=None, op0=ALU.mult), reads=[Bbs], writes=[Bbs])
        P.op("dve", lambda hh: hh.tensor_scalar(out=dl[:, :], in0=pow2[:, :], scalar1=bs[:, 2:3], scalar2=None, op0=ALU.mult),
             reads=[Bbs, B_c2], writes=[Bdl])
        yield
        for i in range(NBIS):
            a, b = 4 + (i % 2), 4 + ((i + 1) % 2)
            jk, Bjk = jk_r.next()
            P.op("act", lambda hh, jk=jk, a=a, i=i: hh.activation(
                out=jk[:, 0:S], in_=isc[:, 0:S], func=AF.Sign, bias=bs[:, a:a + 1], scale=1.0, accum_out=cn[:, i:i + 1]),
                reads=Bi + [Bbs], writes=[Bjk, Bcn])
            yield
            P.op("dve", lambda hh, i=i: hh.tensor_scalar(out=bs[:, 6:7], in0=cn[:, i:i + 1], scalar1=511.5 - S, scalar2=-0.5,
                                                         op0=ALU.is_le, op1=ALU.add), reads=[Bcn], writes=[Bbs])
            P.op("dve", lambda hh, i=i, a=a, b=b: hh.scalar_tensor_tensor(
                out=bs[:, b:b + 1], in0=bs[:, 6:7], scalar=dl[:, i:i + 1], in1=bs[:, a:a + 1], op0=ALU.mult, op1=ALU.add),
                reads=[Bbs, Bdl], writes=[Bbs])
            yield
        f = 4 + (NBIS % 2)
        P.op("dve", lambda hh: hh.scalar_tensor_tensor(
            out=bs[:, 7:8], in0=bs[:, f:f + 1], scalar=-1.0, in1=dl[:, NBIS:NBIS + 1], op0=ALU.mult, op1=ALU.subtract),
            reads=[Bbs, Bdl], writes=[Bbs])
        P.op("dve", lambda hh: hh.tensor_scalar(
            out=negm[:, 0:S], in0=isc[:, 0:S], scalar1=bs[:, 7:8], scalar2=NEG, op0=ALU.is_lt, op1=ALU.mult),
            reads=Bi + [Bbs], writes=[B_negm])

    def run_pipelined(head_gen):
        gens = [head_gen(h) for h in range(8)]
        next(gens[0])
        for h in range(8):
            if h + 1 < 8:
                next(gens[h + 1])
            for _ in gens[h]:
                pass

    class BG:
        def __init__(self, makers):
            self.makers = makers
            self.cur = 0
            self.gen = None
            self.limit = -1

        def _one(self):
            if self.cur >= len(self.makers):
                return False
            if self.gen is None:
                self.gen = self.makers[self.cur]()
            try:
                next(self.gen)
            except StopIteration:
                self.gen = None
                self.cur += 1
            return True

        def step(self):
            if self.cur <= self.limit:
                self._one()

        def force(self, item):
            while self.cur <= item and self.cur < len(self.makers):
                self._one()

    bgref = [None]

    bgB = [None]

    def sp(n=1):
        if bgref[0] is not None:
            for _ in range(n):
                bgref[0].step()
        if bgB[0] is not None:
            try:
                next(bgB[0])
            except StopIteration:
                bgB[0] = None

    def drive_stage(g):
        while True:
            try:
                v = next(g)
            except StopIteration:
                return
            if v == 'S':
                return
            yield

    def run_pipelined_gen(head_gen):
        gens = [head_gen(h) for h in range(8)]
        yield from drive_stage(gens[0])
        for h in range(8):
            if h + 1 < 8:
                yield from drive_stage(gens[h + 1])
            yield from drive_stage(gens[h])

    def advance(gen, n):
        if gen is None:
            return
        for _ in range(n):
            try:
                next(gen)
            except StopIteration:
                return

    def attention(seq, t, ckvT, BcT, ckvtm, Bcm, gnext):
        par = t % 2
        negm, B_negm = negm2[par], B_negm2[par]
        tg = seq * 16 + t
        tok = slice(tg * 128, (tg + 1) * 128)
        S = (t + 1) * 128
        nb = t + 1
        chunks = [(c * 512, min(512, S - c * 512)) for c in range((S + 511) // 512)]
        qa, Bqa = qa_r.next()
        P.dma("sp", qa[:], featT[0:4, :, tok].rearrange("c p t -> p c t"), reads=[b for c in range(0, 4) for b in tb(("f", c), tg, tg + 1)], writes=[Bqa])
        olat, Bol = olat_r.next()
        rc, Brc = rc_r.next()
        off = 1920 - t * 128
        Bacc = [accA[1], accB[1]]
        def head_gen(h):
                sp()
                hp, j = h % 2, h // 2
                psq, Bpq = PSF()
                P.op("pe", lambda hh, psq=psq, hp=hp, j=j, qa=qa: hh.matmul(
                    psq[:, 0:128], lhsT=wuk[hp * 64:(hp + 1) * 64, j, :], rhs=qa[hp * 64:(hp + 1) * 64, j, :],
                    start=True, stop=True), reads=[Bqa, B_c2], writes=[Bpq])
                ql, Bql = qlat_r.next()
                P.op("act", lambda hh, ql=ql, psq=psq: hh.mul(out=ql[:], in_=psq[:, 0:128], mul=0.125),
                     reads=[Bpq], writes=[Bql])
                sp()
                sm, Bsm = sm_r.next()
                mx, Bmx = mx_r.next()
                for ci, (c0, cs) in enumerate(chunks):
                    ps, Bp = PSF()
                    P.op("pe", lambda hh, ps=ps, cs=cs, c0=c0, ql=ql: hh.matmul(
                        ps[:, 0:cs], lhsT=ql[:], rhs=ckvT[:, c0:c0 + cs], start=True, stop=False),
                        reads=[Bql, BcT], writes=[Bp])
                    P.op("pe", lambda hh, ps=ps, cs=cs, c0=c0: hh.matmul(
                        ps[:, 0:cs], lhsT=ident_b[:], rhs=negm[:, c0:c0 + cs], start=False, stop=False),
                        reads=[B_negm, B_const], writes=[Bp])
                    P.op("pe", lambda hh, ps=ps, cs=cs, c0=c0: hh.matmul(
                        ps[:, 0:cs], lhsT=slopeI[:, h, :], rhs=negDh[:, off + c0:off + c0 + cs], start=False, stop=False),
                        reads=[B_c2], writes=[Bp])
                    P.op("pe", lambda hh, ps=ps, cs=cs, c0=c0: hh.matmul(
                        ps[:, 0:cs], lhsT=slopeI[:, h, :], rhs=negDl[:, off + c0:off + c0 + cs], start=False, stop=True),
                        reads=[B_c2], writes=[Bp])
                    P.op("dve", lambda hh, ps=ps, cs=cs, c0=c0, sm=sm, mx=mx, ci=ci: hh.tensor_scalar(
                        out=sm[:, c0:c0 + cs], in0=ps[:, 0:cs], scalar1=1.0, scalar2=-3.0e38, op0=ALU.mult, op1=ALU.max,
                        accum_out=mx[:, ci:ci + 1]), reads=[Bp], writes=[Bsm, Bmx])
                    sp()
                st, Bst = st2_r.next()
                P.op("dve", lambda hh, mx=mx, st=st: hh.tensor_reduce(out=st[:, 0:1], in_=mx[:, 0:len(chunks)], axis=AX.X, op=ALU.max, negate=True),
                     reads=[Bmx], writes=[Bst])
                sp()
                pb, Bpb = pb_r.next()
                P.op("act", lambda hh, sm=sm, st=st, pb=pb: hh.activation(
                    out=pb[:, 0:S], in_=sm[:, 0:S], func=AF.Exp, bias=st[:, 0:1], scale=1.0, accum_out=st[:, 1:2]),
                    reads=[Bsm, Bst], writes=[Bpb, Bst])
                P.op("dve", lambda hh, st=st, rc=rc, h=h: hh.reciprocal(out=rc[:, h:h + 1], in_=st[:, 1:2]), reads=[Bst], writes=[Brc])
                sp()
                yield
                sp()
                pt, Bpt = pt_r.next()
                for b0 in range(0, nb, 8):
                    nbb = min(8, nb - b0)
                    pT, BpT = PSB()
                    for b in range(nbb):
                        P.op("pe", lambda hh, pT=pT, b=b, b0=b0, pb=pb: hh.transpose(
                            out=pT[:, b * 128:(b + 1) * 128], in_=pb[:, (b0 + b) * 128:(b0 + b + 1) * 128], identity=ident_b[:]),
                            reads=[Bpb, B_const], writes=[BpT])
                    evac(pt[:, b0 * 128:(b0 + nbb) * 128], pT[:, 0:nbb * 128], [BpT], [Bpt])
                    sp()
                acc = (accA if h < 4 else accB)[0]
                for b in range(nb):
                    P.op("pe", lambda hh, acc=acc, b=b, h=h, pt=pt: hh.matmul(
                        acc[:, (h % 4) * 128:(h % 4 + 1) * 128], lhsT=pt[:, b * 128:(b + 1) * 128], rhs=ckvtm[:, b, :],
                        start=(b == 0), stop=(b == nb - 1)), reads=[Bpt, Bcm], writes=[Bacc[h // 4]])
        run_pipelined(head_gen)
        for h in range(8):
            acc = (accA if h < 4 else accB)[0]
            P.op("dve", lambda hh, acc=acc, h=h, rc=rc, olat=olat: hh.tensor_scalar(
                out=olat[:, h * 128:(h + 1) * 128], in0=acc[:, (h % 4) * 128:(h % 4 + 1) * 128], scalar1=rc[:, h:h + 1],
                scalar2=None, op0=ALU.mult), reads=[Bacc[h // 4], Brc], writes=[Bol])
        olT, BolT = olatT_r.next()
        pT, BpT = PSB()
        for h in range(8):
            P.op("pe", lambda hh, pT=pT, h=h, olat=olat: hh.transpose(
                out=pT[:, h * 128:(h + 1) * 128], in_=olat[:, h * 128:(h + 1) * 128], identity=ident_b[:]),
                reads=[Bol, B_const], writes=[BpT])
        evac(olT[:, :], pT[:, :], [BpT], [BolT])
        yas, Bya = yas_r.next()
        for half in range(2):
            ps, Bp = PSF()
            for hh_ in range(4):
                h = half * 4 + hh_
                P.op("pe", lambda hh, ps=ps, h=h, hh_=hh_, olT=olT: hh.matmul(
                    ps[0:64, hh_ * 128:(hh_ + 1) * 128], lhsT=wuv[:, h, :], rhs=olT[:, h * 128:(h + 1) * 128],
                    start=True, stop=True), reads=[BolT, B_c2], writes=[Bp])
            evac(yas[:, half * 512:(half + 1) * 512], ps[0:64, :], [Bp], [Bya])
        P.dma("pool", yaT_v[:, :, tok], yas[:].rearrange("p (c t) -> p c t", c=8), reads=[Bya], writes=tb("yaT", tg, tg + 1))

    def mixerB(seq, t):
        tg = seq * 16 + t
        tok = slice(tg * 128, (tg + 1) * 128)
        nk = min(640, (t + 1) * 128)
        c0 = 640 - nk
        ks = seq * SEQ + (t + 1) * 128 - nk
        nbk = nk // 128
        kt0 = ks // 128
        qb, Bqb = qb_r.next()
        kb, Bkb = kb_r.next()
        vb, Bvb = vb_r.next()
        P.dma("sp", qb[:], featT[8:12, :, tok].rearrange("c p t -> p c t"),
              reads=[b for c in range(8, 12) for b in tb(("f", c), tg, tg + 1)], writes=[Bqb])
        P.dma("sp", kb[:, :, 0:nk], featT[12:16, :, ks:ks + nk].rearrange("c p t -> p c t"),
              reads=[b for c in range(12, 16) for b in tb(("f", c), kt0, kt0 + nbk)], writes=[Bkb])
        P.dma("sp", vb[:, 0:nbk, :], vb_s[ks:ks + nk, :].rearrange("(b p) f -> p b f", p=128),
              reads=tb("vb", kt0, kt0 + nbk), writes=[Bvb])
        rc, Brc = rc_r.next()
        n1 = min(nk, 512)
        Bacc = accC[1]
        yield
        def head_gen(h):
                hp, j = h % 2, h // 2
                sB, BsB = sB_r.next()
                parts = [(0, n1)] + ([(512, 128)] if nk > 512 else [])
                for (k0, kn) in parts:
                    ps, Bp = PSF()
                    P.op("pe", lambda hh, ps=ps, k0=k0, kn=kn, hp=hp, j=j, qb=qb, kb=kb: hh.matmul(
                        ps[:, 0:kn], lhsT=qb[hp * 64:(hp + 1) * 64, j, :], rhs=kb[hp * 64:(hp + 1) * 64, j, k0:k0 + kn],
                        start=True, stop=True), reads=[Bqb, Bkb], writes=[Bp])
                    P.op("dve", lambda hh, ps=ps, k0=k0, kn=kn, sB=sB, h=h: hh.scalar_tensor_tensor(
                        out=sB[:, k0:k0 + kn], in0=ps[:, 0:kn], scalar=0.125, in1=biasB[:, h, c0 + k0:c0 + k0 + kn],
                        op0=ALU.mult, op1=ALU.add), reads=[Bp, B_c2], writes=[BsB])
                yield
                st, Bst = st2_r.next()
                P.op("dve", lambda hh, sB=sB, st=st: hh.tensor_reduce(out=st[:, 0:1], in_=sB[:, 0:nk], axis=AX.X, op=ALU.max, negate=True),
                     reads=[BsB], writes=[Bst])
                pB, BpB = pB_r.next()
                P.op("act", lambda hh, sB=sB, st=st, pB=pB: hh.activation(
                    out=pB[:, 0:nk], in_=sB[:, 0:nk], func=AF.Exp, bias=st[:, 0:1], scale=1.0, accum_out=st[:, 1:2]),
                    reads=[BsB, Bst], writes=[BpB, Bst])
                yield
                P.op("dve", lambda hh, st=st, rc=rc, h=h: hh.reciprocal(out=rc[:, h:h + 1], in_=st[:, 1:2]), reads=[Bst], writes=[Brc])
                yield
                yield 'S'
                ptB, BptB = ptB_r.next()
                pT, BpT = PSB()
                for b in range(nbk):
                    P.op("pe", lambda hh, pT=pT, b=b, pB=pB: hh.transpose(
                        out=pT[:, b * 128:(b + 1) * 128], in_=pB[:, b * 128:(b + 1) * 128], identity=ident_b[:]),
                        reads=[BpB, B_const], writes=[BpT])
                evac(ptB[:, 0:nbk * 128], pT[:, 0:nbk * 128], [BpT], [BptB])
                yield
                for b in range(nbk):
                    P.op("pe", lambda hh, b=b, h=h, ptB=ptB, vb=vb: hh.matmul(
                        accC[0][:, h * 64:(h + 1) * 64], lhsT=ptB[:, b * 128:(b + 1) * 128], rhs=vb[:, b, h * 64:(h + 1) * 64],
                        start=(b == 0), stop=(b == nbk - 1)), reads=[BptB, Bvb], writes=[Bacc])
        yield from run_pipelined_gen(head_gen)
        yb, Byb = yb_r.next()
        for h in range(8):
            P.op("dve", lambda hh, h=h, rc=rc, yb=yb: hh.tensor_scalar(
                out=yb[:, h * 64:(h + 1) * 64], in0=accC[0][:, h * 64:(h + 1) * 64], scalar1=rc[:, h:h + 1],
                scalar2=None, op0=ALU.mult), reads=[Bacc, Brc], writes=[Byb])
        yield
        pT, BpT = PSB()
        for b in range(4):
            P.op("pe", lambda hh, pT=pT, b=b, yb=yb: hh.transpose(
                out=pT[:, b * 128:(b + 1) * 128], in_=yb[:, b * 128:(b + 1) * 128], identity=ident_b[:]),
                reads=[Byb, B_const], writes=[BpT])
        ybs, Bybs = ybs_r.next()
        evac(ybs[:, :], pT[:, 0:512], [BpT], [Bybs])
        P.dma("pool", ybT_s[:, :, tok].rearrange("c p t -> p c t"), ybs[:].rearrange("p (c t) -> p c t", c=4), reads=[Bybs], writes=tb("ybT", tg, tg + 1))

    for seq in range(nseq):
        kidx2, Bk = kidx2_r.next()
        ckvT, BcT = ckvT_r.next()
        ckvtm, Bcm = ckvtm_r.next()
        sl = slice(seq * SEQ, (seq + 1) * SEQ)
        P.dma("sp", kidx2[0:64, :], kidxT_s[:, sl], reads=tb("kidxT", seq * 16, seq * 16 + 16), writes=[Bk])
        P.dma("sp", kidx2[64:128, :], kidxT_s[:, sl], reads=tb("kidxT", seq * 16, seq * 16 + 16), writes=[Bk])
        P.dma("sp", ckvT[:], ckvT_s[:, sl], reads=tb("ckvT", seq * 16, seq * 16 + 16), writes=[BcT])
        P.dma("sp", ckvtm[:], ckvtm_s[sl, :].rearrange("(b p) r -> p b r", p=128),
              reads=tb("ckvtm", seq * 16, seq * 16 + 16), writes=[Bcm])
        if "A" in mixers:
            makers = []
            for t in range(16):
                makers.append(lambda t=t: indexer(seq, t, kidx2, Bk))
                makers.append(lambda t=t: thresh_gen(t))
            bg = BG(makers)
            bgref[0] = bg
        for t in range(16):
            if "A" in mixers:
                bg.force(2 * t + 1)
                bg.limit = 2 * (t + 2)
                if "B" in mixers:
                    bgB[0] = mixerB(seq, t)
                attention(seq, t, ckvT, BcT, ckvtm, Bcm, None)
                while bgB[0] is not None:
                    sp(0)
            elif "B" in mixers:
                for _ in mixerB(seq, t):
                    pass
        bgref[0] = None
    P.barrier()
    if stop_after == "CD":
        P.emit()
        return nc

    S3 = Arena(nc, A0, SBMAX)
    B_c3 = Buf()
    wa = S3.t("wa", [128, 4, D], BF16)
    wb = S3.t("wb", [128, 4, D], BF16)
    wo = S3.t("wo", [128, 8, D], BF16)
    ln1g = S3.t("ln1g", [128, D], F32)
    ln1b = S3.t("ln1b", [128, D], F32)
    wr = S3.t("wr", [128, 8, NEXP], BF16)
    brt = S3.t("brt", [128, NEXP], F32)
    P.dma("pool", wa[:], wa_d.rearrange("(k p) n -> p k n", p=128), writes=[B_c3])
    P.dma("pool", wb[:], wb_d.rearrange("(k p) n -> p k n", p=128), writes=[B_c3])
    P.dma("pool", wo[:], wo_d.rearrange("(k p) n -> p k n", p=128), writes=[B_c3])
    P.dma("sp", ln1g[:], ln1g_d, writes=[B_c3])
    P.dma("sp", ln1b[:], ln1b_d, writes=[B_c3])
    P.dma("pool", wr[:], wr_d.rearrange("(k p) n -> p k n", p=128), writes=[B_c3])
    P.dma("sp", brt[:], br_d, writes=[B_c3])
    yag_r = Rot(S3, "yag", [128, 4, 512], BF16, 2)
    ybg_r = Rot(S3, "ybg", [128, 4, 512], BF16, 2)
    sga_r = Rot(S3, "sga", [128, 8, 512], BF16, 2)
    sgb_r = Rot(S3, "sgb", [128, 8, 512], BF16, 2)
    t1_r = Rot(S3, "t1", [128, 512], F32, 2)
    t2_r = Rot(S3, "t2", [128, 512], F32, 2)
    mT_r = Rot(S3, "mT", [128, 8, 512], BF16, 2)
    xr_r = Rot(S3, "xr", [128, D], F32, 2)
    z_r = Rot(S3, "z", [128, D], F32, 2)
    x1t_r = Rot(S3, "x1t", [128, D], F32, 2)
    x1b_r = Rot(S3, "x1b", [128, 1024], BF16, 2)
    x1Tb_r = Rot(S3, "x1Tb", [128, 1024], BF16, 2)
    st3_r = Rot(S3, "st3", [128, 16], F32, 4)
    lg_r = Rot(S3, "lg", [128, NEXP], F32, 2)
    ex_r = Rot(S3, "ex", [128, NEXP], F32, 2)
    gs_r = Rot(S3, "gs", [128, NEXP], F32, 2)
    m8b_r = Rot(S3, "m8b", [128, 8], F32, 2)
    selb_r = Rot(S3, "selb", [128, NEXP], BF16, 2)
    rw_r = Rot(S3, "rw", [128, 4 * NEXP], F32, 2)
    sf_r = Rot(S3, "sf", [128, 12], F32, 3)
    su_r = Rot(S3, "su", [128, 1], U32, 8)
    ustr = S3.t("ustr", [128, 128], BF16)
    onesb = S3.t("onesb", [128, 128], BF16)
    ebase = S3.t("ebase", [128, NEXP], F32)
    P.dma("sp", ustr[:], ustr_d, writes=[B_c3])
    P.dma("sp", onesb[:], onesb_d, writes=[B_c3])
    P.dma("sp", ebase[:], ebase_d, writes=[B_c3])
    rt_tiles = [(S3.t("rt0", [128, NEXP], F32), Buf()), (S3.t("rt1", [128, NEXP], F32), Buf())]
    P.op("dve", lambda h: h.memset(rt_tiles[0][0][:], 0.0), writes=[rt_tiles[0][1]])

    def layer_norm(z, Bz, g_t, b_t, Bg, out, Bout, st_rot):
        st, Bst = st_rot.next()
        P.op("dve", lambda h: h.bn_stats(out=st[:, 0:6], in_=z[:, 0:512]), reads=[Bz], writes=[Bst])
        P.op("dve", lambda h: h.bn_stats(out=st[:, 6:12], in_=z[:, 512:1024]), reads=[Bz], writes=[Bst])
        P.op("dve", lambda h: h.bn_aggr(out=st[:, 12:14], in_=st[:, 0:12]), reads=[Bst], writes=[Bst])
        P.op("act", lambda h: h.activation(out=st[:, 14:15], in_=st[:, 13:14], func=AF.Sqrt, scale=1.0, bias=eps_t[:, 0:1]),
             reads=[Bst, B_const], writes=[Bst])
        P.op("dve", lambda h: h.reciprocal(out=st[:, 15:16], in_=st[:, 14:15]), reads=[Bst], writes=[Bst])
        P.op("dve", lambda h: h.tensor_scalar(out=z[:], in0=z[:], scalar1=st[:, 12:13], scalar2=st[:, 15:16],
                                              op0=ALU.subtract, op1=ALU.mult), reads=[Bz, Bst], writes=[Bz])
        P.op("dve", lambda h: h.tensor_tensor(out=z[:], in0=z[:], in1=g_t[:], op=ALU.mult), reads=[Bz, Bg], writes=[Bz])
        P.op("dve", lambda h: h.tensor_tensor(out=out[:], in0=z[:], in1=b_t[:], op=ALU.add), reads=[Bz, Bg], writes=[Bout])

    for g in range(TOK // 512):
        gs_ = slice(g * 512, (g + 1) * 512)
        t0 = g * 4
        yag, Byag = yag_r.next()
        ybg, Bybg = ybg_r.next()
        sga, Bsga = sga_r.next()
        sgb, Bsgb = sgb_r.next()
        P.dma("sp", yag[:], yaT_s[:, :, gs_].rearrange("c p t -> p c t"), reads=tb("yaT", t0, t0 + 4), writes=[Byag])
        P.dma("sp", ybg[:], ybT_s[:, :, gs_].rearrange("c p t -> p c t"), reads=tb("ybT", t0, t0 + 4), writes=[Bybg])
        P.dma("sp", sga[:], featT[16:24, :, gs_].rearrange("c p t -> p c t"),
              reads=[b for c in range(16, 24) for b in tb(("f", c), t0, t0 + 4)], writes=[Bsga])
        P.dma("sp", sgb[:], featT[24:32, :, gs_].rearrange("c p t -> p c t"),
              reads=[b for c in range(24, 32) for b in tb(("f", c), t0, t0 + 4)], writes=[Bsgb])
        mT, BmT = mT_r.next()
        for n in range(8):
            pa, Bpa = PSF()
            for k in range(4):
                P.op("pe", lambda h, pa=pa, k=k, n=n, yag=yag: h.matmul(
                    pa[:, :], lhsT=wa[:, k, n * 128:(n + 1) * 128], rhs=yag[:, k, :], start=(k == 0), stop=(k == 3)),
                    reads=[B_c3, Byag], writes=[Bpa])
            pb_, Bpb_ = PSF()
            for k in range(4):
                P.op("pe", lambda h, pb_=pb_, k=k, n=n, ybg=ybg: h.matmul(
                    pb_[:, :], lhsT=wb[:, k, n * 128:(n + 1) * 128], rhs=ybg[:, k, :], start=(k == 0), stop=(k == 3)),
                    reads=[B_c3, Bybg], writes=[Bpb_])
            t1, Bt1 = t1_r.next()
            t2, Bt2 = t2_r.next()
            P.op("dve", lambda h, t1=t1, pa=pa, n=n, sga=sga: h.tensor_tensor(out=t1[:], in0=pa[:, :], in1=sga[:, n, :], op=ALU.mult),
                 reads=[Bpa, Bsga], writes=[Bt1])
            P.op("dve", lambda h, t2=t2, pb_=pb_, n=n, sgb=sgb: h.tensor_tensor(out=t2[:], in0=pb_[:, :], in1=sgb[:, n, :], op=ALU.mult),
                 reads=[Bpb_, Bsgb], writes=[Bt2])
            P.op("dve", lambda h, t1=t1, t2=t2, n=n, mT=mT: h.tensor_tensor(out=mT[:, n, :], in0=t1[:], in1=t2[:], op=ALU.add),
                 reads=[Bt1, Bt2], writes=[BmT])
        for tt in range(4):
            t = t0 + tt
            tok = slice(t * 128, (t + 1) * 128)
            xr, Bxr = xr_r.next()
            P.dma("sp", xr[:], x_d[tok, :], writes=[Bxr])
            z, Bz = z_r.next()
            for nh in range(2):
                po, Bpo = PSF()
                for k in range(8):
                    P.op("pe", lambda h, po=po, k=k, nh=nh, tt=tt, mT=mT: h.matmul(
                        po[:, :], lhsT=mT[:, k, tt * 128:(tt + 1) * 128], rhs=wo[:, k, nh * 512:(nh + 1) * 512],
                        start=(k == 0), stop=(k == 7)), reads=[BmT, B_c3], writes=[Bpo])
                P.op("dve", lambda h, po=po, nh=nh, xr=xr, z=z: h.scalar_tensor_tensor(
                    out=z[:, nh * 512:(nh + 1) * 512], in0=xr[:, nh * 512:(nh + 1) * 512], scalar=ALPHA, in1=po[:, :],
                    op0=ALU.mult, op1=ALU.add), reads=[Bpo, Bxr], writes=[Bz])
            x1t, Bx1 = x1t_r.next()
            layer_norm(z, Bz, ln1g, ln1b, B_c3, x1t, Bx1, st3_r)
            P.dma("pool", x1_s[tok, :], x1t[:], reads=[Bx1], writes=tb("x1", t, t + 1))
            x1b, Bx1b = x1b_r.next()
            P.op("act", lambda h, x1b=x1b, x1t=x1t: h.copy(out=x1b[:], in_=x1t[:]), reads=[Bx1], writes=[Bx1b])
            x1Tb, Bxb = x1Tb_r.next()
            pt_, Bpt_ = PSB()
            for b in range(8):
                P.op("pe", lambda h, pt_=pt_, b=b, x1b=x1b: h.transpose(
                    out=pt_[:, b * 128:(b + 1) * 128], in_=x1b[:, b * 128:(b + 1) * 128], identity=ident_b[:]),
                    reads=[Bx1b, B_const], writes=[Bpt_])
            evac(x1Tb[:, :], pt_[:, :], [Bpt_], [Bxb])
            P.dma("pool", x1T_s[:, :, tok].rearrange("c p t -> p c t"), x1Tb[:].rearrange("p (c t) -> p c t", c=8), reads=[Bxb], writes=tb("x1T", t, t + 1))
            pr, Bpr = PSF()
            for k in range(8):
                P.op("pe", lambda h, pr=pr, k=k, x1Tb=x1Tb: h.matmul(
                    pr[:, 0:NEXP], lhsT=x1Tb[:, k * 128:(k + 1) * 128], rhs=wr[:, k, :], start=(k == 0), stop=(k == 7)),
                    reads=[Bxb, B_c3], writes=[Bpr])
            lg, Blg = lg_r.next()
            P.op("dve", lambda h, lg=lg, pr=pr: h.tensor_tensor(out=lg[:], in0=pr[:, 0:NEXP], in1=brt[:], op=ALU.add),
                 reads=[Bpr, B_c3], writes=[Blg])
            m8, Bm8 = m8b_r.next()
            P.op("dve", lambda h, lg=lg, m8=m8: h.max(out=m8[:, 0:8], in_=lg[:]), reads=[Blg], writes=[Bm8])
            st, Bst = st3_r.next()
            P.op("dve", lambda h, m8=m8, st=st: h.tensor_scalar(out=st[:, 0:1], in0=m8[:, 0:1], scalar1=-1.0, scalar2=None, op0=ALU.mult),
                 reads=[Bm8], writes=[Bst])
            ex, Bex = ex_r.next()
            P.op("act", lambda h, ex=ex, lg=lg, st=st: h.activation(out=ex[:], in_=lg[:], func=AF.Exp, bias=st[:, 0:1], scale=1.0),
                 reads=[Blg, Bst], writes=[Bex])
            gs, Bgs = gs_r.next()
            P.op("dve", lambda h, gs=gs, lg=lg, m8=m8, ex=ex: h.scalar_tensor_tensor(
                out=gs[:], in0=lg[:], scalar=m8[:, 3:4], in1=ex[:], op0=ALU.is_ge, op1=ALU.mult),
                reads=[Blg, Bm8, Bex], writes=[Bgs])
            P.op("dve", lambda h, gs=gs, st=st: h.tensor_reduce(out=st[:, 1:2], in_=gs[:], axis=AX.X, op=ALU.add),
                 reads=[Bgs], writes=[Bst])
            P.op("dve", lambda h, st=st: h.reciprocal(out=st[:, 2:3], in_=st[:, 1:2]), reads=[Bst], writes=[Bst])
            P.op("dve", lambda h, gs=gs, st=st: h.tensor_scalar(out=gs[:], in0=gs[:], scalar1=st[:, 2:3], scalar2=None, op0=ALU.mult),
                 reads=[Bgs, Bst], writes=[Bgs])
            selb, Bsel = selb_r.next()
            P.op("dve", lambda h, selb=selb, gs=gs: h.tensor_scalar(out=selb[:], in0=gs[:], scalar1=0.0, scalar2=None, op0=ALU.is_gt),
                 reads=[Bgs], writes=[Bsel])
            pp, Bpp = PSF()
            P.op("pe", lambda h, pp=pp, selb=selb: h.matmul(pp[:, 0:NEXP], lhsT=ustr[:], rhs=selb[:], start=True, stop=True),
                 reads=[Bsel, B_c3], writes=[Bpp])
            P.op("pe", lambda h, pp=pp, selb=selb: h.matmul(pp[:, NEXP:2 * NEXP], lhsT=onesb[:], rhs=selb[:], start=True, stop=True),
                 reads=[Bsel, B_c3], writes=[Bpp])
            rt_old, Brt_old = rt_tiles[t % 2]
            rt_new, Brt_new = rt_tiles[(t + 1) % 2]
            rw, Brw = rw_r.next()
            P.op("dve", lambda h, rw=rw, pp=pp, rt_old=rt_old: h.tensor_tensor(out=rw[:, 0:NEXP], in0=pp[:, 0:NEXP], in1=rt_old[:], op=ALU.add),
                 reads=[Bpp, Brt_old], writes=[Brw])
            P.op("dve", lambda h, pp=pp, rt_old=rt_old, rt_new=rt_new: h.tensor_tensor(out=rt_new[:], in0=pp[:, NEXP:2 * NEXP], in1=rt_old[:], op=ALU.add),
                 reads=[Bpp, Brt_old], writes=[Brt_new])
            P.op("dve", lambda h, rw=rw: h.tensor_scalar(out=rw[:, 2 * NEXP:3 * NEXP], in0=rw[:, 0:NEXP], scalar1=float(CAP), scalar2=100000.0,
                                                         op0=ALU.is_ge, op1=ALU.mult), reads=[Brw], writes=[Brw])
            P.op("dve", lambda h, rw=rw: h.tensor_tensor(out=rw[:, NEXP:2 * NEXP], in0=rw[:, 0:NEXP], in1=ebase[:], op=ALU.add),
                 reads=[Brw, B_c3], writes=[Brw])
            P.op("dve", lambda h, rw=rw: h.tensor_tensor(out=rw[:, NEXP:2 * NEXP], in0=rw[:, NEXP:2 * NEXP], in1=rw[:, 2 * NEXP:3 * NEXP], op=ALU.add),
                 reads=[Brw], writes=[Brw])
            P.op("dve", lambda h, rw=rw: h.tensor_scalar(out=rw[:, 2 * NEXP:3 * NEXP], in0=rw[:, NEXP:2 * NEXP], scalar1=-1.0, scalar2=BIGM,
                                                         op0=ALU.mult, op1=ALU.add), reads=[Brw], writes=[Brw])
            P.op("dve", lambda h, rw=rw, selb=selb: h.tensor_tensor(out=rw[:, 3 * NEXP:4 * NEXP], in0=rw[:, 2 * NEXP:3 * NEXP], in1=selb[:], op=ALU.mult),
                 reads=[Brw, Bsel], writes=[Brw])
            k8, Bk8 = m8b_r.next()
            P.op("dve", lambda h, rw=rw, k8=k8: h.max(out=k8[:, 0:8], in_=rw[:, 3 * NEXP:4 * NEXP]), reads=[Brw], writes=[Bk8])
            sf, Bsf = sf_r.next()
            P.op("dve", lambda h, sf=sf, k8=k8: h.tensor_scalar(out=sf[:, 0:4], in0=k8[:, 0:4], scalar1=-1.0, scalar2=BIGM, op0=ALU.mult, op1=ALU.add),
                 reads=[Bk8], writes=[Bsf])
            for k in range(4):
                P.op("dve", lambda h, rw=rw, sf=sf, gs=gs, k=k: h.scalar_tensor_tensor(
                    out=rw[:, 2 * NEXP:3 * NEXP], in0=rw[:, NEXP:2 * NEXP], scalar=sf[:, k:k + 1], in1=gs[:],
                    op0=ALU.is_equal, op1=ALU.mult, accum_out=sf[:, 4 + k:5 + k]), reads=[Brw, Bsf, Bgs], writes=[Brw, Bsf])
            P.op("dve", lambda h, sf=sf: h.tensor_scalar(out=sf[:, 8:12], in0=sf[:, 0:4], scalar1=float(NSLOT), scalar2=None, op0=ALU.is_lt),
                 reads=[Bsf], writes=[Bsf])
            P.op("dve", lambda h, sf=sf: h.tensor_tensor(out=sf[:, 4:8], in0=sf[:, 4:8], in1=sf[:, 8:12], op=ALU.mult),
                 reads=[Bsf], writes=[Bsf])
            P.dma("pool", route_s[tok, :], sf[:, 0:8], reads=[Bsf], writes=tb("route", t, t + 1))
            for k in range(4):
                su, Bsu = su_r.next()
                P.op("dve", lambda h, su=su, sf=sf, k=k: h.tensor_copy(out=su[:], in_=sf[:, k:k + 1]), reads=[Bsf], writes=[Bsu])
                P.idma(xd_s, bass.IndirectOffsetOnAxis(ap=su[:, 0:1], axis=0), x1b[:], None, reads=[Bx1b, Bsu], writes=[], bc=NSLOT - 1)
    P.barrier()
    if stop_after == "E":
        P.emit()
        return nc

    S4 = Arena(nc, A0, SBMAX)
    B_c4 = Buf()
    bgt = S4.t("bgt", [128, NEXP, 8], F32)
    but = S4.t("but", [128, NEXP, 8], F32)
    ones1 = S4.t("ones1", [1, 128], BF16)
    P.op("dve", lambda h: h.memset(ones1[:], 1.0), writes=[B_c4])
    P.dma("sp", bgt[:], bg_d, writes=[B_c4])
    P.dma("sp", but[:], bu_d, writes=[B_c4])
    P.op("dve", lambda h: h.tensor_scalar(out=but[:], in0=but[:], scalar1=1.0, scalar2=None, op0=ALU.add), reads=[B_c4], writes=[B_c4])
    wg_r = Rot(S4, "wg", [128, 8, DFF], BF16, 2)
    wu_r = Rot(S4, "wu", [128, 8, DFF], BF16, 2)
    wd_r = Rot(S4, "wd", [128, 8, D], BF16, 2)
    bde_r = Rot(S4, "bde", [1, D], BF16, 2)
    xrow_r = Rot(S4, "xrow", [128, D], BF16, 6)
    xT_r = Rot(S4, "xTe", [128, 8, 512], BF16, 2)
    hT_r = Rot(S4, "hT", [128, 8, 512], BF16, 2)
    hg_r = Rot(S4, "hg", [128, 512], F32, 2)
    sg_r = Rot(S4, "sg", [128, 512], F32, 2)
    v_r = Rot(S4, "v", [128, 512], F32, 2)
    hu_r = Rot(S4, "hu", [128, 512], F32, 2)
    g2_r = Rot(S4, "g2", [128, 512], F32, 2)
    ysb_r = Rot(S4, "ysb", [128, D], F32, 3)
    SUB = [(0, 512), (512, 512), (1024, CAP - 1024)]
    def wload(e):
        wg, Bwg = wg_r.next()
        wu, Bwu = wu_r.next()
        wd, Bwd = wd_r.next()
        bde, Bbde = bde_r.next()
        P.dma("pool", wg[:], wg_d[e].rearrange("(k p) f -> p k f", p=128), writes=[Bwg])
        P.dma("pool", wu[:], wu_d[e].rearrange("(k p) f -> p k f", p=128), writes=[Bwu])
        P.dma("pool", wd[:], wd_d[e].rearrange("(k p) f -> p k f", p=128), writes=[Bwd])
        P.dma("pool", bde[:], bd_d[e:e + 1, :], writes=[Bbde])
        return (wg, Bwg, wu, Bwu, wd, Bwd, bde, Bbde)

    wnext = wload(0)
    for e in range(NEXP):
        wg, Bwg, wu, Bwu, wd, Bwd, bde, Bbde = wnext
        if e + 1 < NEXP:
            wnext = wload(e + 1)
        for (s0, ns) in SUB:
            nt_ = ns // 128
            xT, BxT = xT_r.next()
            for tt in range(nt_):
                r0 = e * CAP + s0 + tt * 128
                xrow, Bxrow = xrow_r.next()
                P.dma("sp", xrow[:], xd_s[r0:r0 + 128, :], writes=[Bxrow])
                pt_, Bpt_ = PSB()
                for b in range(8):
                    P.op("pe", lambda h, pt_=pt_, b=b, xrow=xrow: h.transpose(
                        out=pt_[:, b * 128:(b + 1) * 128], in_=xrow[:, b * 128:(b + 1) * 128], identity=ident_b[:]),
                        reads=[Bxrow, B_const], writes=[Bpt_])
                evac(xT[:, :, tt * 128:(tt + 1) * 128], pt_[:, :].rearrange("p (b t) -> p b t", b=8), [Bpt_], [BxT])
            hT, BhT = hT_r.next()
            for fc in range(8):
                pg, Bpg = PSF()
                for k in range(8):
                    P.op("pe", lambda h, pg=pg, k=k, fc=fc, wg=wg, xT=xT, ns=ns: h.matmul(
                        pg[:, 0:ns], lhsT=wg[:, k, fc * 128:(fc + 1) * 128], rhs=xT[:, k, 0:ns], start=(k == 0), stop=(k == 7)),
                        reads=[Bwg, BxT], writes=[Bpg])
                pu, Bpu = PSF()
                for k in range(8):
                    P.op("pe", lambda h, pu=pu, k=k, fc=fc, wu=wu, xT=xT, ns=ns: h.matmul(
                        pu[:, 0:ns], lhsT=wu[:, k, fc * 128:(fc + 1) * 128], rhs=xT[:, k, 0:ns], start=(k == 0), stop=(k == 7)),
                        reads=[Bwu, BxT], writes=[Bpu])
                hg, Bhg = hg_r.next()
                sg, Bsg = sg_r.next()
                v, Bv = v_r.next()
                hu, Bhu = hu_r.next()
                g2, Bg2 = g2_r.next()
                P.op("dve", lambda h, hg=hg, pg=pg, e=e, fc=fc, ns=ns: h.tensor_scalar(
                    out=hg[:, 0:ns], in0=pg[:, 0:ns], scalar1=bgt[:, e, fc:fc + 1], scalar2=7.0, op0=ALU.add, op1=ALU.min),
                    reads=[Bpg, B_c4], writes=[Bhg])
                P.op("act", lambda h, sg=sg, hg=hg, ns=ns: h.activation(out=sg[:, 0:ns], in_=hg[:, 0:ns], func=AF.Sigmoid, scale=1.702),
                     reads=[Bhg], writes=[Bsg])
                P.op("act", lambda h, v=v, pu=pu, e=e, fc=fc, ns=ns: h.activation(
                    out=v[:, 0:ns], in_=pu[:, 0:ns], func=AF.Identity, bias=but[:, e, fc:fc + 1], scale=1.0),
                    reads=[Bpu, B_c4], writes=[Bv])
                P.op("dve", lambda h, hu=hu, v=v, ns=ns: h.tensor_scalar(out=hu[:, 0:ns], in0=v[:, 0:ns], scalar1=8.0, scalar2=-6.0,
                                                                         op0=ALU.min, op1=ALU.max), reads=[Bv], writes=[Bhu])
                P.op("dve", lambda h, g2=g2, hg=hg, sg=sg, ns=ns: h.tensor_tensor(out=g2[:, 0:ns], in0=hg[:, 0:ns], in1=sg[:, 0:ns], op=ALU.mult),
                     reads=[Bhg, Bsg], writes=[Bg2])
                P.op("dve", lambda h, hT=hT, fc=fc, hu=hu, g2=g2, ns=ns: h.tensor_tensor(out=hT[:, fc, 0:ns], in0=hu[:, 0:ns], in1=g2[:, 0:ns], op=ALU.mult),
                     reads=[Bhu, Bg2], writes=[BhT])
            for tt in range(nt_):
                r0 = e * CAP + s0 + tt * 128
                ysb, Bysb = ysb_r.next()
                for nh in range(2):
                    py, Bpy = PSF()
                    for fc in range(8):
                        P.op("pe", lambda h, py=py, fc=fc, tt=tt, nh=nh, hT=hT, wd=wd: h.matmul(
                            py[:, :], lhsT=hT[:, fc, tt * 128:(tt + 1) * 128], rhs=wd[:, fc, nh * 512:(nh + 1) * 512],
                            start=(fc == 0), stop=False), reads=[BhT, Bwd], writes=[Bpy])
                    P.op("pe", lambda h, py=py, nh=nh, bde=bde: h.matmul(
                        py[:, :], lhsT=ones1[:], rhs=bde[:, nh * 512:(nh + 1) * 512], start=False, stop=True),
                        reads=[Bbde, B_c4], writes=[Bpy])
                    evac(ysb[:, nh * 512:(nh + 1) * 512], py[:, :], [Bpy], [Bysb])
                for hf in range(2):
                    P.dma("pool", yd_h[hf][r0:r0 + 128, :], ysb[:, hf * 512:(hf + 1) * 512], reads=[Bysb], writes=[])
    P.barrier()
    if stop_after == "F":
        P.emit()
        return nc

    S5 = Arena(nc, A0, SBMAX)
    B_c5 = Buf()
    ln2g = S5.t("ln2g", [128, D], F32)
    ln2b = S5.t("ln2b", [128, D], F32)
    P.dma("sp", ln2g[:], ln2g_d, writes=[B_c5])
    P.dma("sp", ln2b[:], ln2b_d, writes=[B_c5])
    xo_r = Rot(S5, "xo", [128, D], F32, 5)
    yk_r = Rot(S5, "yk", [128, D], F32, 16)
    for i_ in range(len(yk_r.items)):
        yk_r.items[i_] = (yk_r.items[i_][0], [Buf(), Buf()])
    rtile_r = Rot(S5, "rtile", [128, 8], F32, 6)
    su5_r = Rot(S5, "su5", [128, 1], U32, 16)
    st5_r = Rot(S5, "st5", [128, 16], F32, 4)
    for (ykt, Byk) in yk_r.items:
        P.op("dve", lambda h, ykt=ykt: h.memset(ykt[:], 0.0), writes=Byk)
    for t in range(NT):
        tok = slice(t * 128, (t + 1) * 128)
        xo, Bxo = xo_r.next()
        rtile, Brt = rtile_r.next()
        P.dma("sp", xo[:], x1_s[tok, :], reads=tb("x1", t, t + 1), writes=[Bxo])
        P.dma("sp", rtile[:], route_s[tok, :], reads=tb("route", t, t + 1), writes=[Brt])
        yks = []
        for k in range(4):
            su, Bsu = su5_r.next()
            P.op("dve", lambda h, su=su, rtile=rtile, k=k: h.tensor_copy(out=su[:], in_=rtile[:, k:k + 1]), reads=[Brt], writes=[Bsu])
            ykt, Byk = yk_r.next()
            for hf in range(2):
                P.idma(ykt[:, hf * 512:(hf + 1) * 512], None, yd_h[hf], bass.IndirectOffsetOnAxis(ap=su[:, 0:1], axis=0),
                       reads=[Bsu], writes=[Byk[hf]], bc=NSLOT - 1)
            yks.append((ykt, Byk))
        P.op("dve", lambda h, xo=xo: h.tensor_scalar(out=xo[:], in0=xo[:], scalar1=ALPHA, scalar2=None, op0=ALU.mult),
             reads=[Bxo], writes=[Bxo])
        for k in range(4):
            ykt, Byk = yks[k]
            P.op("dve", lambda h, xo=xo, ykt=ykt, rtile=rtile, k=k: h.scalar_tensor_tensor(
                out=xo[:], in0=ykt[:], scalar=rtile[:, 4 + k:5 + k], in1=xo[:], op0=ALU.mult, op1=ALU.add),
                reads=[Bxo, Brt] + Byk, writes=[Bxo])
        layer_norm(xo, Bxo, ln2g, ln2b, B_c5, xo, Bxo, st5_r)
        P.dma("act", out_d[tok, :], xo[:], reads=[Bxo])
    P.barrier()
    P.emit()
    return nc


def _consts():
    p = np.arange(128)[:, None]
    j = np.arange(2048)[None, :]
    dist = np.abs(p - j + 1920)
    negD = ((-16.0 * (dist // 16)).astype(np.float32), (-(dist % 16)).astype(np.float32))
    jj = np.arange(640)[None, :]
    kp = jj - 512
    cq = p // 64
    valid = (kp >= cq * 64 - 512) & (kp < cq * 64 + 64)
    rel = np.clip(p - kp, -128, 128) + 128
    maskB = np.where(valid, 0.0, NEG).astype(np.float32)
    return negD, rel, maskB


def prep_inputs(inp, nseq=4, ncores=NCORES):
    f = lambda a: np.ascontiguousarray(np.asarray(a, dtype=np.float32))
    negD, rel, maskB = _consts()
    bc = lambda v, n=128: np.ascontiguousarray(np.broadcast_to(np.asarray(v, np.float32).reshape(1, -1), (n, v.size)))
    w_uk = f(inp["w_uk"][0])
    w_uk2 = np.ascontiguousarray(w_uk.reshape(4, 2, 64, 128).transpose(1, 2, 0, 3).reshape(128, 4, 128))
    relb = np.ascontiguousarray(f(inp["rel_bias"][0])[:, rel])
    bgp = np.ascontiguousarray(f(inp["b_gate"][0]).reshape(NEXP, 8, 128).transpose(2, 0, 1))
    bup = np.ascontiguousarray(f(inp["b_up"][0]).reshape(NEXP, 8, 128).transpose(2, 0, 1))
    shared = {
        "w_in": f(inp["w_in"][0]), "kvg_bc": bc(inp["kv_norm_g"][0]), "ikg_bc": bc(inp["idx_k_norm_g"][0]),
        "ikb_bc": bc(inp["idx_k_norm_b"][0]), "w_uk2": w_uk2, "w_uv": f(inp["w_uv"][0]), "relb": relb,
        "maskB": maskB, "negDh": negD[0].astype(ml_dtypes.bfloat16), "negDl": negD[1].astype(ml_dtypes.bfloat16),
        "slopeI": np.ascontiguousarray(np.stack([np.eye(128, dtype=np.float32) * 2.0 ** (-(h + 1)) for h in range(8)], axis=1)).astype(ml_dtypes.bfloat16), "w_branch_a": f(inp["w_branch_a"][0]), "w_branch_b": f(inp["w_branch_b"][0]),
        "w_out": f(inp["w_out"][0]), "ln1g_bc": bc(inp["ln1_g"][0]), "ln1b_bc": bc(inp["ln1_b"][0]),
        "w_router": f(inp["w_router"][0]), "br_bc": bc(inp["b_router"][0]), "w_gate": f(inp["w_gate"][0]),
        "w_up": f(inp["w_up"][0]), "w_down": f(inp["w_down"][0]), "b_gate_p": bgp, "b_up_p": bup,
        "b_down": f(inp["b_down"][0]), "ln2g_bc": bc(inp["ln2_g"][0]), "ln2b_bc": bc(inp["ln2_b"][0]),
        "ident_bf": np.eye(128, dtype=np.float32).astype(ml_dtypes.bfloat16), "ident_f": np.eye(128, dtype=np.float32),
        "ustrict": np.triu(np.ones((128, 128), np.float32), 1).astype(ml_dtypes.bfloat16),
        "pow2": np.ascontiguousarray(np.broadcast_to((2.0 ** -(np.arange(21, dtype=np.float64) + 1)).astype(np.float32)[None, :], (128, 21))),
        "ones_bf": np.ones((128, 128), np.float32).astype(ml_dtypes.bfloat16),
        "ebase": np.ascontiguousarray(np.broadcast_to((np.arange(NEXP, dtype=np.float32) * CAP)[None, :], (128, NEXP))),
    }
    x = np.asarray(inp["x"], dtype=np.float32)
    maps = []
    for c in range(ncores):
        xs = x[c * nseq:(c + 1) * nseq].reshape(nseq * SEQ, D)
        m = dict(shared)
        m["x"] = np.ascontiguousarray(xs)
        m["xT"] = np.ascontiguousarray(xs.T)
        maps.append(m)
    return maps


def kernel(**inputs):
    nseq = inputs["x"].shape[0] // NCORES
    nc = build(nseq)
    maps = prep_inputs(inputs, nseq)
    res = run_bass_kernel_spmd(nc, maps, core_ids=list(range(NCORES)))
    out = np.concatenate([np.asarray(r["out"]).reshape(nseq, SEQ, D) for r in res.results], axis=0)
    return out.astype(np.float32)
```

```python
import numpy as np
import ml_dtypes
import concourse.bass as bass
import concourse.mybir as mybir
from concourse.bass_utils import run_bass_kernel_spmd

F32 = mybir.dt.float32
BF16 = mybir.dt.bfloat16
ALU = mybir.AluOpType
AF = mybir.ActivationFunctionType
AX = mybir.AxisListType

D = 1024
SEQ = 2048
NCORES = 8
NEXP = 32
DFF = 1024
INW = 4808
ALPHA = 2.0 ** 0.25
EPS = 1e-5
IDX_SCALE = 512.0 ** -0.5
NEG = -30000.0
TOPK = 256
CAP = 1280
NSLOT = NEXP * CAP
BIGM = 262144.0
U32 = mybir.dt.uint32


class Buf:
    __slots__ = ("w", "r")

    def __init__(self):
        self.w = {}
        self.r = {}


class Op:
    __slots__ = ("eng", "fn", "deps", "dma", "sem", "val", "signals", "cnt")

    def __init__(self, eng, fn, dma):
        self.eng = eng
        self.fn = fn
        self.dma = dma
        self.deps = []
        self.sem = None
        self.val = 0
        self.signals = False
        self.cnt = 0


class Prog:
    ENG = ("pe", "act", "dve", "pool", "sp")

    def __init__(self, nc, ndma=48):
        self.nc = nc
        self.ops = {e: [] for e in self.ENG}
        self.ndma = ndma
        self.dma_uses = [0] * ndma
        self.dma_last = [None] * ndma
        self.dma_n = 0
        self.live_dma = []

    def _add(self, op, reads, writes):
        deps = {}
        for b in reads:
            for o in b.w.values():
                deps[o] = True
        for b in writes:
            for o in b.w.values():
                deps.setdefault(o, False)
            for o in b.r.values():
                deps.setdefault(o, False)
        for o, raw in deps.items():
            if o is not op:
                op.deps.append((o, raw))
        key = op if op.dma else op.eng
        for b in reads:
            b.r[key] = op
        for b in writes:
            b.w = {key: op}
            b.r = {}
        self.ops[op.eng].append(op)
        return op

    def op(self, eng, fn, reads=(), writes=()):
        return self._add(Op(eng, fn, False), reads, writes)

    def dma(self, q, out, in_, reads=(), writes=()):
        op = Op(q, lambda h: h.dma_start(out=out, in_=in_), True)
        slot = self.dma_n % self.ndma
        self.dma_n += 1
        prev = self.dma_last[slot]
        if prev is not None:
            op.deps.append((prev, True))
        self.dma_uses[slot] += 1
        self.dma_last[slot] = op
        op.sem = slot
        op.val = 16 * self.dma_uses[slot]
        self.live_dma.append(op)
        return self._add(op, reads, writes)

    def idma(self, out, out_off, in_, in_off, reads=(), writes=(), bc=None):
        def fn(h):
            if getattr(self, "_bcreg", None) is None:
                self._bcreg = h.alloc_register("bcreg")
                h.reg_mov(self._bcreg, bc)
                self._bcval = bc
            assert self._bcval == bc
            return h.indirect_dma_start(out=out, out_offset=out_off, in_=in_, in_offset=in_off,
                                        bounds_check=self._bcreg, oob_is_err=False)
        op = Op("pool", fn, True)
        slot = self.dma_n % self.ndma
        self.dma_n += 1
        prev = self.dma_last[slot]
        if prev is not None:
            op.deps.append((prev, True))
        self.dma_uses[slot] += 1
        self.dma_last[slot] = op
        op.sem = slot
        op.val = 16 * self.dma_uses[slot]
        self.live_dma.append(op)
        return self._add(op, reads, writes)

    def barrier(self):
        last = {e: (self.ops[e][-1] if self.ops[e] else None) for e in self.ENG}
        live = self.live_dma
        self.live_dma = []
        for e in self.ENG:
            b = Op(e, None, False)
            for e2 in self.ENG:
                if e2 != e and last[e2] is not None and not last[e2].dma and last[e2].fn is not None:
                    b.deps.append((last[e2], True))
            for d in live:
                b.deps.append((d, True))
            self.ops[e].append(b)

    @staticmethod
    def _skip(op, d, raw):
        if d.dma or op.dma:
            return False
        if d.eng != op.eng:
            return False
        if op.eng == "pe":
            return True
        return not raw

    def emit(self):
        nc = self.nc
        for e in self.ENG:
            for op in self.ops[e]:
                for d, raw in op.deps:
                    if not self._skip(op, d, raw) and not d.dma:
                        d.signals = True
        for e in self.ENG:
            c = 0
            for op in self.ops[e]:
                if op.signals and not op.dma:
                    c += 1
                    op.cnt = c
        import contextlib
        with contextlib.ExitStack() as st:
            esem = {e: st.enter_context(nc.semaphore("s_" + e)) for e in self.ENG}
            dsem = [st.enter_context(nc.semaphore("d%d" % i)) for i in range(self.ndma)]
            block = st.enter_context(nc.Block())

            def run(e, h):
                waited = {}
                for op in self.ops[e]:
                    for d, raw in op.deps:
                        if self._skip(op, d, raw):
                            continue
                        if d.dma:
                            key, sem, val = ("d", d.sem), dsem[d.sem], d.val
                        else:
                            key, sem, val = ("e", d.eng), esem[d.eng], d.cnt
                        if waited.get(key, 0) >= val:
                            continue
                        waited[key] = val
                        h.wait_ge(sem, val)
                    if op.fn is None:
                        continue
                    ins = op.fn(h)
                    if op.dma:
                        ins.then_inc(dsem[op.sem], 16)
                    elif op.signals:
                        ins.then_inc(esem[e], 1)

            @block.tensor
            def _(h):
                run("pe", h)

            @block.scalar
            def _(h):
                run("act", h)

            @block.vector
            def _(h):
                run("dve", h)

            @block.gpsimd
            def _(h):
                run("pool", h)

            @block.sync
            def _(h):
                run("sp", h)


class Arena:
    def __init__(self, nc, lo, hi):
        self.nc, self.lo, self.hi, self.n = nc, lo, hi, 0

    def t(self, name, shape, dt):
        esz = 4 if dt == F32 else 2
        per = int(np.prod(shape[1:])) * esz
        per = (per + 63) // 64 * 64
        assert self.lo + per <= self.hi, (name, self.lo, per, self.hi)
        h = self.nc.alloc_sbuf_tensor_at("%s_%d" % (name, self.lo), list(shape), dt, offset=self.lo)
        self.lo += per
        return h


class Rot:
    def __init__(self, arena, name, shape, dt, n):
        self.items = [(arena.t("%s%d" % (name, i), shape, dt), Buf()) for i in range(n)]
        self.i = 0

    def next(self):
        it = self.items[self.i % len(self.items)]
        self.i += 1
        return it


def build(nseq=4, debug=False, stop_after=None, mixers="AB"):
    nc = bass.Bass("TRN2", target_bir_lowering=False)
    P = Prog(nc)
    TOK = nseq * SEQ
    NT = TOK // 128
    dk = "ExternalOutput" if debug else "Internal"

    def din(name, shape, dt=F32):
        return nc.dram_tensor(name, list(shape), dt, kind="ExternalInput").ap()

    def dscr(name, shape, dt):
        return nc.dram_tensor(name, list(shape), dt, kind=dk).ap()

    xT_d = din("xT", [D, TOK])
    x_d = din("x", [TOK, D])
    win_d = din("w_in", [D, INW])
    kvg_d = din("kvg_bc", [128, 128])
    ikg_d = din("ikg_bc", [128, 64])
    ikb_d = din("ikb_bc", [128, 64])
    wuk_d = din("w_uk2", [128, 4, 128])
    wuv_d = din("w_uv", [8, 128, 64])
    relb_d = din("relb", [8, 128, 640])
    maskb_d = din("maskB", [128, 640])
    negdh_d = din("negDh", [128, 2048], BF16)
    negdl_d = din("negDl", [128, 2048], BF16)
    slopei_d = din("slopeI", [128, 8, 128], BF16)
    wa_d = din("w_branch_a", [512, D])
    wb_d = din("w_branch_b", [512, D])
    wo_d = din("w_out", [D, D])
    ln1g_d = din("ln1g_bc", [128, D])
    ln1b_d = din("ln1b_bc", [128, D])
    wr_d = din("w_router", [D, NEXP])
    br_d = din("br_bc", [128, NEXP])
    wg_d = din("w_gate", [NEXP, D, DFF])
    wu_d = din("w_up", [NEXP, D, DFF])
    wd_d = din("w_down", [NEXP, DFF, D])
    bg_d = din("b_gate_p", [128, NEXP, 8])
    bu_d = din("b_up_p", [128, NEXP, 8])
    bd_d = din("b_down", [NEXP, D])
    ln2g_d = din("ln2g_bc", [128, D])
    ln2b_d = din("ln2b_bc", [128, D])
    identb_d = din("ident_bf", [128, 128], BF16)
    identf_d = din("ident_f", [128, 128])
    ustr_d = din("ustrict", [128, 128], BF16)
    onesb_d = din("ones_bf", [128, 128], BF16)
    ebase_d = din("ebase", [128, NEXP])
    pow2_d = din("pow2", [128, 21])
    out_d = nc.dram_tensor("out", [TOK, D], F32, kind="ExternalOutput").ap()

    featT = dscr("featT", [32, 128, TOK], BF16)
    ckvtm_s = dscr("ckv_tm", [TOK, 128], BF16)
    ckvT_s = dscr("ckvT", [128, TOK], BF16)
    kidxT_s = dscr("kidxT", [64, TOK], BF16)
    widx_s = dscr("widx", [TOK, 8], F32)
    vb_s = dscr("vB", [TOK, 512], BF16)
    yaT_s = dscr("yaT", [4, 128, TOK], BF16)
    ybT_s = dscr("ybT", [4, 128, TOK], BF16)
    x1_s = dscr("x1", [TOK, D], F32)
    x1T_s = dscr("x1T", [8, 128, TOK], BF16)
    gates_s = dscr("gates", [TOK, NEXP], F32)
    route_s = dscr("route", [TOK, 8], F32)
    xd_s = dscr("xd", [NSLOT, D], BF16)
    yd_h = [dscr("yd0", [NSLOT, 512], F32), dscr("yd1", [NSLOT, 512], F32)]

    sb = {}

    def tb(name, t0, t1):
        return [sb.setdefault((name, t), Buf()) for t in range(t0, t1)]

    psf = [(nc.alloc_psum_tensor("psf%d" % i, [128, 512], F32), Buf()) for i in range(6)]
    psb = [(nc.alloc_psum_tensor("psb%d" % i, [128, 1024], BF16), Buf()) for i in range(2)]
    psi = [0, 0]
    psn = [6]

    def PSF():
        psi[0] += 1
        return psf[psi[0] % psn[0]]

    accA, accB, accC = psf[4], psf[5], psf[3]

    def PSB():
        psi[1] += 1
        return psb[psi[1] % 2]

    SBMAX = 224 * 1024
    C = Arena(nc, 16640, 60 * 1024)
    ident_b = C.t("identb", [128, 128], BF16)
    ident_f = C.t("identf", [128, 128], F32)
    B_const = Buf()
    P.dma("sp", ident_b[:], identb_d, writes=[B_const])
    P.dma("sp", ident_f[:], identf_d, writes=[B_const])
    eps_t = C.t("eps", [128, 1], F32)
    P.op("dve", lambda h: h.memset(eps_t[:], EPS), writes=[B_const])
    A0 = C.lo

    A = Arena(nc, A0, SBMAX)
    win = A.t("win", [128, 8, INW], BF16)
    B_w = Buf()
    for kc in range(8):
        P.dma("pool", win[:, kc, :], win_d[kc * 128:(kc + 1) * 128, :], writes=[B_w])
    kvg = A.t("kvg", [128, 128], F32)
    ikg = A.t("ikg", [128, 64], F32)
    ikb = A.t("ikb", [128, 64], F32)
    P.dma("sp", kvg[:], kvg_d, writes=[B_const])
    P.dma("sp", ikg[:], ikg_d, writes=[B_const])
    P.dma("sp", ikb[:], ikb_d, writes=[B_const])

    xTg_r = Rot(A, "xTg", [128, 8, 512], BF16, 2)
    stg_r = Rot(A, "stg", [128, 512], BF16, 4)
    ckvb_r = Rot(A, "ckvb", [128, 128], BF16, 5)
    ckvTs_r = Rot(A, "ckvTs", [128, 128], BF16, 2)
    knf_r = Rot(A, "knf", [128, 64], F32, 2)
    knb_r = Rot(A, "knb", [128, 64], BF16, 5)
    kTs_r = Rot(A, "kTs", [64, 128], BF16, 2)
    ws_r = Rot(A, "ws", [128, 8], F32, 2)
    vbs_r = Rot(A, "vbs", [128, 512], BF16, 2)
    junk_r = Rot(A, "junk", [128, 128], F32, 2)
    st_r = Rot(A, "st", [128, 16], F32, 4)

    xT_v = xT_d.rearrange("(kc p) t -> p kc t", p=128)
    FM = [0, 128, 256, 384, 640, 768, 896, 1024, 1224, 1352, 1480, 1608, 1736, 1864, 1992, 2120] + \
         [2760 + 128 * i for i in range(16)]

    def phaseA(seq):
        for g in range(seq * 4, seq * 4 + 4):
            t0 = g * 4
            xTg, Bx = xTg_r.next()
            P.dma("pool", xTg[:], xT_v[:, :, g * 512:(g + 1) * 512], writes=[Bx])
            deferred = []
            for tt in range(4):
                t = t0 + tt
                tok = slice(t * 128, (t + 1) * 128)
                psA, BpA = PSF()
                for kc in range(8):
                    P.op("pe", lambda h, ps=psA, kc=kc, tt=tt, xTg=xTg: h.matmul(
                        ps[:, 0:128], lhsT=xTg[:, kc, tt * 128:(tt + 1) * 128], rhs=win[:, kc, 512:640],
                        start=(kc == 0), stop=(kc == 7)), reads=[B_w, Bx], writes=[BpA])
                for kc in range(8):
                    P.op("pe", lambda h, ps=psA, kc=kc, tt=tt, xTg=xTg: h.matmul(
                        ps[:, 128:200], lhsT=xTg[:, kc, tt * 128:(tt + 1) * 128], rhs=win[:, kc, 1152:1224],
                        start=(kc == 0), stop=(kc == 7)), reads=[B_w, Bx], writes=[BpA])
                psV, BpV = PSF()
                for kc in range(8):
                    P.op("pe", lambda h, ps=psV, kc=kc, tt=tt, xTg=xTg: h.matmul(
                        ps[:, :], lhsT=xTg[:, kc, tt * 128:(tt + 1) * 128], rhs=win[:, kc, 2248:2760],
                        start=(kc == 0), stop=(kc == 7)), reads=[B_w, Bx], writes=[BpV])
                vbs, Bv = vbs_r.next()
                P.op("act", lambda h, vbs=vbs, ps=psV: h.copy(out=vbs[:], in_=ps[:, :]), reads=[BpV], writes=[Bv])
                P.dma("sp", vb_s[tok, :], vbs[:], reads=[Bv], writes=tb("vb", t, t + 1))
                st, Bst = st_r.next()
                junk, Bj = junk_r.next()
                P.op("act", lambda h, junk=junk, ps=psA, st=st: h.activation(
                    out=junk[:, 0:128], in_=ps[:, 0:128], func=AF.Square, accum_out=st[:, 0:1]),
                    reads=[BpA], writes=[Bj, Bst])
                P.op("act", lambda h, st=st: h.activation(out=st[:, 1:2], in_=st[:, 0:1], func=AF.Sqrt,
                                                          scale=1.0 / 128.0, bias=eps_t[:, 0:1]),
                     reads=[Bst, B_const], writes=[Bst])
                P.op("dve", lambda h, st=st: h.reciprocal(out=st[:, 2:3], in_=st[:, 1:2]), reads=[Bst], writes=[Bst])
                ckvb, Bc = ckvb_r.next()
                deferred.append((t, tok, ckvb, Bc, None, None))
                P.op("dve", lambda h, ckvb=ckvb, ps=psA, st=st: h.scalar_tensor_tensor(
                    out=ckvb[:], in0=ps[:, 0:128], scalar=st[:, 2:3], in1=kvg[:], op0=ALU.mult, op1=ALU.mult),
                    reads=[BpA, Bst, B_const], writes=[Bc])
                P.dma("sp", ckvtm_s[tok, :], ckvb[:], reads=[Bc], writes=tb("ckvtm", t, t + 1))
                P.op("dve", lambda h, st=st, ps=psA: h.bn_stats(out=st[:, 4:10], in_=ps[:, 128:192]),
                     reads=[BpA], writes=[Bst])
                P.op("dve", lambda h, st=st: h.bn_aggr(out=st[:, 10:12], in_=st[:, 4:10]), reads=[Bst], writes=[Bst])
                P.op("act", lambda h, st=st: h.activation(out=st[:, 12:13], in_=st[:, 11:12], func=AF.Sqrt,
                                                          scale=1.0, bias=eps_t[:, 0:1]),
                     reads=[Bst, B_const], writes=[Bst])
                P.op("dve", lambda h, st=st: h.reciprocal(out=st[:, 13:14], in_=st[:, 12:13]), reads=[Bst], writes=[Bst])
                knf, Bkf = knf_r.next()
                P.op("dve", lambda h, knf=knf, ps=psA, st=st: h.tensor_scalar(
                    out=knf[:], in0=ps[:, 128:192], scalar1=st[:, 10:11], scalar2=st[:, 13:14],
                    op0=ALU.subtract, op1=ALU.mult), reads=[BpA, Bst], writes=[Bkf])
                P.op("dve", lambda h, knf=knf: h.tensor_tensor(out=knf[:], in0=knf[:], in1=ikg[:], op=ALU.mult),
                     reads=[Bkf, B_const], writes=[Bkf])
                knb, Bkb = knb_r.next()
                deferred.append((t, tok, None, None, knb, Bkb))
                P.op("dve", lambda h, knf=knf, knb=knb: h.tensor_tensor(out=knb[:], in0=knf[:], in1=ikb[:], op=ALU.add),
                     reads=[Bkf, B_const], writes=[Bkb])
                ws, Bws = ws_r.next()
                P.op("dve", lambda h, ws=ws, ps=psA: h.tensor_scalar(
                    out=ws[:], in0=ps[:, 192:200], scalar1=IDX_SCALE, scalar2=None, op0=ALU.mult),
                    reads=[BpA], writes=[Bws])
                P.dma("sp", widx_s[tok, :], ws[:], reads=[Bws], writes=tb("widx", t, t + 1))
            for ci, c0 in enumerate(FM):
                ps, Bp = PSF()
                for kc in range(8):
                    P.op("pe", lambda h, ps=ps, kc=kc, c0=c0, xTg=xTg: h.matmul(
                        ps[:, :], lhsT=win[:, kc, c0:c0 + 128], rhs=xTg[:, kc, :], start=(kc == 0), stop=(kc == 7)),
                        reads=[B_w, Bx], writes=[Bp])
                stg, Bs = stg_r.next()
                if ci >= 16:
                    P.op("act", lambda h, stg=stg, ps=ps: h.activation(out=stg[:], in_=ps[:, :], func=AF.Sigmoid),
                         reads=[Bp], writes=[Bs])
                elif ci % 2 == 0:
                    P.op("act", lambda h, stg=stg, ps=ps: h.copy(out=stg[:], in_=ps[:, :]), reads=[Bp], writes=[Bs])
                else:
                    P.op("dve", lambda h, stg=stg, ps=ps: h.tensor_copy(out=stg[:], in_=ps[:, :]), reads=[Bp], writes=[Bs])
                P.dma("sp", featT[ci, :, g * 512:(g + 1) * 512], stg[:], reads=[Bs], writes=tb(("f", ci), t0, t0 + 4))
            for (t, tok, ckvb, Bc, knb, Bkb) in deferred:
                if ckvb is not None:
                    pT, BpT = PSB()
                    P.op("pe", lambda h, pT=pT, ckvb=ckvb: h.transpose(out=pT[:, 0:128], in_=ckvb[:], identity=ident_b[:]),
                         reads=[Bc, B_const], writes=[BpT])
                    cts, Bct = ckvTs_r.next()
                    P.op("act", lambda h, cts=cts, pT=pT: h.copy(out=cts[:], in_=pT[:, 0:128]), reads=[BpT], writes=[Bct])
                    P.dma("sp", ckvT_s[:, tok], cts[:], reads=[Bct], writes=tb("ckvT", t, t + 1))

                else:
                    pT2, BpT2 = PSB()
                    P.op("pe", lambda h, pT2=pT2, knb=knb: h.transpose(out=pT2[0:64, 0:128], in_=knb[:], identity=ident_b[:]),
                         reads=[Bkb, B_const], writes=[BpT2])
                    kts, Bkt = kTs_r.next()
                    P.op("act", lambda h, kts=kts, pT2=pT2: h.copy(out=kts[:], in_=pT2[0:64, 0:128]), reads=[BpT2], writes=[Bkt])
                    P.dma("sp", kidxT_s[:, tok], kts[:], reads=[Bkt], writes=tb("kidxT", t, t + 1))


    for seq in range(nseq):
        phaseA(seq)
    P.barrier()
    if stop_after == "A":
        P.emit()
        return nc

    S2 = Arena(nc, A0, SBMAX)
    psn[0] = 3
    B_c2 = Buf()
    wuk = S2.t("wuk", [128, 4, 128], BF16)
    wuv = S2.t("wuv", [128, 8, 64], BF16)
    negDh = S2.t("negDh", [128, 2048], BF16)
    negDl = S2.t("negDl", [128, 2048], BF16)
    slopeI = S2.t("slopeI", [128, 8, 128], BF16)
    biasB = S2.t("biasB", [128, 8, 640], F32)
    maskB = S2.t("maskB", [128, 640], F32)
    P.dma("pool", wuk[:], wuk_d, writes=[B_c2])
    P.dma("pool", wuv[:], wuv_d.rearrange("h r d -> r h d"), writes=[B_c2])
    P.dma("sp", negDh[:], negdh_d, writes=[B_c2])
    P.dma("sp", negDl[:], negdl_d, writes=[B_c2])
    P.dma("sp", slopeI[:], slopei_d, writes=[B_c2])
    P.dma("sp", maskB[:], maskb_d, writes=[B_c2])
    for h in range(8):
        P.dma("sp", biasB[:, h, :], relb_d[h], writes=[B_c2])
    for h in range(8):
        P.op("dve", lambda hh, h=h: hh.tensor_tensor(out=biasB[:, h, :], in0=biasB[:, h, :], in1=maskB[:], op=ALU.add),
             reads=[B_c2], writes=[B_c2])

    kidx2_r = Rot(S2, "kidx2", [128, 2048], BF16, 1)
    ckvT_r = Rot(S2, "ckvTr", [128, 2048], BF16, 1)
    ckvtm_r = Rot(S2, "ckvtmr", [128, 16, 128], BF16, 1)
    isc2 = [S2.t("isc%d" % i, [128, 2048], F32) for i in range(2)]
    B_isc2 = [[Buf() for _ in range(4)] for _ in range(2)]
    negm2 = [S2.t("negm%d" % i, [128, 2048], BF16) for i in range(2)]
    B_negm2 = [Buf(), Buf()]
    bs_r = Rot(S2, "bs", [128, 8], F32, 2)
    dl_r = Rot(S2, "dl", [128, 24], F32, 2)
    cn_r = Rot(S2, "cn", [128, 24], F32, 2)
    jk_r = Rot(S2, "jk", [128, 2048], BF16, 2)
    pow2 = S2.t("pow2", [128, 24], F32)
    P.dma("sp", pow2[:, 0:21], pow2_d, writes=[B_c2])
    qi_r = Rot(S2, "qi", [128, 4, 128], BF16, 2)
    qa_r = Rot(S2, "qa", [128, 4, 128], BF16, 2)
    wt_r = Rot(S2, "wt", [128, 8], F32, 2)
    rl_r = Rot(S2, "rl", [128, 512], F32, 2)
    m8_r = Rot(S2, "m8", [128, 8], F32, 2)
    qlat_r = Rot(S2, "qlat", [128, 128], BF16, 2)
    sm_r = Rot(S2, "sm", [128, 2048], F32, 2)
    pb_r = Rot(S2, "pb", [128, 2048], BF16, 2)
    pt_r = Rot(S2, "pt", [128, 2048], BF16, 2)
    st2_r = Rot(S2, "st2", [128, 4], F32, 8)
    mx_r = Rot(S2, "mx", [128, 4], F32, 4)
    rc_r = Rot(S2, "rc", [128, 8], F32, 2)
    olat_r = Rot(S2, "olat", [128, 1024], BF16, 1)
    olatT_r = Rot(S2, "olatT", [128, 1024], BF16, 1)
    yas_r = Rot(S2, "yas", [64, 1024], BF16, 2)
    qb_r = Rot(S2, "qb", [128, 4, 128], BF16, 2)
    kb_r = Rot(S2, "kb", [128, 4, 640], BF16, 2)
    vb_r = Rot(S2, "vb", [128, 5, 512], BF16, 2)
    sB_r = Rot(S2, "sB", [128, 640], F32, 2)
    pB_r = Rot(S2, "pB", [128, 640], BF16, 2)
    ptB_r = Rot(S2, "ptB", [128, 640], BF16, 2)
    yb_r = Rot(S2, "yb", [128, 512], BF16, 1)
    ybs_r = Rot(S2, "ybs", [128, 512], BF16, 2)
    SLOPES = [2.0 ** (-(h + 1)) for h in range(8)]
    yaT_v = yaT_s.rearrange("j (hp d) t -> d (j hp) t", hp=2)
    cpy = [0]

    def evac(out, in_, reads, writes):
        cpy[0] += 1
        if cpy[0] % 2:
            P.op("act", lambda h: h.copy(out=out, in_=in_), reads=reads, writes=writes)
        else:
            P.op("dve", lambda h: h.tensor_copy(out=out, in_=in_), reads=reads, writes=writes)

    NBIS = 20

    def indexer(seq, t, kidx2, Bk):
        par = t % 2
        isc, B_isc = isc2[par], B_isc2[par]
        tg = seq * 16 + t
        tok = slice(tg * 128, (tg + 1) * 128)
        S = (t + 1) * 128
        chunks = [(c * 512, min(512, S - c * 512)) for c in range((S + 511) // 512)]
        qi, Bqi = qi_r.next()
        wt, Bwt = wt_r.next()
        P.dma("sp", qi[:], featT[4:8, :, tok].rearrange("c p t -> p c t"), reads=[b for c in range(4, 8) for b in tb(("f", c), tg, tg + 1)], writes=[Bqi])
        P.dma("sp", wt[:], widx_s[tok, :], reads=tb("widx", tg, tg + 1), writes=[Bwt])
        for h in range(8):
            hp, j = h % 2, h // 2
            for ci, (c0, cs) in enumerate(chunks):
                ps, Bp = PSF()
                P.op("pe", lambda hh, ps=ps, cs=cs, c0=c0, hp=hp, j=j, qi=qi: hh.matmul(
                    ps[:, 0:cs], lhsT=qi[hp * 64:(hp + 1) * 64, j, :], rhs=kidx2[hp * 64:(hp + 1) * 64, c0:c0 + cs],
                    start=True, stop=True), reads=[Bqi, Bk], writes=[Bp])
                rl, Brl = rl_r.next()
                P.op("act", lambda hh, rl=rl, ps=ps, cs=cs: hh.activation(out=rl[:, 0:cs], in_=ps[:, 0:cs], func=AF.Relu),
                     reads=[Bp], writes=[Brl])
                if h == 0:
                    P.op("dve", lambda hh, rl=rl, cs=cs, c0=c0, wt=wt: hh.tensor_scalar(
                        out=isc[:, c0:c0 + cs], in0=rl[:, 0:cs], scalar1=wt[:, 0:1], scalar2=None, op0=ALU.mult),
                        reads=[Brl, Bwt], writes=[B_isc[ci]])
                else:
                    P.op("dve", lambda hh, rl=rl, cs=cs, c0=c0, wt=wt, h=h: hh.scalar_tensor_tensor(
                        out=isc[:, c0:c0 + cs], in0=rl[:, 0:cs], scalar=wt[:, h:h + 1], in1=isc[:, c0:c0 + cs],
                        op0=ALU.mult, op1=ALU.add), reads=[Brl, Bwt, B_isc[ci]], writes=[B_isc[ci]])
                yield
        P.op("dve", lambda hh: hh.memset(isc[0:64, t * 128 + 64:(t + 1) * 128], -1e30),
             reads=B_isc[:len(chunks)], writes=B_isc[:len(chunks)])

    def thresh_gen(t):
        par = t % 2
        isc, B_isc, negm, B_negm = isc2[par], B_isc2[par], negm2[par], B_negm2[par]
        S = (t + 1) * 128
        nch = (S + 511) // 512
        Bi = B_isc[:nch]
        if t < 2:
            P.op("dve", lambda hh: hh.tensor_scalar(
                out=negm[:, 0:S], in0=isc[:, 0:S], scalar1=-1e29, scalar2=NEG, op0=ALU.is_lt, op1=ALU.mult),
                reads=Bi, writes=[B_negm])
            return
        bs, Bbs = bs_r.next()
        dl, Bdl = dl_r.next()
        cn, Bcn = cn_r.next()
        P.op("dve", lambda hh: hh.tensor_reduce(out=bs[:, 0:1], in_=isc[:, 0:S], axis=AX.X, op=ALU.max), reads=Bi, writes=[Bbs])
        yield
        P.op("dve", lambda hh: hh.tensor_reduce(out=bs[:, 1:2], in_=isc[:, 0:S - 128], axis=AX.X, op=ALU.min), reads=Bi, writes=[Bbs])
        yield
        P.op("dve", lambda hh: hh.tensor_tensor(out=bs[:, 2:3], in0=bs[:, 0:1], in1=bs[:, 1:2], op=ALU.subtract), reads=[Bbs], writes=[Bbs])
        P.op("dve", lambda hh: hh.tensor_tensor(out=bs[:, 3:4], in0=bs[:, 0:1], in1=bs[:, 1:2], op=ALU.add), reads=[Bbs], writes=[Bbs])
        P.op("dve", lambda hh: hh.tensor_scalar(out=bs[:, 4:5], in0=bs[:, 3:4], scalar1=-0.5, scalar2=None, op0=ALU.mult), reads=[Bbs], writes=[Bbs])
        P.op("dve", lambda hh: hh.tensor_scalar(out=dl[:, :], in0=pow2[:, :], scalar1=bs[:, 2:3], scalar2=None, op0=ALU.mult),
             reads=[Bbs, B_c2], writes=[Bdl])
        yield
        for i in range(NBIS):
            a, b = 4 + (i % 2), 4 + ((i + 1) % 2)
            jk, Bjk = jk_r.next()
            P.op("act", lambda hh, jk=jk, a=a, i=i: hh.activation(
                out=jk[:, 0:S], in_=isc[:, 0:S], func=AF.Sign, bias=bs[:, a:a + 1], scale=1.0, accum_out=cn[:, i:i + 1]),
                reads=Bi + [Bbs], writes=[Bjk, Bcn])
            yield
            P.op("dve", lambda hh, i=i: hh.tensor_scalar(out=bs[:, 6:7], in0=cn[:, i:i + 1], scalar1=511.5 - S, scalar2=-0.5,
                                                         op0=ALU.is_le, op1=ALU.add), reads=[Bcn], writes=[Bbs])
            P.op("dve", lambda hh, i=i, a=a, b=b: hh.scalar_tensor_tensor(
                out=bs[:, b:b + 1], in0=bs[:, 6:7], scalar=dl[:, i:i + 1], in1=bs[:, a:a + 1], op0=ALU.mult, op1=ALU.add),
                reads=[Bbs, Bdl], writes=[Bbs])
            yield
        f = 4 + (NBIS % 2)
        P.op("dve", lambda hh: hh.scalar_tensor_tensor(
            out=bs[:, 7:8], in0=bs[:, f:f + 1], scalar=-1.0, in1=dl[:, NBIS:NBIS + 1], op0=ALU.mult, op1=ALU.subtract),
            reads=[Bbs, Bdl], writes=[Bbs])
        P.op("dve", lambda hh: hh.tensor_scalar(
            out=negm[:, 0:S], in0=isc[:, 0:S], scalar1=bs[:, 7:8], scalar2=NEG, op0=ALU.is_lt, op1=ALU.mult),
            reads=Bi + [Bbs], writes=[B_negm])

    def run_pipelined(head_gen):
        gens = [head_gen(h) for h in range(8)]
        next(gens[0])
        for h in range(8):
            if h + 1 < 8:
                next(gens[h + 1])
            for _ in gens[h]:
                pass

    class BG:
        def __init__(self, makers):
            self.makers = makers
            self.cur = 0
            self.gen = None
            self.limit = -1

        def _one(self):
            if self.cur >= len(self.makers):
                return False
            if self.gen is None:
                self.gen = self.makers[self.cur]()
            try:
                next(self.gen)
            except StopIteration:
                self.gen = None
                self.cur += 1
            return True

        def step(self):
            if self.cur <= self.limit:
                self._one()

        def force(self, item):
            while self.cur <= item and self.cur < len(self.makers):
                self._one()

    bgref = [None]

    bgB = [None]

    def sp(n=1):
        if bgref[0] is not None:
            for _ in range(n):
                bgref[0].step()
        if bgB[0] is not None:
            try:
                next(bgB[0])
            except StopIteration:
                bgB[0] = None

    def drive_stage(g):
        while True:
            try:
                v = next(g)
            except StopIteration:
                return
            if v == 'S':
                return
            yield

    def run_pipelined_gen(head_gen):
        gens = [head_gen(h) for h in range(8)]
        yield from drive_stage(gens[0])
        for h in range(8):
            if h + 1 < 8:
                yield from drive_stage(gens[h + 1])
            yield from drive_stage(gens[h])

    def advance(gen, n):
        if gen is None:
            return
        for _ in range(n):
            try:
                next(gen)
            except StopIteration:
                return

    def attention(seq, t, ckvT, BcT, ckvtm, Bcm, gnext):
        par = t % 2
        negm, B_negm = negm2[par], B_negm2[par]
        tg = seq * 16 + t
        tok = slice(tg * 128, (tg + 1) * 128)
        S = (t + 1) * 128
        nb = t + 1
        chunks = [(c * 512, min(512, S - c * 512)) for c in range((S + 511) // 512)]
        qa, Bqa = qa_r.next()
        P.dma("sp", qa[:], featT[0:4, :, tok].rearrange("c p t -> p c t"), reads=[b for c in range(0, 4) for b in tb(("f", c), tg, tg + 1)], writes=[Bqa])
        olat, Bol = olat_r.next()
        rc, Brc = rc_r.next()
        off = 1920 - t * 128
        Bacc = [accA[1], accB[1]]
        def head_gen(h):
                sp()
                hp, j = h % 2, h // 2
                psq, Bpq = PSF()
                P.op("pe", lambda hh, psq=psq, hp=hp, j=j, qa=qa: hh.matmul(
                    psq[:, 0:128], lhsT=wuk[hp * 64:(hp + 1) * 64, j, :], rhs=qa[hp * 64:(hp + 1) * 64, j, :],
                    start=True, stop=True), reads=[Bqa, B_c2], writes=[Bpq])
                ql, Bql = qlat_r.next()
                P.op("act", lambda hh, ql=ql, psq=psq: hh.mul(out=ql[:], in_=psq[:, 0:128], mul=0.125),
                     reads=[Bpq], writes=[Bql])
                sp()
                sm, Bsm = sm_r.next()
                mx, Bmx = mx_r.next()
                for ci, (c0, cs) in enumerate(chunks):
                    ps, Bp = PSF()
                    P.op("pe", lambda hh, ps=ps, cs=cs, c0=c0, ql=ql: hh.matmul(
                        ps[:, 0:cs], lhsT=ql[:], rhs=ckvT[:, c0:c0 + cs], start=True, stop=False),
                        reads=[Bql, BcT], writes=[Bp])
                    P.op("pe", lambda hh, ps=ps, cs=cs, c0=c0: hh.matmul(
                        ps[:, 0:cs], lhsT=ident_b[:], rhs=negm[:, c0:c0 + cs], start=False, stop=False),
                        reads=[B_negm, B_const], writes=[Bp])
                    P.op("pe", lambda hh, ps=ps, cs=cs, c0=c0: hh.matmul(
                        ps[:, 0:cs], lhsT=slopeI[:, h, :], rhs=negDh[:, off + c0:off + c0 + cs], start=False, stop=False),
                        reads=[B_c2], writes=[Bp])
                    P.op("pe", lambda hh, ps=ps, cs=cs, c0=c0: hh.matmul(
                        ps[:, 0:cs], lhsT=slopeI[:, h, :], rhs=negDl[:, off + c0:off + c0 + cs], start=False, stop=True),
                        reads=[B_c2], writes=[Bp])
                    P.op("dve", lambda hh, ps=ps, cs=cs, c0=c0, sm=sm, mx=mx, ci=ci: hh.tensor_scalar(
                        out=sm[:, c0:c0 + cs], in0=ps[:, 0:cs], scalar1=1.0, scalar2=-3.0e38, op0=ALU.mult, op1=ALU.max,
                        accum_out=mx[:, ci:ci + 1]), reads=[Bp], writes=[Bsm, Bmx])
                    sp()
                st, Bst = st2_r.next()
                P.op("dve", lambda hh, mx=mx, st=st: hh.tensor_reduce(out=st[:, 0:1], in_=mx[:, 0:len(chunks)], axis=AX.X, op=ALU.max, negate=True),
                     reads=[Bmx], writes=[Bst])
                sp()
                pb, Bpb = pb_r.next()
                P.op("act", lambda hh, sm=sm, st=st, pb=pb: hh.activation(
                    out=pb[:, 0:S], in_=sm[:, 0:S], func=AF.Exp, bias=st[:, 0:1], scale=1.0, accum_out=st[:, 1:2]),
                    reads=[Bsm, Bst], writes=[Bpb, Bst])
                P.op("dve", lambda hh, st=st, rc=rc, h=h: hh.reciprocal(out=rc[:, h:h + 1], in_=st[:, 1:2]), reads=[Bst], writes=[Brc])
                sp()
                yield
                sp()
                pt, Bpt = pt_r.next()
                for b0 in range(0, nb, 8):
                    nbb = min(8, nb - b0)
                    pT, BpT = PSB()
                    for b in range(nbb):
                        P.op("pe", lambda hh, pT=pT, b=b, b0=b0, pb=pb: hh.transpose(
                            out=pT[:, b * 128:(b + 1) * 128], in_=pb[:, (b0 + b) * 128:(b0 + b + 1) * 128], identity=ident_b[:]),
                            reads=[Bpb, B_const], writes=[BpT])
                    evac(pt[:, b0 * 128:(b0 + nbb) * 128], pT[:, 0:nbb * 128], [BpT], [Bpt])
                    sp()
                acc = (accA if h < 4 else accB)[0]
                for b in range(nb):
                    P.op("pe", lambda hh, acc=acc, b=b, h=h, pt=pt: hh.matmul(
                        acc[:, (h % 4) * 128:(h % 4 + 1) * 128], lhsT=pt[:, b * 128:(b + 1) * 128], rhs=ckvtm[:, b, :],
                        start=(b == 0), stop=(b == nb - 1)), reads=[Bpt, Bcm], writes=[Bacc[h // 4]])
        run_pipelined(head_gen)
        for h in range(8):
            acc = (accA if h < 4 else accB)[0]
            P.op("dve", lambda hh, acc=acc, h=h, rc=rc, olat=olat: hh.tensor_scalar(
                out=olat[:, h * 128:(h + 1) * 128], in0=acc[:, (h % 4) * 128:(h % 4 + 1) * 128], scalar1=rc[:, h:h + 1],
                scalar2=None, op0=ALU.mult), reads=[Bacc[h // 4], Brc], writes=[Bol])
        olT, BolT = olatT_r.next()
        pT, BpT = PSB()
        for h in range(8):
            P.op("pe", lambda hh, pT=pT, h=h, olat=olat: hh.transpose(
                out=pT[:, h * 128:(h + 1) * 128], in_=olat[:, h * 128:(h + 1) * 128], identity=ident_b[:]),
                reads=[Bol, B_const], writes=[BpT])
        evac(olT[:, :], pT[:, :], [BpT], [BolT])
        yas, Bya = yas_r.next()
        for half in range(2):
            ps, Bp = PSF()
            for hh_ in range(4):
                h = half * 4 + hh_
                P.op("pe", lambda hh, ps=ps, h=h, hh_=hh_, olT=olT: hh.matmul(
                    ps[0:64, hh_ * 128:(hh_ + 1) * 128], lhsT=wuv[:, h, :], rhs=olT[:, h * 128:(h + 1) * 128],
                    start=True, stop=True), reads=[BolT, B_c2], writes=[Bp])
            evac(yas[:, half * 512:(half + 1) * 512], ps[0:64, :], [Bp], [Bya])
        P.dma("pool", yaT_v[:, :, tok], yas[:].rearrange("p (c t) -> p c t", c=8), reads=[Bya], writes=tb("yaT", tg, tg + 1))

    def mixerB(seq, t):
        tg = seq * 16 + t
        tok = slice(tg * 128, (tg + 1) * 128)
        nk = min(640, (t + 1) * 128)
        c0 = 640 - nk
        ks = seq * SEQ + (t + 1) * 128 - nk
        nbk = nk // 128
        kt0 = ks // 128
        qb, Bqb = qb_r.next()
        kb, Bkb = kb_r.next()
        vb, Bvb = vb_r.next()
        P.dma("sp", qb[:], featT[8:12, :, tok].rearrange("c p t -> p c t"),
              reads=[b for c in range(8, 12) for b in tb(("f", c), tg, tg + 1)], writes=[Bqb])
        P.dma("sp", kb[:, :, 0:nk], featT[12:16, :, ks:ks + nk].rearrange("c p t -> p c t"),
              reads=[b for c in range(12, 16) for b in tb(("f", c), kt0, kt0 + nbk)], writes=[Bkb])
        P.dma("sp", vb[:, 0:nbk, :], vb_s[ks:ks + nk, :].rearrange("(b p) f -> p b f", p=128),
              reads=tb("vb", kt0, kt0 + nbk), writes=[Bvb])
        rc, Brc = rc_r.next()
        n1 = min(nk, 512)
        Bacc = accC[1]
        yield
        def head_gen(h):
                hp, j = h % 2, h // 2
                sB, BsB = sB_r.next()
                parts = [(0, n1)] + ([(512, 128)] if nk > 512 else [])
                for (k0, kn) in parts:
                    ps, Bp = PSF()
                    P.op("pe", lambda hh, ps=ps, k0=k0, kn=kn, hp=hp, j=j, qb=qb, kb=kb: hh.matmul(
                        ps[:, 0:kn], lhsT=qb[hp * 64:(hp + 1) * 64, j, :], rhs=kb[hp * 64:(hp + 1) * 64, j, k0:k0 + kn],
                        start=True, stop=True), reads=[Bqb, Bkb], writes=[Bp])
                    P.op("dve", lambda hh, ps=ps, k0=k0, kn=kn, sB=sB, h=h: hh.scalar_tensor_tensor(
                        out=sB[:, k0:k0 + kn], in0=ps[:, 0:kn], scalar=0.125, in1=biasB[:, h, c0 + k0:c0 + k0 + kn],
                        op0=ALU.mult, op1=ALU.add), reads=[Bp, B_c2], writes=[BsB])
                yield
                st, Bst = st2_r.next()
                P.op("dve", lambda hh, sB=sB, st=st: hh.tensor_reduce(out=st[:, 0:1], in_=sB[:, 0:nk], axis=AX.X, op=ALU.max, negate=True),
                     reads=[BsB], writes=[Bst])
                pB, BpB = pB_r.next()
                P.op("act", lambda hh, sB=sB, st=st, pB=pB: hh.activation(
                    out=pB[:, 0:nk], in_=sB[:, 0:nk], func=AF.Exp, bias=st[:, 0:1], scale=1.0, accum_out=st[:, 1:2]),
                    reads=[BsB, Bst], writes=[BpB, Bst])
                yield
                P.op("dve", lambda hh, st=st, rc=rc, h=h: hh.reciprocal(out=rc[:, h:h + 1], in_=st[:, 1:2]), reads=[Bst], writes=[Brc])
                yield
                yield 'S'
                ptB, BptB = ptB_r.next()
                pT, BpT = PSB()
                for b in range(nbk):
                    P.op("pe", lambda hh, pT=pT, b=b, pB=pB: hh.transpose(
                        out=pT[:, b * 128:(b + 1) * 128], in_=pB[:, b * 128:(b + 1) * 128], identity=ident_b[:]),
                        reads=[BpB, B_const], writes=[BpT])
                evac(ptB[:, 0:nbk * 128], pT[:, 0:nbk * 128], [BpT], [BptB])
                yield
                for b in range(nbk):
                    P.op("pe", lambda hh, b=b, h=h, ptB=ptB, vb=vb: hh.matmul(
                        accC[0][:, h * 64:(h + 1) * 64], lhsT=ptB[:, b * 128:(b + 1) * 128], rhs=vb[:, b, h * 64:(h + 1) * 64],
                        start=(b == 0), stop=(b == nbk - 1)), reads=[BptB, Bvb], writes=[Bacc])
        yield from run_pipelined_gen(head_gen)
        yb, Byb = yb_r.next()
        for h in range(8):
            P.op("dve", lambda hh, h=h, rc=rc, yb=yb: hh.tensor_scalar(
                out=yb[:, h * 64:(h + 1) * 64], in0=accC[0][:, h * 64:(h + 1) * 64], scalar1=rc[:, h:h + 1],
                scalar2=None, op0=ALU.mult), reads=[Bacc, Brc], writes=[Byb])
        yield
        pT, BpT = PSB()
        for b in range(4):
            P.op("pe", lambda hh, pT=pT, b=b, yb=yb: hh.transpose(
                out=pT[:, b * 128:(b + 1) * 128], in_=yb[:, b * 128:(b + 1) * 128], identity=ident_b[:]),
                reads=[Byb, B_const], writes=[BpT])
        ybs, Bybs = ybs_r.next()
        evac(ybs[:, :], pT[:, 0:512], [BpT], [Bybs])
        P.dma("pool", ybT_s[:, :, tok].rearrange("c p t -> p c t"), ybs[:].rearrange("p (c t) -> p c t", c=4), reads=[Bybs], writes=tb("ybT", tg, tg + 1))

    for seq in range(nseq):
        kidx2, Bk = kidx2_r.next()
        ckvT, BcT = ckvT_r.next()
        ckvtm, Bcm = ckvtm_r.next()
        sl = slice(seq * SEQ, (seq + 1) * SEQ)
        P.dma("sp", kidx2[0:64, :], kidxT_s[:, sl], reads=tb("kidxT", seq * 16, seq * 16 + 16), writes=[Bk])
        P.dma("sp", kidx2[64:128, :], kidxT_s[:, sl], reads=tb("kidxT", seq * 16, seq * 16 + 16), writes=[Bk])
        P.dma("sp", ckvT[:], ckvT_s[:, sl], reads=tb("ckvT", seq * 16, seq * 16 + 16), writes=[BcT])
        P.dma("sp", ckvtm[:], ckvtm_s[sl, :].rearrange("(b p) r -> p b r", p=128),
              reads=tb("ckvtm", seq * 16, seq * 16 + 16), writes=[Bcm])
        if "A" in mixers:
            makers = []
            for t in range(16):
                makers.append(lambda t=t: indexer(seq, t, kidx2, Bk))
                makers.append(lambda t=t: thresh_gen(t))
            bg = BG(makers)
            bgref[0] = bg
        for t in range(16):
            if "A" in mixers:
                bg.force(2 * t + 1)
                bg.limit = 2 * (t + 2)
                if "B" in mixers:
                    bgB[0] = mixerB(seq, t)
                attention(seq, t, ckvT, BcT, ckvtm, Bcm, None)
                while bgB[0] is not None:
                    sp(0)
            elif "B" in mixers:
                for _ in mixerB(seq, t):
                    pass
        bgref[0] = None
    P.barrier()
    if stop_after == "CD":
        P.emit()
        return nc

    S3 = Arena(nc, A0, SBMAX)
    psn[0] = 6
    B_c3 = Buf()
    wa = S3.t("wa", [128, 4, D], BF16)
    wb = S3.t("wb", [128, 4, D], BF16)
    wo = S3.t("wo", [128, 8, D], BF16)
    ln1g = S3.t("ln1g", [128, D], F32)
    ln1b = S3.t("ln1b", [128, D], F32)
    wr = S3.t("wr", [128, 8, NEXP], BF16)
    brt = S3.t("brt", [128, NEXP], F32)
    P.dma("pool", wa[:], wa_d.rearrange("(k p) n -> p k n", p=128), writes=[B_c3])
    P.dma("pool", wb[:], wb_d.rearrange("(k p) n -> p k n", p=128), writes=[B_c3])
    P.dma("pool", wo[:], wo_d.rearrange("(k p) n -> p k n", p=128), writes=[B_c3])
    P.dma("sp", ln1g[:], ln1g_d, writes=[B_c3])
    P.dma("sp", ln1b[:], ln1b_d, writes=[B_c3])
    P.dma("pool", wr[:], wr_d.rearrange("(k p) n -> p k n", p=128), writes=[B_c3])
    P.dma("sp", brt[:], br_d, writes=[B_c3])
    yag_r = Rot(S3, "yag", [128, 4, 512], BF16, 2)
    ybg_r = Rot(S3, "ybg", [128, 4, 512], BF16, 2)
    sga_r = Rot(S3, "sga", [128, 8, 512], BF16, 2)
    sgb_r = Rot(S3, "sgb", [128, 8, 512], BF16, 2)
    t1_r = Rot(S3, "t1", [128, 512], F32, 2)
    t2_r = Rot(S3, "t2", [128, 512], F32, 2)
    mT_r = Rot(S3, "mT", [128, 8, 512], BF16, 2)
    xr_r = Rot(S3, "xr", [128, D], F32, 2)
    z_r = Rot(S3, "z", [128, D], F32, 2)
    x1t_r = Rot(S3, "x1t", [128, D], F32, 2)
    x1b_r = Rot(S3, "x1b", [128, 1024], BF16, 2)
    x1Tb_r = Rot(S3, "x1Tb", [128, 1024], BF16, 2)
    st3_r = Rot(S3, "st3", [128, 16], F32, 4)
    lg_r = Rot(S3, "lg", [128, NEXP], F32, 2)
    ex_r = Rot(S3, "ex", [128, NEXP], F32, 2)
    gs_r = Rot(S3, "gs", [128, NEXP], F32, 2)
    m8b_r = Rot(S3, "m8b", [128, 8], F32, 2)
    selb_r = Rot(S3, "selb", [128, NEXP], BF16, 2)
    rw_r = Rot(S3, "rw", [128, 4 * NEXP], F32, 2)
    sf_r = Rot(S3, "sf", [128, 12], F32, 3)
    su_r = Rot(S3, "su", [128, 1], U32, 8)
    ustr = S3.t("ustr", [128, 128], BF16)
    onesb = S3.t("onesb", [128, 128], BF16)
    ebase = S3.t("ebase", [128, NEXP], F32)
    P.dma("sp", ustr[:], ustr_d, writes=[B_c3])
    P.dma("sp", onesb[:], onesb_d, writes=[B_c3])
    P.dma("sp", ebase[:], ebase_d, writes=[B_c3])
    rt_tiles = [(S3.t("rt0", [128, NEXP], F32), Buf()), (S3.t("rt1", [128, NEXP], F32), Buf())]
    P.op("dve", lambda h: h.memset(rt_tiles[0][0][:], 0.0), writes=[rt_tiles[0][1]])

    def layer_norm(z, Bz, g_t, b_t, Bg, out, Bout, st_rot):
        st, Bst = st_rot.next()
        P.op("dve", lambda h: h.bn_stats(out=st[:, 0:6], in_=z[:, 0:512]), reads=[Bz], writes=[Bst])
        P.op("dve", lambda h: h.bn_stats(out=st[:, 6:12], in_=z[:, 512:1024]), reads=[Bz], writes=[Bst])
        P.op("dve", lambda h: h.bn_aggr(out=st[:, 12:14], in_=st[:, 0:12]), reads=[Bst], writes=[Bst])
        P.op("act", lambda h: h.activation(out=st[:, 14:15], in_=st[:, 13:14], func=AF.Sqrt, scale=1.0, bias=eps_t[:, 0:1]),
             reads=[Bst, B_const], writes=[Bst])
        P.op("dve", lambda h: h.reciprocal(out=st[:, 15:16], in_=st[:, 14:15]), reads=[Bst], writes=[Bst])
        P.op("dve", lambda h: h.tensor_scalar(out=z[:], in0=z[:], scalar1=st[:, 12:13], scalar2=st[:, 15:16],
                                              op0=ALU.subtract, op1=ALU.mult), reads=[Bz, Bst], writes=[Bz])
        P.op("dve", lambda h: h.tensor_tensor(out=z[:], in0=z[:], in1=g_t[:], op=ALU.mult), reads=[Bz, Bg], writes=[Bz])
        P.op("dve", lambda h: h.tensor_tensor(out=out[:], in0=z[:], in1=b_t[:], op=ALU.add), reads=[Bz, Bg], writes=[Bout])

    for g in range(TOK // 512):
        gs_ = slice(g * 512, (g + 1) * 512)
        t0 = g * 4
        yag, Byag = yag_r.next()
        ybg, Bybg = ybg_r.next()
        sga, Bsga = sga_r.next()
        sgb, Bsgb = sgb_r.next()
        P.dma("sp", yag[:], yaT_s[:, :, gs_].rearrange("c p t -> p c t"), reads=tb("yaT", t0, t0 + 4), writes=[Byag])
        P.dma("sp", ybg[:], ybT_s[:, :, gs_].rearrange("c p t -> p c t"), reads=tb("ybT", t0, t0 + 4), writes=[Bybg])
        P.dma("sp", sga[:], featT[16:24, :, gs_].rearrange("c p t -> p c t"),
              reads=[b for c in range(16, 24) for b in tb(("f", c), t0, t0 + 4)], writes=[Bsga])
        P.dma("sp", sgb[:], featT[24:32, :, gs_].rearrange("c p t -> p c t"),
              reads=[b for c in range(24, 32) for b in tb(("f", c), t0, t0 + 4)], writes=[Bsgb])
        mT, BmT = mT_r.next()
        for n in range(8):
            pa, Bpa = PSF()
            for k in range(4):
                P.op("pe", lambda h, pa=pa, k=k, n=n, yag=yag: h.matmul(
                    pa[:, :], lhsT=wa[:, k, n * 128:(n + 1) * 128], rhs=yag[:, k, :], start=(k == 0), stop=(k == 3)),
                    reads=[B_c3, Byag], writes=[Bpa])
            pb_, Bpb_ = PSF()
            for k in range(4):
                P.op("pe", lambda h, pb_=pb_, k=k, n=n, ybg=ybg: h.matmul(
                    pb_[:, :], lhsT=wb[:, k, n * 128:(n + 1) * 128], rhs=ybg[:, k, :], start=(k == 0), stop=(k == 3)),
                    reads=[B_c3, Bybg], writes=[Bpb_])
            t1, Bt1 = t1_r.next()
            t2, Bt2 = t2_r.next()
            P.op("dve", lambda h, t1=t1, pa=pa, n=n, sga=sga: h.tensor_tensor(out=t1[:], in0=pa[:, :], in1=sga[:, n, :], op=ALU.mult),
                 reads=[Bpa, Bsga], writes=[Bt1])
            P.op("dve", lambda h, t2=t2, pb_=pb_, n=n, sgb=sgb: h.tensor_tensor(out=t2[:], in0=pb_[:, :], in1=sgb[:, n, :], op=ALU.mult),
                 reads=[Bpb_, Bsgb], writes=[Bt2])
            P.op("dve", lambda h, t1=t1, t2=t2, n=n, mT=mT: h.tensor_tensor(out=mT[:, n, :], in0=t1[:], in1=t2[:], op=ALU.add),
                 reads=[Bt1, Bt2], writes=[BmT])
        for tt in range(4):
            t = t0 + tt
            tok = slice(t * 128, (t + 1) * 128)
            xr, Bxr = xr_r.next()
            P.dma("sp", xr[:], x_d[tok, :], writes=[Bxr])
            z, Bz = z_r.next()
            for nh in range(2):
                po, Bpo = PSF()
                for k in range(8):
                    P.op("pe", lambda h, po=po, k=k, nh=nh, tt=tt, mT=mT: h.matmul(
                        po[:, :], lhsT=mT[:, k, tt * 128:(tt + 1) * 128], rhs=wo[:, k, nh * 512:(nh + 1) * 512],
                        start=(k == 0), stop=(k == 7)), reads=[BmT, B_c3], writes=[Bpo])
                P.op("dve", lambda h, po=po, nh=nh, xr=xr, z=z: h.scalar_tensor_tensor(
                    out=z[:, nh * 512:(nh + 1) * 512], in0=xr[:, nh * 512:(nh + 1) * 512], scalar=ALPHA, in1=po[:, :],
                    op0=ALU.mult, op1=ALU.add), reads=[Bpo, Bxr], writes=[Bz])
            x1t, Bx1 = x1t_r.next()
            layer_norm(z, Bz, ln1g, ln1b, B_c3, x1t, Bx1, st3_r)
            P.dma("pool", x1_s[tok, :], x1t[:], reads=[Bx1], writes=tb("x1", t, t + 1))
            x1b, Bx1b = x1b_r.next()
            P.op("act", lambda h, x1b=x1b, x1t=x1t: h.copy(out=x1b[:], in_=x1t[:]), reads=[Bx1], writes=[Bx1b])
            x1Tb, Bxb = x1Tb_r.next()
            pt_, Bpt_ = PSB()
            for b in range(8):
                P.op("pe", lambda h, pt_=pt_, b=b, x1b=x1b: h.transpose(
                    out=pt_[:, b * 128:(b + 1) * 128], in_=x1b[:, b * 128:(b + 1) * 128], identity=ident_b[:]),
                    reads=[Bx1b, B_const], writes=[Bpt_])
            evac(x1Tb[:, :], pt_[:, :], [Bpt_], [Bxb])
            P.dma("pool", x1T_s[:, :, tok].rearrange("c p t -> p c t"), x1Tb[:].rearrange("p (c t) -> p c t", c=8), reads=[Bxb], writes=tb("x1T", t, t + 1))
            pr, Bpr = PSF()
            for k in range(8):
                P.op("pe", lambda h, pr=pr, k=k, x1Tb=x1Tb: h.matmul(
                    pr[:, 0:NEXP], lhsT=x1Tb[:, k * 128:(k + 1) * 128], rhs=wr[:, k, :], start=(k == 0), stop=(k == 7)),
                    reads=[Bxb, B_c3], writes=[Bpr])
            lg, Blg = lg_r.next()
            P.op("dve", lambda h, lg=lg, pr=pr: h.tensor_tensor(out=lg[:], in0=pr[:, 0:NEXP], in1=brt[:], op=ALU.add),
                 reads=[Bpr, B_c3], writes=[Blg])
            m8, Bm8 = m8b_r.next()
            P.op("dve", lambda h, lg=lg, m8=m8: h.max(out=m8[:, 0:8], in_=lg[:]), reads=[Blg], writes=[Bm8])
            st, Bst = st3_r.next()
            P.op("dve", lambda h, m8=m8, st=st: h.tensor_scalar(out=st[:, 0:1], in0=m8[:, 0:1], scalar1=-1.0, scalar2=None, op0=ALU.mult),
                 reads=[Bm8], writes=[Bst])
            ex, Bex = ex_r.next()
            P.op("act", lambda h, ex=ex, lg=lg, st=st: h.activation(out=ex[:], in_=lg[:], func=AF.Exp, bias=st[:, 0:1], scale=1.0),
                 reads=[Blg, Bst], writes=[Bex])
            gs, Bgs = gs_r.next()
            P.op("dve", lambda h, gs=gs, lg=lg, m8=m8, ex=ex: h.scalar_tensor_tensor(
                out=gs[:], in0=lg[:], scalar=m8[:, 3:4], in1=ex[:], op0=ALU.is_ge, op1=ALU.mult),
                reads=[Blg, Bm8, Bex], writes=[Bgs])
            P.op("dve", lambda h, gs=gs, st=st: h.tensor_reduce(out=st[:, 1:2], in_=gs[:], axis=AX.X, op=ALU.add),
                 reads=[Bgs], writes=[Bst])
            P.op("dve", lambda h, st=st: h.reciprocal(out=st[:, 2:3], in_=st[:, 1:2]), reads=[Bst], writes=[Bst])
            P.op("dve", lambda h, gs=gs, st=st: h.tensor_scalar(out=gs[:], in0=gs[:], scalar1=st[:, 2:3], scalar2=None, op0=ALU.mult),
                 reads=[Bgs, Bst], writes=[Bgs])
            selb, Bsel = selb_r.next()
            P.op("dve", lambda h, selb=selb, gs=gs: h.tensor_scalar(out=selb[:], in0=gs[:], scalar1=0.0, scalar2=None, op0=ALU.is_gt),
                 reads=[Bgs], writes=[Bsel])
            pp, Bpp = PSF()
            P.op("pe", lambda h, pp=pp, selb=selb: h.matmul(pp[:, 0:NEXP], lhsT=ustr[:], rhs=selb[:], start=True, stop=True),
                 reads=[Bsel, B_c3], writes=[Bpp])
            P.op("pe", lambda h, pp=pp, selb=selb: h.matmul(pp[:, NEXP:2 * NEXP], lhsT=onesb[:], rhs=selb[:], start=True, stop=True),
                 reads=[Bsel, B_c3], writes=[Bpp])
            rt_old, Brt_old = rt_tiles[t % 2]
            rt_new, Brt_new = rt_tiles[(t + 1) % 2]
            rw, Brw = rw_r.next()
            P.op("dve", lambda h, rw=rw, pp=pp, rt_old=rt_old: h.tensor_tensor(out=rw[:, 0:NEXP], in0=pp[:, 0:NEXP], in1=rt_old[:], op=ALU.add),
                 reads=[Bpp, Brt_old], writes=[Brw])
            P.op("dve", lambda h, pp=pp, rt_old=rt_old, rt_new=rt_new: h.tensor_tensor(out=rt_new[:], in0=pp[:, NEXP:2 * NEXP], in1=rt_old[:], op=ALU.add),
                 reads=[Bpp, Brt_old], writes=[Brt_new])
            P.op("dve", lambda h, rw=rw: h.tensor_scalar(out=rw[:, 2 * NEXP:3 * NEXP], in0=rw[:, 0:NEXP], scalar1=float(CAP), scalar2=100000.0,
                                                         op0=ALU.is_ge, op1=ALU.mult), reads=[Brw], writes=[Brw])
            P.op("dve", lambda h, rw=rw: h.tensor_tensor(out=rw[:, NEXP:2 * NEXP], in0=rw[:, 0:NEXP], in1=ebase[:], op=ALU.add),
                 reads=[Brw, B_c3], writes=[Brw])
            P.op("dve", lambda h, rw=rw: h.tensor_tensor(out=rw[:, NEXP:2 * NEXP], in0=rw[:, NEXP:2 * NEXP], in1=rw[:, 2 * NEXP:3 * NEXP], op=ALU.add),
                 reads=[Brw], writes=[Brw])
            P.op("dve", lambda h, rw=rw: h.tensor_scalar(out=rw[:, 2 * NEXP:3 * NEXP], in0=rw[:, NEXP:2 * NEXP], scalar1=-1.0, scalar2=BIGM,
                                                         op0=ALU.mult, op1=ALU.add), reads=[Brw], writes=[Brw])
            P.op("dve", lambda h, rw=rw, selb=selb: h.tensor_tensor(out=rw[:, 3 * NEXP:4 * NEXP], in0=rw[:, 2 * NEXP:3 * NEXP], in1=selb[:], op=ALU.mult),
                 reads=[Brw, Bsel], writes=[Brw])
            k8, Bk8 = m8b_r.next()
            P.op("dve", lambda h, rw=rw, k8=k8: h.max(out=k8[:, 0:8], in_=rw[:, 3 * NEXP:4 * NEXP]), reads=[Brw], writes=[Bk8])
            sf, Bsf = sf_r.next()
            P.op("dve", lambda h, sf=sf, k8=k8: h.tensor_scalar(out=sf[:, 0:4], in0=k8[:, 0:4], scalar1=-1.0, scalar2=BIGM, op0=ALU.mult, op1=ALU.add),
                 reads=[Bk8], writes=[Bsf])
            for k in range(4):
                P.op("dve", lambda h, rw=rw, sf=sf, gs=gs, k=k: h.scalar_tensor_tensor(
                    out=rw[:, 2 * NEXP:3 * NEXP], in0=rw[:, NEXP:2 * NEXP], scalar=sf[:, k:k + 1], in1=gs[:],
                    op0=ALU.is_equal, op1=ALU.mult, accum_out=sf[:, 4 + k:5 + k]), reads=[Brw, Bsf, Bgs], writes=[Brw, Bsf])
            P.op("dve", lambda h, sf=sf: h.tensor_scalar(out=sf[:, 8:12], in0=sf[:, 0:4], scalar1=float(NSLOT), scalar2=None, op0=ALU.is_lt),
                 reads=[Bsf], writes=[Bsf])
            P.op("dve", lambda h, sf=sf: h.tensor_tensor(out=sf[:, 4:8], in0=sf[:, 4:8], in1=sf[:, 8:12], op=ALU.mult),
                 reads=[Bsf], writes=[Bsf])
            P.dma("pool", route_s[tok, :], sf[:, 0:8], reads=[Bsf], writes=tb("route", t, t + 1))
            for k in range(4):
                su, Bsu = su_r.next()
                P.op("dve", lambda h, su=su, sf=sf, k=k: h.tensor_copy(out=su[:], in_=sf[:, k:k + 1]), reads=[Bsf], writes=[Bsu])
                P.idma(xd_s, bass.IndirectOffsetOnAxis(ap=su[:, 0:1], axis=0), x1b[:], None, reads=[Bx1b, Bsu], writes=[], bc=NSLOT - 1)
    P.barrier()
    if stop_after == "E":
        P.emit()
        return nc

    S4 = Arena(nc, A0, SBMAX)
    psn[0] = 6
    B_c4 = Buf()
    bgt = S4.t("bgt", [128, NEXP, 8], F32)
    but = S4.t("but", [128, NEXP, 8], F32)
    ones1 = S4.t("ones1", [1, 128], BF16)
    P.op("dve", lambda h: h.memset(ones1[:], 1.0), writes=[B_c4])
    P.dma("sp", bgt[:], bg_d, writes=[B_c4])
    P.dma("sp", but[:], bu_d, writes=[B_c4])
    P.op("dve", lambda h: h.tensor_scalar(out=but[:], in0=but[:], scalar1=1.0, scalar2=None, op0=ALU.add), reads=[B_c4], writes=[B_c4])
    wg_r = Rot(S4, "wg", [128, 8, DFF], BF16, 2)
    wu_r = Rot(S4, "wu", [128, 8, DFF], BF16, 2)
    wd_r = Rot(S4, "wd", [128, 8, D], BF16, 2)
    bde_r = Rot(S4, "bde", [1, D], BF16, 2)
    xrow_r = Rot(S4, "xrow", [128, D], BF16, 6)
    xT_r = Rot(S4, "xTe", [128, 8, 512], BF16, 2)
    hT_r = Rot(S4, "hT", [128, 8, 512], BF16, 2)
    hg_r = Rot(S4, "hg", [128, 512], F32, 2)
    sg_r = Rot(S4, "sg", [128, 512], F32, 2)
    v_r = Rot(S4, "v", [128, 512], F32, 2)
    hu_r = Rot(S4, "hu", [128, 512], F32, 2)
    g2_r = Rot(S4, "g2", [128, 512], F32, 2)
    ysb_r = Rot(S4, "ysb", [128, D], F32, 3)
    SUB = [(0, 512), (512, 512), (1024, CAP - 1024)]
    def wload(e):
        wg, Bwg = wg_r.next()
        wu, Bwu = wu_r.next()
        wd, Bwd = wd_r.next()
        bde, Bbde = bde_r.next()
        P.dma("pool", wg[:], wg_d[e].rearrange("(k p) f -> p k f", p=128), writes=[Bwg])
        P.dma("pool", wu[:], wu_d[e].rearrange("(k p) f -> p k f", p=128), writes=[Bwu])
        P.dma("pool", wd[:], wd_d[e].rearrange("(k p) f -> p k f", p=128), writes=[Bwd])
        P.dma("pool", bde[:], bd_d[e:e + 1, :], writes=[Bbde])
        return (wg, Bwg, wu, Bwu, wd, Bwd, bde, Bbde)

    pend_down = [None]

    def down_proj(e, s0, nt_, hT, BhT, wd, Bwd, bde, Bbde):
        for tt in range(nt_):
            r0 = e * CAP + s0 + tt * 128
            ysb, Bysb = ysb_r.next()
            for nh in range(2):
                py, Bpy = PSF()
                for fc in range(8):
                    P.op("pe", lambda h, py=py, fc=fc, tt=tt, nh=nh, hT=hT, wd=wd: h.matmul(
                        py[:, :], lhsT=hT[:, fc, tt * 128:(tt + 1) * 128], rhs=wd[:, fc, nh * 512:(nh + 1) * 512],
                        start=(fc == 0), stop=False), reads=[BhT, Bwd], writes=[Bpy])
                P.op("pe", lambda h, py=py, nh=nh, bde=bde: h.matmul(
                    py[:, :], lhsT=ones1[:], rhs=bde[:, nh * 512:(nh + 1) * 512], start=False, stop=True),
                    reads=[Bbde, B_c4], writes=[Bpy])
                evac(ysb[:, nh * 512:(nh + 1) * 512], py[:, :], [Bpy], [Bysb])
            for hf in range(2):
                P.dma("pool", yd_h[hf][r0:r0 + 128, :], ysb[:, hf * 512:(hf + 1) * 512], reads=[Bysb], writes=[])

    wnext = wload(0)
    for e in range(NEXP):
        wg, Bwg, wu, Bwu, wd, Bwd, bde, Bbde = wnext
        for si, (s0, ns) in enumerate(SUB):
            nt_ = ns // 128
            xT, BxT = xT_r.next()
            for tt in range(nt_):
                r0 = e * CAP + s0 + tt * 128
                xrow, Bxrow = xrow_r.next()
                P.dma("sp", xrow[:], xd_s[r0:r0 + 128, :], writes=[Bxrow])
                pt_, Bpt_ = PSB()
                for b in range(8):
                    P.op("pe", lambda h, pt_=pt_, b=b, xrow=xrow: h.transpose(
                        out=pt_[:, b * 128:(b + 1) * 128], in_=xrow[:, b * 128:(b + 1) * 128], identity=ident_b[:]),
                        reads=[Bxrow, B_const], writes=[Bpt_])
                evac(xT[:, :, tt * 128:(tt + 1) * 128], pt_[:, :].rearrange("p (b t) -> p b t", b=8), [Bpt_], [BxT])
            hT, BhT = hT_r.next()
            for fc in range(8):
                pg, Bpg = PSF()
                for k in range(8):
                    P.op("pe", lambda h, pg=pg, k=k, fc=fc, wg=wg, xT=xT, ns=ns: h.matmul(
                        pg[:, 0:ns], lhsT=wg[:, k, fc * 128:(fc + 1) * 128], rhs=xT[:, k, 0:ns], start=(k == 0), stop=(k == 7)),
                        reads=[Bwg, BxT], writes=[Bpg])
                pu, Bpu = PSF()
                for k in range(8):
                    P.op("pe", lambda h, pu=pu, k=k, fc=fc, wu=wu, xT=xT, ns=ns: h.matmul(
                        pu[:, 0:ns], lhsT=wu[:, k, fc * 128:(fc + 1) * 128], rhs=xT[:, k, 0:ns], start=(k == 0), stop=(k == 7)),
                        reads=[Bwu, BxT], writes=[Bpu])
                hg, Bhg = hg_r.next()
                sg, Bsg = sg_r.next()
                v, Bv = v_r.next()
                hu, Bhu = hu_r.next()
                g2, Bg2 = g2_r.next()
                P.op("dve", lambda h, hg=hg, pg=pg, e=e, fc=fc, ns=ns: h.tensor_scalar(
                    out=hg[:, 0:ns], in0=pg[:, 0:ns], scalar1=bgt[:, e, fc:fc + 1], scalar2=7.0, op0=ALU.add, op1=ALU.min),
                    reads=[Bpg, B_c4], writes=[Bhg])
                P.op("act", lambda h, sg=sg, hg=hg, ns=ns: h.activation(out=sg[:, 0:ns], in_=hg[:, 0:ns], func=AF.Sigmoid, scale=1.702),
                     reads=[Bhg], writes=[Bsg])
                P.op("act", lambda h, v=v, pu=pu, e=e, fc=fc, ns=ns: h.activation(
                    out=v[:, 0:ns], in_=pu[:, 0:ns], func=AF.Identity, bias=but[:, e, fc:fc + 1], scale=1.0),
                    reads=[Bpu, B_c4], writes=[Bv])
                P.op("dve", lambda h, hu=hu, v=v, ns=ns: h.tensor_scalar(out=hu[:, 0:ns], in0=v[:, 0:ns], scalar1=8.0, scalar2=-6.0,
                                                                         op0=ALU.min, op1=ALU.max), reads=[Bv], writes=[Bhu])
                P.op("dve", lambda h, g2=g2, hg=hg, sg=sg, ns=ns: h.tensor_tensor(out=g2[:, 0:ns], in0=hg[:, 0:ns], in1=sg[:, 0:ns], op=ALU.mult),
                     reads=[Bhg, Bsg], writes=[Bg2])
                P.op("dve", lambda h, hT=hT, fc=fc, hu=hu, g2=g2, ns=ns: h.tensor_tensor(out=hT[:, fc, 0:ns], in0=hu[:, 0:ns], in1=g2[:, 0:ns], op=ALU.mult),
                     reads=[Bhu, Bg2], writes=[BhT])
            if pend_down[0] is not None:
                pend_down[0]()
            if si == 0 and e + 1 < NEXP:
                wnext = wload(e + 1)
            pend_down[0] = (lambda e=e, s0=s0, nt_=nt_, hT=hT, BhT=BhT, wd=wd, Bwd=Bwd, bde=bde, Bbde=Bbde:
                            down_proj(e, s0, nt_, hT, BhT, wd, Bwd, bde, Bbde))
    pend_down[0]()

    P.barrier()
    if stop_after == "F":
        P.emit()
        return nc

    S5 = Arena(nc, A0, SBMAX)
    B_c5 = Buf()
    ln2g = S5.t("ln2g", [128, D], F32)
    ln2b = S5.t("ln2b", [128, D], F32)
    P.dma("sp", ln2g[:], ln2g_d, writes=[B_c5])
    P.dma("sp", ln2b[:], ln2b_d, writes=[B_c5])
    xo_r = Rot(S5, "xo", [128, D], F32, 5)
    yk_r = Rot(S5, "yk", [128, D], F32, 16)
    for i_ in range(len(yk_r.items)):
        yk_r.items[i_] = (yk_r.items[i_][0], [Buf(), Buf()])
    rtile_r = Rot(S5, "rtile", [128, 8], F32, 6)
    su5_r = Rot(S5, "su5", [128, 1], U32, 16)
    st5_r = Rot(S5, "st5", [128, 16], F32, 4)
    for (ykt, Byk) in yk_r.items:
        P.op("dve", lambda h, ykt=ykt: h.memset(ykt[:], 0.0), writes=Byk)
    for t in range(NT):
        tok = slice(t * 128, (t + 1) * 128)
        xo, Bxo = xo_r.next()
        rtile, Brt = rtile_r.next()
        P.dma("sp", xo[:], x1_s[tok, :], reads=tb("x1", t, t + 1), writes=[Bxo])
        P.dma("sp", rtile[:], route_s[tok, :], reads=tb("route", t, t + 1), writes=[Brt])
        yks = []
        for k in range(4):
            su, Bsu = su5_r.next()
            P.op("dve", lambda h, su=su, rtile=rtile, k=k: h.tensor_copy(out=su[:], in_=rtile[:, k:k + 1]), reads=[Brt], writes=[Bsu])
            ykt, Byk = yk_r.next()
            for hf in range(2):
                P.idma(ykt[:, hf * 512:(hf + 1) * 512], None, yd_h[hf], bass.IndirectOffsetOnAxis(ap=su[:, 0:1], axis=0),
                       reads=[Bsu], writes=[Byk[hf]], bc=NSLOT - 1)
            yks.append((ykt, Byk))
        P.op("dve", lambda h, xo=xo: h.tensor_scalar(out=xo[:], in0=xo[:], scalar1=ALPHA, scalar2=None, op0=ALU.mult),
             reads=[Bxo], writes=[Bxo])
        for k in range(4):
            ykt, Byk = yks[k]
            P.op("dve", lambda h, xo=xo, ykt=ykt, rtile=rtile, k=k: h.scalar_tensor_tensor(
                out=xo[:], in0=ykt[:], scalar=rtile[:, 4 + k:5 + k], in1=xo[:], op0=ALU.mult, op1=ALU.add),
                reads=[Bxo, Brt] + Byk, writes=[Bxo])
        layer_norm(xo, Bxo, ln2g, ln2b, B_c5, xo, Bxo, st5_r)
        P.dma("act", out_d[tok, :], xo[:], reads=[Bxo])
    P.barrier()
    P.emit()
    return nc


def _consts():
    p = np.arange(128)[:, None]
    j = np.arange(2048)[None, :]
    dist = np.abs(p - j + 1920)
    negD = ((-16.0 * (dist // 16)).astype(np.float32), (-(dist % 16)).astype(np.float32))
    jj = np.arange(640)[None, :]
    kp = jj - 512
    cq = p // 64
    valid = (kp >= cq * 64 - 512) & (kp < cq * 64 + 64)
    rel = np.clip(p - kp, -128, 128) + 128
    maskB = np.where(valid, 0.0, NEG).astype(np.float32)
    return negD, rel, maskB


def prep_inputs(inp, nseq=4, ncores=NCORES):
    f = lambda a: np.ascontiguousarray(np.asarray(a, dtype=np.float32))
    negD, rel, maskB = _consts()
    bc = lambda v, n=128: np.ascontiguousarray(np.broadcast_to(np.asarray(v, np.float32).reshape(1, -1), (n, v.size)))
    w_uk = f(inp["w_uk"][0])
    w_uk2 = np.ascontiguousarray(w_uk.reshape(4, 2, 64, 128).transpose(1, 2, 0, 3).reshape(128, 4, 128))
    relb = np.ascontiguousarray(f(inp["rel_bias"][0])[:, rel])
    bgp = np.ascontiguousarray(f(inp["b_gate"][0]).reshape(NEXP, 8, 128).transpose(2, 0, 1))
    bup = np.ascontiguousarray(f(inp["b_up"][0]).reshape(NEXP, 8, 128).transpose(2, 0, 1))
    shared = {
        "w_in": f(inp["w_in"][0]), "kvg_bc": bc(inp["kv_norm_g"][0]), "ikg_bc": bc(inp["idx_k_norm_g"][0]),
        "ikb_bc": bc(inp["idx_k_norm_b"][0]), "w_uk2": w_uk2, "w_uv": f(inp["w_uv"][0]), "relb": relb,
        "maskB": maskB, "negDh": negD[0].astype(ml_dtypes.bfloat16), "negDl": negD[1].astype(ml_dtypes.bfloat16),
        "slopeI": np.ascontiguousarray(np.stack([np.eye(128, dtype=np.float32) * 2.0 ** (-(h + 1)) for h in range(8)], axis=1)).astype(ml_dtypes.bfloat16), "w_branch_a": f(inp["w_branch_a"][0]), "w_branch_b": f(inp["w_branch_b"][0]),
        "w_out": f(inp["w_out"][0]), "ln1g_bc": bc(inp["ln1_g"][0]), "ln1b_bc": bc(inp["ln1_b"][0]),
        "w_router": f(inp["w_router"][0]), "br_bc": bc(inp["b_router"][0]), "w_gate": f(inp["w_gate"][0]),
        "w_up": f(inp["w_up"][0]), "w_down": f(inp["w_down"][0]), "b_gate_p": bgp, "b_up_p": bup,
        "b_down": f(inp["b_down"][0]), "ln2g_bc": bc(inp["ln2_g"][0]), "ln2b_bc": bc(inp["ln2_b"][0]),
        "ident_bf": np.eye(128, dtype=np.float32).astype(ml_dtypes.bfloat16), "ident_f": np.eye(128, dtype=np.float32),
        "ustrict": np.triu(np.ones((128, 128), np.float32), 1).astype(ml_dtypes.bfloat16),
        "pow2": np.ascontiguousarray(np.broadcast_to((2.0 ** -(np.arange(21, dtype=np.float64) + 1)).astype(np.float32)[None, :], (128, 21))),
        "ones_bf": np.ones((128, 128), np.float32).astype(ml_dtypes.bfloat16),
        "ebase": np.ascontiguousarray(np.broadcast_to((np.arange(NEXP, dtype=np.float32) * CAP)[None, :], (128, NEXP))),
    }
    x = np.asarray(inp["x"], dtype=np.float32)
    maps = []
    for c in range(ncores):
        xs = x[c * nseq:(c + 1) * nseq].reshape(nseq * SEQ, D)
        m = dict(shared)
        m["x"] = np.ascontiguousarray(xs)
        m["xT"] = np.ascontiguousarray(xs.T)
        maps.append(m)
    return maps


def kernel(**inputs):
    nseq = inputs["x"].shape[0] // NCORES
    nc = build(nseq)
    maps = prep_inputs(inputs, nseq)
    res = run_bass_kernel_spmd(nc, maps, core_ids=list(range(NCORES)))
    out = np.concatenate([np.asarray(r["out"]).reshape(nseq, SEQ, D) for r in res.results], axis=0)
    return out.astype(np.float32)
```

```python
import numpy as np
import ml_dtypes
import concourse.bass as bass
import concourse.mybir as mybir
from concourse.bass_utils import run_bass_kernel_spmd

F32 = mybir.dt.float32
BF16 = mybir.dt.bfloat16
ALU = mybir.AluOpType
AF = mybir.ActivationFunctionType
AX = mybir.AxisListType

D = 1024
SEQ = 2048
NCORES = 8
NEXP = 32
DFF = 1024
INW = 4808
ALPHA = 2.0 ** 0.25
EPS = 1e-5
IDX_SCALE = 512.0 ** -0.5
NEG = -30000.0
TOPK = 256
CAP = 1280
NSLOT = NEXP * CAP
BIGM = 262144.0
U32 = mybir.dt.uint32


class Buf:
    __slots__ = ("w", "r")

    def __init__(self):
        self.w = {}
        self.r = {}


class Op:
    __slots__ = ("eng", "fn", "deps", "dma", "sem", "val", "signals", "cnt")

    def __init__(self, eng, fn, dma):
        self.eng = eng
        self.fn = fn
        self.dma = dma
        self.deps = []
        self.sem = None
        self.val = 0
        self.signals = False
        self.cnt = 0


class Prog:
    ENG = ("pe", "act", "dve", "pool", "sp")

    def __init__(self, nc, ndma=48):
        self.nc = nc
        self.ops = {e: [] for e in self.ENG}
        self.ndma = ndma
        self.dma_uses = [0] * ndma
        self.dma_last = [None] * ndma
        self.dma_n = 0
        self.live_dma = []

    def _add(self, op, reads, writes):
        deps = {}
        for b in reads:
            for o in b.w.values():
                deps[o] = True
        for b in writes:
            for o in b.w.values():
                deps.setdefault(o, False)
            for o in b.r.values():
                deps.setdefault(o, False)
        for o, raw in deps.items():
            if o is not op:
                op.deps.append((o, raw))
        key = op if op.dma else op.eng
        for b in reads:
            b.r[key] = op
        for b in writes:
            b.w = {key: op}
            b.r = {}
        self.ops[op.eng].append(op)
        return op

    def op(self, eng, fn, reads=(), writes=()):
        return self._add(Op(eng, fn, False), reads, writes)

    def dma(self, q, out, in_, reads=(), writes=()):
        op = Op(q, lambda h: h.dma_start(out=out, in_=in_), True)
        slot = self.dma_n % self.ndma
        self.dma_n += 1
        prev = self.dma_last[slot]
        if prev is not None:
            op.deps.append((prev, True))
        self.dma_uses[slot] += 1
        self.dma_last[slot] = op
        op.sem = slot
        op.val = 16 * self.dma_uses[slot]
        self.live_dma.append(op)
        return self._add(op, reads, writes)

    def idma(self, out, out_off, in_, in_off, reads=(), writes=(), bc=None):
        def fn(h):
            if getattr(self, "_bcreg", None) is None:
                self._bcreg = h.alloc_register("bcreg")
                h.reg_mov(self._bcreg, bc)
                self._bcval = bc
            assert self._bcval == bc
            return h.indirect_dma_start(out=out, out_offset=out_off, in_=in_, in_offset=in_off,
                                        bounds_check=self._bcreg, oob_is_err=False)
        op = Op("pool", fn, True)
        slot = self.dma_n % self.ndma
        self.dma_n += 1
        prev = self.dma_last[slot]
        if prev is not None:
            op.deps.append((prev, True))
        self.dma_uses[slot] += 1
        self.dma_last[slot] = op
        op.sem = slot
        op.val = 16 * self.dma_uses[slot]
        self.live_dma.append(op)
        return self._add(op, reads, writes)

    def barrier(self):
        last = {e: (self.ops[e][-1] if self.ops[e] else None) for e in self.ENG}
        live = self.live_dma
        self.live_dma = []
        for e in self.ENG:
            b = Op(e, None, False)
            for e2 in self.ENG:
                if e2 != e and last[e2] is not None and not last[e2].dma and last[e2].fn is not None:
                    b.deps.append((last[e2], True))
            for d in live:
                b.deps.append((d, True))
            self.ops[e].append(b)

    @staticmethod
    def _skip(op, d, raw):
        if d.dma or op.dma:
            return False
        if d.eng != op.eng:
            return False
        if op.eng == "pe":
            return True
        return not raw

    def emit(self):
        nc = self.nc
        for e in self.ENG:
            for op in self.ops[e]:
                for d, raw in op.deps:
                    if not self._skip(op, d, raw) and not d.dma:
                        d.signals = True
        for e in self.ENG:
            c = 0
            for op in self.ops[e]:
                if op.signals and not op.dma:
                    c += 1
                    op.cnt = c
        import contextlib
        with contextlib.ExitStack() as st:
            esem = {e: st.enter_context(nc.semaphore("s_" + e)) for e in self.ENG}
            dsem = [st.enter_context(nc.semaphore("d%d" % i)) for i in range(self.ndma)]
            block = st.enter_context(nc.Block())

            def run(e, h):
                waited = {}
                for op in self.ops[e]:
                    for d, raw in op.deps:
                        if self._skip(op, d, raw):
                            continue
                        if d.dma:
                            key, sem, val = ("d", d.sem), dsem[d.sem], d.val
                        else:
                            key, sem, val = ("e", d.eng), esem[d.eng], d.cnt
                        if waited.get(key, 0) >= val:
                            continue
                        waited[key] = val
                        h.wait_ge(sem, val)
                    if op.fn is None:
                        continue
                    ins = op.fn(h)
                    if op.dma:
                        ins.then_inc(dsem[op.sem], 16)
                    elif op.signals:
                        ins.then_inc(esem[e], 1)

            @block.tensor
            def _(h):
                run("pe", h)

            @block.scalar
            def _(h):
                run("act", h)

            @block.vector
            def _(h):
                run("dve", h)

            @block.gpsimd
            def _(h):
                run("pool", h)

            @block.sync
            def _(h):
                run("sp", h)


class Arena:
    def __init__(self, nc, lo, hi):
        self.nc, self.lo, self.hi, self.n = nc, lo, hi, 0

    def t(self, name, shape, dt):
        esz = 4 if dt == F32 else 2
        per = int(np.prod(shape[1:])) * esz
        per = (per + 63) // 64 * 64
        assert self.lo + per <= self.hi, (name, self.lo, per, self.hi)
        h = self.nc.alloc_sbuf_tensor_at("%s_%d" % (name, self.lo), list(shape), dt, offset=self.lo)
        self.lo += per
        return h


class Rot:
    def __init__(self, arena, name, shape, dt, n):
        self.items = [(arena.t("%s%d" % (name, i), shape, dt), Buf()) for i in range(n)]
        self.i = 0

    def next(self):
        it = self.items[self.i % len(self.items)]
        self.i += 1
        return it


def build(nseq=4, debug=False, stop_after=None, mixers="AB"):
    nc = bass.Bass("TRN2", target_bir_lowering=False)
    P = Prog(nc)
    TOK = nseq * SEQ
    NT = TOK // 128
    dk = "ExternalOutput" if debug else "Internal"

    def din(name, shape, dt=F32):
        return nc.dram_tensor(name, list(shape), dt, kind="ExternalInput").ap()

    def dscr(name, shape, dt):
        return nc.dram_tensor(name, list(shape), dt, kind=dk).ap()

    xT_d = din("xT", [D, TOK])
    x_d = din("x", [TOK, D])
    win_d = din("w_in", [D, INW])
    kvg_d = din("kvg_bc", [128, 128])
    ikg_d = din("ikg_bc", [128, 64])
    ikb_d = din("ikb_bc", [128, 64])
    wuk_d = din("w_uk2", [128, 4, 128])
    wuv_d = din("w_uv", [8, 128, 64])
    relb_d = din("relb", [8, 128, 640])
    maskb_d = din("maskB", [128, 640])
    negdh_d = din("negDh", [128, 2048], BF16)
    negdl_d = din("negDl", [128, 2048], BF16)
    slopei_d = din("slopeI", [128, 8, 128], BF16)
    wa_d = din("w_branch_a", [512, D])
    wb_d = din("w_branch_b", [512, D])
    wo_d = din("w_out", [D, D])
    ln1g_d = din("ln1g_bc", [128, D])
    ln1b_d = din("ln1b_bc", [128, D])
    wr_d = din("w_router", [D, NEXP])
    br_d = din("br_bc", [128, NEXP])
    wg_d = din("w_gate", [NEXP, D, DFF])
    wu_d = din("w_up", [NEXP, D, DFF])
    wd_d = din("w_down", [NEXP, DFF, D])
    bg_d = din("b_gate_p", [128, NEXP, 8])
    bu_d = din("b_up_p", [128, NEXP, 8])
    bd_d = din("b_down", [NEXP, D])
    ln2g_d = din("ln2g_bc", [128, D])
    ln2b_d = din("ln2b_bc", [128, D])
    identb_d = din("ident_bf", [128, 128], BF16)
    identf_d = din("ident_f", [128, 128])
    ustr_d = din("ustrict", [128, 128], BF16)
    onesb_d = din("ones_bf", [128, 128], BF16)
    ebase_d = din("ebase", [128, NEXP])
    pow2_d = din("pow2", [128, 21])
    out_d = nc.dram_tensor("out", [TOK, D], F32, kind="ExternalOutput").ap()

    featT = dscr("featT", [32, 128, TOK], BF16)
    ckvtm_s = dscr("ckv_tm", [TOK, 128], BF16)
    ckvT_s = dscr("ckvT", [128, TOK], BF16)
    kidxT_s = dscr("kidxT", [64, TOK], BF16)
    widx_s = dscr("widx", [TOK, 8], F32)
    vb_s = dscr("vB", [TOK, 512], BF16)
    yaT_s = dscr("yaT", [4, 128, TOK], BF16)
    ybT_s = dscr("ybT", [4, 128, TOK], BF16)
    x1_s = dscr("x1", [TOK, D], F32)
    x1T_s = dscr("x1T", [8, 128, TOK], BF16)
    gates_s = dscr("gates", [TOK, NEXP], F32)
    route_s = dscr("route", [TOK, 8], F32)
    xd_s = dscr("xd", [NSLOT, D], BF16)
    yd_h = [dscr("yd0", [NSLOT, 512], F32), dscr("yd1", [NSLOT, 512], F32)]

    sb = {}

    def tb(name, t0, t1):
        return [sb.setdefault((name, t), Buf()) for t in range(t0, t1)]

    psf = [(nc.alloc_psum_tensor("psf%d" % i, [128, 512], F32), Buf()) for i in range(6)]
    psb = [(nc.alloc_psum_tensor("psb%d" % i, [128, 1024], BF16), Buf()) for i in range(2)]
    psi = [0, 0]
    psn = [6]

    def PSF():
        psi[0] += 1
        return psf[psi[0] % psn[0]]

    accA, accB, accC = psf[4], psf[5], psf[3]

    def PSB():
        psi[1] += 1
        return psb[psi[1] % 2]

    SBMAX = 224 * 1024
    C = Arena(nc, 16640, 60 * 1024)
    ident_b = C.t("identb", [128, 128], BF16)
    ident_f = C.t("identf", [128, 128], F32)
    B_const = Buf()
    P.dma("sp", ident_b[:], identb_d, writes=[B_const])
    P.dma("sp", ident_f[:], identf_d, writes=[B_const])
    eps_t = C.t("eps", [128, 1], F32)
    P.op("dve", lambda h: h.memset(eps_t[:], EPS), writes=[B_const])
    A0 = C.lo

    A = Arena(nc, A0, SBMAX)
    win = A.t("win", [128, 8, INW], BF16)
    B_w = Buf()
    for kc in range(8):
        P.dma("pool", win[:, kc, :], win_d[kc * 128:(kc + 1) * 128, :], writes=[B_w])
    kvg = A.t("kvg", [128, 128], F32)
    ikg = A.t("ikg", [128, 64], F32)
    ikb = A.t("ikb", [128, 64], F32)
    P.dma("sp", kvg[:], kvg_d, writes=[B_const])
    P.dma("sp", ikg[:], ikg_d, writes=[B_const])
    P.dma("sp", ikb[:], ikb_d, writes=[B_const])

    xTg_r = Rot(A, "xTg", [128, 8, 512], BF16, 2)
    stg_r = Rot(A, "stg", [128, 512], BF16, 4)
    ckvb_r = Rot(A, "ckvb", [128, 128], BF16, 5)
    ckvTs_r = Rot(A, "ckvTs", [128, 128], BF16, 2)
    knf_r = Rot(A, "knf", [128, 64], F32, 2)
    knb_r = Rot(A, "knb", [128, 64], BF16, 5)
    kTs_r = Rot(A, "kTs", [64, 128], BF16, 2)
    ws_r = Rot(A, "ws", [128, 8], F32, 2)
    vbs_r = Rot(A, "vbs", [128, 512], BF16, 2)
    junk_r = Rot(A, "junk", [128, 128], F32, 2)
    st_r = Rot(A, "st", [128, 16], F32, 4)

    xT_v = xT_d.rearrange("(kc p) t -> p kc t", p=128)
    FM = [0, 128, 256, 384, 640, 768, 896, 1024, 1224, 1352, 1480, 1608, 1736, 1864, 1992, 2120] + \
         [2760 + 128 * i for i in range(16)]

    def phaseA(seq):
        for g in range(seq * 4, seq * 4 + 4):
            t0 = g * 4
            xTg, Bx = xTg_r.next()
            P.dma("pool", xTg[:], xT_v[:, :, g * 512:(g + 1) * 512], writes=[Bx])
            deferred = []
            for tt in range(4):
                t = t0 + tt
                tok = slice(t * 128, (t + 1) * 128)
                psA, BpA = PSF()
                for kc in range(8):
                    P.op("pe", lambda h, ps=psA, kc=kc, tt=tt, xTg=xTg: h.matmul(
                        ps[:, 0:128], lhsT=xTg[:, kc, tt * 128:(tt + 1) * 128], rhs=win[:, kc, 512:640],
                        start=(kc == 0), stop=(kc == 7)), reads=[B_w, Bx], writes=[BpA])
                for kc in range(8):
                    P.op("pe", lambda h, ps=psA, kc=kc, tt=tt, xTg=xTg: h.matmul(
                        ps[:, 128:200], lhsT=xTg[:, kc, tt * 128:(tt + 1) * 128], rhs=win[:, kc, 1152:1224],
                        start=(kc == 0), stop=(kc == 7)), reads=[B_w, Bx], writes=[BpA])
                psV, BpV = PSF()
                for kc in range(8):
                    P.op("pe", lambda h, ps=psV, kc=kc, tt=tt, xTg=xTg: h.matmul(
                        ps[:, :], lhsT=xTg[:, kc, tt * 128:(tt + 1) * 128], rhs=win[:, kc, 2248:2760],
                        start=(kc == 0), stop=(kc == 7)), reads=[B_w, Bx], writes=[BpV])
                vbs, Bv = vbs_r.next()
                P.op("act", lambda h, vbs=vbs, ps=psV: h.copy(out=vbs[:], in_=ps[:, :]), reads=[BpV], writes=[Bv])
                P.dma("sp", vb_s[tok, :], vbs[:], reads=[Bv], writes=tb("vb", t, t + 1))
                st, Bst = st_r.next()
                junk, Bj = junk_r.next()
                P.op("act", lambda h, junk=junk, ps=psA, st=st: h.activation(
                    out=junk[:, 0:128], in_=ps[:, 0:128], func=AF.Square, accum_out=st[:, 0:1]),
                    reads=[BpA], writes=[Bj, Bst])
                P.op("act", lambda h, st=st: h.activation(out=st[:, 1:2], in_=st[:, 0:1], func=AF.Sqrt,
                                                          scale=1.0 / 128.0, bias=eps_t[:, 0:1]),
                     reads=[Bst, B_const], writes=[Bst])
                P.op("dve", lambda h, st=st: h.reciprocal(out=st[:, 2:3], in_=st[:, 1:2]), reads=[Bst], writes=[Bst])
                ckvb, Bc = ckvb_r.next()
                deferred.append((t, tok, ckvb, Bc, None, None))
                P.op("dve", lambda h, ckvb=ckvb, ps=psA, st=st: h.scalar_tensor_tensor(
                    out=ckvb[:], in0=ps[:, 0:128], scalar=st[:, 2:3], in1=kvg[:], op0=ALU.mult, op1=ALU.mult),
                    reads=[BpA, Bst, B_const], writes=[Bc])
                P.dma("sp", ckvtm_s[tok, :], ckvb[:], reads=[Bc], writes=tb("ckvtm", t, t + 1))
                P.op("dve", lambda h, st=st, ps=psA: h.bn_stats(out=st[:, 4:10], in_=ps[:, 128:192]),
                     reads=[BpA], writes=[Bst])
                P.op("dve", lambda h, st=st: h.bn_aggr(out=st[:, 10:12], in_=st[:, 4:10]), reads=[Bst], writes=[Bst])
                P.op("act", lambda h, st=st: h.activation(out=st[:, 12:13], in_=st[:, 11:12], func=AF.Sqrt,
                                                          scale=1.0, bias=eps_t[:, 0:1]),
                     reads=[Bst, B_const], writes=[Bst])
                P.op("dve", lambda h, st=st: h.reciprocal(out=st[:, 13:14], in_=st[:, 12:13]), reads=[Bst], writes=[Bst])
                knf, Bkf = knf_r.next()
                P.op("dve", lambda h, knf=knf, ps=psA, st=st: h.tensor_scalar(
                    out=knf[:], in0=ps[:, 128:192], scalar1=st[:, 10:11], scalar2=st[:, 13:14],
                    op0=ALU.subtract, op1=ALU.mult), reads=[BpA, Bst], writes=[Bkf])
                P.op("dve", lambda h, knf=knf: h.tensor_tensor(out=knf[:], in0=knf[:], in1=ikg[:], op=ALU.mult),
                     reads=[Bkf, B_const], writes=[Bkf])
                knb, Bkb = knb_r.next()
                deferred.append((t, tok, None, None, knb, Bkb))
                P.op("dve", lambda h, knf=knf, knb=knb: h.tensor_tensor(out=knb[:], in0=knf[:], in1=ikb[:], op=ALU.add),
                     reads=[Bkf, B_const], writes=[Bkb])
                ws, Bws = ws_r.next()
                P.op("dve", lambda h, ws=ws, ps=psA: h.tensor_scalar(
                    out=ws[:], in0=ps[:, 192:200], scalar1=IDX_SCALE, scalar2=None, op0=ALU.mult),
                    reads=[BpA], writes=[Bws])
                P.dma("sp", widx_s[tok, :], ws[:], reads=[Bws], writes=tb("widx", t, t + 1))
            for ci, c0 in enumerate(FM):
                ps, Bp = PSF()
                for kc in range(8):
                    P.op("pe", lambda h, ps=ps, kc=kc, c0=c0, xTg=xTg: h.matmul(
                        ps[:, :], lhsT=win[:, kc, c0:c0 + 128], rhs=xTg[:, kc, :], start=(kc == 0), stop=(kc == 7)),
                        reads=[B_w, Bx], writes=[Bp])
                stg, Bs = stg_r.next()
                if ci >= 16:
                    P.op("act", lambda h, stg=stg, ps=ps: h.activation(out=stg[:], in_=ps[:, :], func=AF.Sigmoid),
                         reads=[Bp], writes=[Bs])
                elif ci % 2 == 0:
                    P.op("act", lambda h, stg=stg, ps=ps: h.copy(out=stg[:], in_=ps[:, :]), reads=[Bp], writes=[Bs])
                else:
                    P.op("dve", lambda h, stg=stg, ps=ps: h.tensor_copy(out=stg[:], in_=ps[:, :]), reads=[Bp], writes=[Bs])
                P.dma("sp", featT[ci, :, g * 512:(g + 1) * 512], stg[:], reads=[Bs], writes=tb(("f", ci), t0, t0 + 4))
            for (t, tok, ckvb, Bc, knb, Bkb) in deferred:
                if ckvb is not None:
                    pT, BpT = PSB()
                    P.op("pe", lambda h, pT=pT, ckvb=ckvb: h.transpose(out=pT[:, 0:128], in_=ckvb[:], identity=ident_b[:]),
                         reads=[Bc, B_const], writes=[BpT])
                    cts, Bct = ckvTs_r.next()
                    P.op("act", lambda h, cts=cts, pT=pT: h.copy(out=cts[:], in_=pT[:, 0:128]), reads=[BpT], writes=[Bct])
                    P.dma("sp", ckvT_s[:, tok], cts[:], reads=[Bct], writes=tb("ckvT", t, t + 1))

                else:
                    pT2, BpT2 = PSB()
                    P.op("pe", lambda h, pT2=pT2, knb=knb: h.transpose(out=pT2[0:64, 0:128], in_=knb[:], identity=ident_b[:]),
                         reads=[Bkb, B_const], writes=[BpT2])
                    kts, Bkt = kTs_r.next()
                    P.op("act", lambda h, kts=kts, pT2=pT2: h.copy(out=kts[:], in_=pT2[0:64, 0:128]), reads=[BpT2], writes=[Bkt])
                    P.dma("sp", kidxT_s[:, tok], kts[:], reads=[Bkt], writes=tb("kidxT", t, t + 1))


    for seq in range(nseq):
        phaseA(seq)
    P.barrier()
    if stop_after == "A":
        P.emit()
        return nc

    S2 = Arena(nc, A0, SBMAX)
    psn[0] = 3
    B_c2 = Buf()
    wuk = S2.t("wuk", [128, 4, 128], BF16)
    wuv = S2.t("wuv", [128, 8, 64], BF16)
    negDh = S2.t("negDh", [128, 2048], BF16)
    negDl = S2.t("negDl", [128, 2048], BF16)
    slopeI = S2.t("slopeI", [128, 8, 128], BF16)
    biasB = S2.t("biasB", [128, 8, 640], F32)
    maskB = S2.t("maskB", [128, 640], F32)
    P.dma("pool", wuk[:], wuk_d, writes=[B_c2])
    P.dma("pool", wuv[:], wuv_d.rearrange("h r d -> r h d"), writes=[B_c2])
    P.dma("sp", negDh[:], negdh_d, writes=[B_c2])
    P.dma("sp", negDl[:], negdl_d, writes=[B_c2])
    P.dma("sp", slopeI[:], slopei_d, writes=[B_c2])
    P.dma("sp", maskB[:], maskb_d, writes=[B_c2])
    for h in range(8):
        P.dma("sp", biasB[:, h, :], relb_d[h], writes=[B_c2])
    for h in range(8):
        P.op("dve", lambda hh, h=h: hh.tensor_tensor(out=biasB[:, h, :], in0=biasB[:, h, :], in1=maskB[:], op=ALU.add),
             reads=[B_c2], writes=[B_c2])

    kidx2_r = Rot(S2, "kidx2", [128, 2048], BF16, 1)
    ckvT_r = Rot(S2, "ckvTr", [128, 2048], BF16, 1)
    ckvtm_r = Rot(S2, "ckvtmr", [128, 16, 128], BF16, 1)
    isc2 = [S2.t("isc%d" % i, [128, 2048], F32) for i in range(2)]
    B_isc2 = [[Buf() for _ in range(4)] for _ in range(2)]
    negm2 = [S2.t("negm%d" % i, [128, 2048], BF16) for i in range(2)]
    B_negm2 = [Buf(), Buf()]
    bs_r = Rot(S2, "bs", [128, 8], F32, 2)
    dl_r = Rot(S2, "dl", [128, 24], F32, 2)
    cn_r = Rot(S2, "cn", [128, 24], F32, 2)
    jk_r = Rot(S2, "jk", [128, 2048], BF16, 2)
    pow2 = S2.t("pow2", [128, 24], F32)
    P.dma("sp", pow2[:, 0:21], pow2_d, writes=[B_c2])
    qi_r = Rot(S2, "qi", [128, 4, 128], BF16, 2)
    qa_r = Rot(S2, "qa", [128, 4, 128], BF16, 2)
    wt_r = Rot(S2, "wt", [128, 8], F32, 2)
    rl_r = Rot(S2, "rl", [128, 512], F32, 2)
    m8_r = Rot(S2, "m8", [128, 8], F32, 2)
    qlat_r = Rot(S2, "qlat", [128, 128], BF16, 2)
    sm_r = Rot(S2, "sm", [128, 2048], F32, 2)
    pb_r = Rot(S2, "pb", [128, 2048], BF16, 2)
    pt_r = Rot(S2, "pt", [128, 2048], BF16, 2)
    st2_r = Rot(S2, "st2", [128, 4], F32, 8)
    mx_r = Rot(S2, "mx", [128, 4], F32, 4)
    rc_r = Rot(S2, "rc", [128, 8], F32, 2)
    olat_r = Rot(S2, "olat", [128, 1024], BF16, 1)
    olatT_r = Rot(S2, "olatT", [128, 1024], BF16, 1)
    yas_r = Rot(S2, "yas", [64, 1024], BF16, 2)
    qb_r = Rot(S2, "qb", [128, 4, 128], BF16, 2)
    kb_r = Rot(S2, "kb", [128, 4, 640], BF16, 2)
    vb_r = Rot(S2, "vb", [128, 5, 512], BF16, 2)
    sB_r = Rot(S2, "sB", [128, 640], F32, 2)
    pB_r = Rot(S2, "pB", [128, 640], BF16, 2)
    ptB_r = Rot(S2, "ptB", [128, 640], BF16, 2)
    yb_r = Rot(S2, "yb", [128, 512], BF16, 1)
    ybs_r = Rot(S2, "ybs", [128, 512], BF16, 2)
    SLOPES = [2.0 ** (-(h + 1)) for h in range(8)]
    yaT_v = yaT_s.rearrange("j (hp d) t -> d (j hp) t", hp=2)
    cpy = [0]

    def evac(out, in_, reads, writes):
        cpy[0] += 1
        if cpy[0] % 2:
            P.op("act", lambda h: h.copy(out=out, in_=in_), reads=reads, writes=writes)
        else:
            P.op("dve", lambda h: h.tensor_copy(out=out, in_=in_), reads=reads, writes=writes)

    NBIS = 20

    def indexer(seq, t, kidx2, Bk):
        par = t % 2
        isc, B_isc = isc2[par], B_isc2[par]
        tg = seq * 16 + t
        tok = slice(tg * 128, (tg + 1) * 128)
        S = (t + 1) * 128
        chunks = [(c * 512, min(512, S - c * 512)) for c in range((S + 511) // 512)]
        qi, Bqi = qi_r.next()
        wt, Bwt = wt_r.next()
        P.dma("sp", qi[:], featT[4:8, :, tok].rearrange("c p t -> p c t"), reads=[b for c in range(4, 8) for b in tb(("f", c), tg, tg + 1)], writes=[Bqi])
        P.dma("sp", wt[:], widx_s[tok, :], reads=tb("widx", tg, tg + 1), writes=[Bwt])
        for h in range(8):
            hp, j = h % 2, h // 2
            for ci, (c0, cs) in enumerate(chunks):
                ps, Bp = PSF()
                P.op("pe", lambda hh, ps=ps, cs=cs, c0=c0, hp=hp, j=j, qi=qi: hh.matmul(
                    ps[:, 0:cs], lhsT=qi[hp * 64:(hp + 1) * 64, j, :], rhs=kidx2[hp * 64:(hp + 1) * 64, c0:c0 + cs],
                    start=True, stop=True), reads=[Bqi, Bk], writes=[Bp])
                rl, Brl = rl_r.next()
                P.op("act", lambda hh, rl=rl, ps=ps, cs=cs: hh.activation(out=rl[:, 0:cs], in_=ps[:, 0:cs], func=AF.Relu),
                     reads=[Bp], writes=[Brl])
                if h == 0:
                    P.op("dve", lambda hh, rl=rl, cs=cs, c0=c0, wt=wt: hh.tensor_scalar(
                        out=isc[:, c0:c0 + cs], in0=rl[:, 0:cs], scalar1=wt[:, 0:1], scalar2=None, op0=ALU.mult),
                        reads=[Brl, Bwt], writes=[B_isc[ci]])
                else:
                    P.op("dve", lambda hh, rl=rl, cs=cs, c0=c0, wt=wt, h=h: hh.scalar_tensor_tensor(
                        out=isc[:, c0:c0 + cs], in0=rl[:, 0:cs], scalar=wt[:, h:h + 1], in1=isc[:, c0:c0 + cs],
                        op0=ALU.mult, op1=ALU.add), reads=[Brl, Bwt, B_isc[ci]], writes=[B_isc[ci]])
                yield
        P.op("dve", lambda hh: hh.memset(isc[0:64, t * 128 + 64:(t + 1) * 128], -1e30),
             reads=B_isc[:len(chunks)], writes=B_isc[:len(chunks)])

    def thresh_gen(t):
        par = t % 2
        isc, B_isc, negm, B_negm = isc2[par], B_isc2[par], negm2[par], B_negm2[par]
        S = (t + 1) * 128
        nch = (S + 511) // 512
        Bi = B_isc[:nch]
        if t < 2:
            P.op("dve", lambda hh: hh.tensor_scalar(
                out=negm[:, 0:S], in0=isc[:, 0:S], scalar1=-1e29, scalar2=NEG, op0=ALU.is_lt, op1=ALU.mult),
                reads=Bi, writes=[B_negm])
            return
        bs, Bbs = bs_r.next()
        dl, Bdl = dl_r.next()
        cn, Bcn = cn_r.next()
        P.op("dve", lambda hh: hh.tensor_reduce(out=bs[:, 0:1], in_=isc[:, 0:S], axis=AX.X, op=ALU.max), reads=Bi, writes=[Bbs])
        yield
        P.op("dve", lambda hh: hh.tensor_reduce(out=bs[:, 1:2], in_=isc[:, 0:S - 128], axis=AX.X, op=ALU.min), reads=Bi, writes=[Bbs])
        yield
        P.op("dve", lambda hh: hh.tensor_tensor(out=bs[:, 2:3], in0=bs[:, 0:1], in1=bs[:, 1:2], op=ALU.subtract), reads=[Bbs], writes=[Bbs])
        P.op("dve", lambda hh: hh.tensor_tensor(out=bs[:, 3:4], in0=bs[:, 0:1], in1=bs[:, 1:2], op=ALU.add), reads=[Bbs], writes=[Bbs])
        P.op("dve", lambda hh: hh.tensor_scalar(out=bs[:, 4:5], in0=bs[:, 3:4], scalar1=-0.5, scalar2=None, op0=ALU.mult), reads=[Bbs], writes=[Bbs])
        P.op("dve", lambda hh: hh.tensor_scalar(out=dl[:, :], in0=pow2[:, :], scalar1=bs[:, 2:3], scalar2=None, op0=ALU.mult),
             reads=[Bbs, B_c2], writes=[Bdl])
        yield
        for i in range(NBIS):
            a, b = 4 + (i % 2), 4 + ((i + 1) % 2)
            jk, Bjk = jk_r.next()
            P.op("act", lambda hh, jk=jk, a=a, i=i: hh.activation(
                out=jk[:, 0:S], in_=isc[:, 0:S], func=AF.Sign, bias=bs[:, a:a + 1], scale=1.0, accum_out=cn[:, i:i + 1]),
                reads=Bi + [Bbs], writes=[Bjk, Bcn])
            yield
            P.op("dve", lambda hh, i=i: hh.tensor_scalar(out=bs[:, 6:7], in0=cn[:, i:i + 1], scalar1=511.5 - S, scalar2=-0.5,
                                                         op0=ALU.is_le, op1=ALU.add), reads=[Bcn], writes=[Bbs])
            P.op("dve", lambda hh, i=i, a=a, b=b: hh.scalar_tensor_tensor(
                out=bs[:, b:b + 1], in0=bs[:, 6:7], scalar=dl[:, i:i + 1], in1=bs[:, a:a + 1], op0=ALU.mult, op1=ALU.add),
                reads=[Bbs, Bdl], writes=[Bbs])
            yield
        f = 4 + (NBIS % 2)
        P.op("dve", lambda hh: hh.scalar_tensor_tensor(
            out=bs[:, 7:8], in0=bs[:, f:f + 1], scalar=-1.0, in1=dl[:, NBIS:NBIS + 1], op0=ALU.mult, op1=ALU.subtract),
            reads=[Bbs, Bdl], writes=[Bbs])
        P.op("dve", lambda hh: hh.tensor_scalar(
            out=negm[:, 0:S], in0=isc[:, 0:S], scalar1=bs[:, 7:8], scalar2=NEG, op0=ALU.is_lt, op1=ALU.mult),
            reads=Bi + [Bbs], writes=[B_negm])

    def run_pipelined(head_gen):
        gens = [head_gen(h) for h in range(8)]
        next(gens[0])
        for h in range(8):
            if h + 1 < 8:
                next(gens[h + 1])
            for _ in gens[h]:
                pass

    class BG:
        def __init__(self, makers):
            self.makers = makers
            self.cur = 0
            self.gen = None
            self.limit = -1

        def _one(self):
            if self.cur >= len(self.makers):
                return False
            if self.gen is None:
                self.gen = self.makers[self.cur]()
            try:
                next(self.gen)
            except StopIteration:
                self.gen = None
                self.cur += 1
            return True

        def step(self):
            if self.cur <= self.limit:
                self._one()

        def force(self, item):
            while self.cur <= item and self.cur < len(self.makers):
                self._one()

    bgref = [None]

    bgB = [None]

    def sp(n=1):
        if bgref[0] is not None:
            for _ in range(n):
                bgref[0].step()
        if bgB[0] is not None:
            try:
                next(bgB[0])
            except StopIteration:
                bgB[0] = None

    def drive_stage(g):
        while True:
            try:
                v = next(g)
            except StopIteration:
                return
            if v == 'S':
                return
            yield

    def run_pipelined_gen(head_gen):
        gens = [head_gen(h) for h in range(8)]
        yield from drive_stage(gens[0])
        for h in range(8):
            if h + 1 < 8:
                yield from drive_stage(gens[h + 1])
            yield from drive_stage(gens[h])

    def advance(gen, n):
        if gen is None:
            return
        for _ in range(n):
            try:
                next(gen)
            except StopIteration:
                return

    def attention(seq, t, ckvT, BcT, ckvtm, Bcm, gnext):
        par = t % 2
        negm, B_negm = negm2[par], B_negm2[par]
        tg = seq * 16 + t
        tok = slice(tg * 128, (tg + 1) * 128)
        S = (t + 1) * 128
        nb = t + 1
        chunks = [(c * 512, min(512, S - c * 512)) for c in range((S + 511) // 512)]
        qa, Bqa = qa_r.next()
        P.dma("sp", qa[:], featT[0:4, :, tok].rearrange("c p t -> p c t"), reads=[b for c in range(0, 4) for b in tb(("f", c), tg, tg + 1)], writes=[Bqa])
        olat, Bol = olat_r.next()
        rc, Brc = rc_r.next()
        off = 1920 - t * 128
        Bacc = [accA[1], accB[1]]
        def head_gen(h):
                sp()
                hp, j = h % 2, h // 2
                psq, Bpq = PSF()
                P.op("pe", lambda hh, psq=psq, hp=hp, j=j, qa=qa: hh.matmul(
                    psq[:, 0:128], lhsT=wuk[hp * 64:(hp + 1) * 64, j, :], rhs=qa[hp * 64:(hp + 1) * 64, j, :],
                    start=True, stop=True), reads=[Bqa, B_c2], writes=[Bpq])
                ql, Bql = qlat_r.next()
                P.op("act", lambda hh, ql=ql, psq=psq: hh.mul(out=ql[:], in_=psq[:, 0:128], mul=0.125),
                     reads=[Bpq], writes=[Bql])
                sp()
                sm, Bsm = sm_r.next()
                mx, Bmx = mx_r.next()
                for ci, (c0, cs) in enumerate(chunks):
                    ps, Bp = PSF()
                    P.op("pe", lambda hh, ps=ps, cs=cs, c0=c0, ql=ql: hh.matmul(
                        ps[:, 0:cs], lhsT=ql[:], rhs=ckvT[:, c0:c0 + cs], start=True, stop=False),
                        reads=[Bql, BcT], writes=[Bp])
                    P.op("pe", lambda hh, ps=ps, cs=cs, c0=c0: hh.matmul(
                        ps[:, 0:cs], lhsT=ident_b[:], rhs=negm[:, c0:c0 + cs], start=False, stop=False),
                        reads=[B_negm, B_const], writes=[Bp])
                    P.op("pe", lambda hh, ps=ps, cs=cs, c0=c0: hh.matmul(
                        ps[:, 0:cs], lhsT=slopeI[:, h, :], rhs=negDh[:, off + c0:off + c0 + cs], start=False, stop=False),
                        reads=[B_c2], writes=[Bp])
                    P.op("pe", lambda hh, ps=ps, cs=cs, c0=c0: hh.matmul(
                        ps[:, 0:cs], lhsT=slopeI[:, h, :], rhs=negDl[:, off + c0:off + c0 + cs], start=False, stop=True),
                        reads=[B_c2], writes=[Bp])
                    P.op("dve", lambda hh, ps=ps, cs=cs, c0=c0, sm=sm, mx=mx, ci=ci: hh.tensor_scalar(
                        out=sm[:, c0:c0 + cs], in0=ps[:, 0:cs], scalar1=1.0, scalar2=-3.0e38, op0=ALU.mult, op1=ALU.max,
                        accum_out=mx[:, ci:ci + 1]), reads=[Bp], writes=[Bsm, Bmx])
                    sp()
                st, Bst = st2_r.next()
                P.op("dve", lambda hh, mx=mx, st=st: hh.tensor_reduce(out=st[:, 0:1], in_=mx[:, 0:len(chunks)], axis=AX.X, op=ALU.max, negate=True),
                     reads=[Bmx], writes=[Bst])
                sp()
                pb, Bpb = pb_r.next()
                P.op("act", lambda hh, sm=sm, st=st, pb=pb: hh.activation(
                    out=pb[:, 0:S], in_=sm[:, 0:S], func=AF.Exp, bias=st[:, 0:1], scale=1.0, accum_out=st[:, 1:2]),
                    reads=[Bsm, Bst], writes=[Bpb, Bst])
                P.op("dve", lambda hh, st=st, rc=rc, h=h: hh.reciprocal(out=rc[:, h:h + 1], in_=st[:, 1:2]), reads=[Bst], writes=[Brc])
                sp()
                yield
                sp()
                pt, Bpt = pt_r.next()
                for b0 in range(0, nb, 8):
                    nbb = min(8, nb - b0)
                    pT, BpT = PSB()
                    for b in range(nbb):
                        P.op("pe", lambda hh, pT=pT, b=b, b0=b0, pb=pb: hh.transpose(
                            out=pT[:, b * 128:(b + 1) * 128], in_=pb[:, (b0 + b) * 128:(b0 + b + 1) * 128], identity=ident_b[:]),
                            reads=[Bpb, B_const], writes=[BpT])
                    evac(pt[:, b0 * 128:(b0 + nbb) * 128], pT[:, 0:nbb * 128], [BpT], [Bpt])
                    sp()
                acc = (accA if h < 4 else accB)[0]
                for b in range(nb):
                    P.op("pe", lambda hh, acc=acc, b=b, h=h, pt=pt: hh.matmul(
                        acc[:, (h % 4) * 128:(h % 4 + 1) * 128], lhsT=pt[:, b * 128:(b + 1) * 128], rhs=ckvtm[:, b, :],
                        start=(b == 0), stop=(b == nb - 1)), reads=[Bpt, Bcm], writes=[Bacc[h // 4]])
        run_pipelined(head_gen)
        for h in range(8):
            acc = (accA if h < 4 else accB)[0]
            P.op("dve", lambda hh, acc=acc, h=h, rc=rc, olat=olat: hh.tensor_scalar(
                out=olat[:, h * 128:(h + 1) * 128], in0=acc[:, (h % 4) * 128:(h % 4 + 1) * 128], scalar1=rc[:, h:h + 1],
                scalar2=None, op0=ALU.mult), reads=[Bacc[h // 4], Brc], writes=[Bol])
        olT, BolT = olatT_r.next()
        pT, BpT = PSB()
        for h in range(8):
            P.op("pe", lambda hh, pT=pT, h=h, olat=olat: hh.transpose(
                out=pT[:, h * 128:(h + 1) * 128], in_=olat[:, h * 128:(h + 1) * 128], identity=ident_b[:]),
                reads=[Bol, B_const], writes=[BpT])
        evac(olT[:, :], pT[:, :], [BpT], [BolT])
        yas, Bya = yas_r.next()
        for half in range(2):
            ps, Bp = PSF()
            for hh_ in range(4):
                h = half * 4 + hh_
                P.op("pe", lambda hh, ps=ps, h=h, hh_=hh_, olT=olT: hh.matmul(
                    ps[0:64, hh_ * 128:(hh_ + 1) * 128], lhsT=wuv[:, h, :], rhs=olT[:, h * 128:(h + 1) * 128],
                    start=True, stop=True), reads=[BolT, B_c2], writes=[Bp])
            evac(yas[:, half * 512:(half + 1) * 512], ps[0:64, :], [Bp], [Bya])
        P.dma("pool", yaT_v[:, :, tok], yas[:].rearrange("p (c t) -> p c t", c=8), reads=[Bya], writes=tb("yaT", tg, tg + 1))

    def mixerB(seq, t):
        tg = seq * 16 + t
        tok = slice(tg * 128, (tg + 1) * 128)
        nk = min(640, (t + 1) * 128)
        c0 = 640 - nk
        ks = seq * SEQ + (t + 1) * 128 - nk
        nbk = nk // 128
        kt0 = ks // 128
        qb, Bqb = qb_r.next()
        kb, Bkb = kb_r.next()
        vb, Bvb = vb_r.next()
        P.dma("sp", qb[:], featT[8:12, :, tok].rearrange("c p t -> p c t"),
              reads=[b for c in range(8, 12) for b in tb(("f", c), tg, tg + 1)], writes=[Bqb])
        P.dma("sp", kb[:, :, 0:nk], featT[12:16, :, ks:ks + nk].rearrange("c p t -> p c t"),
              reads=[b for c in range(12, 16) for b in tb(("f", c), kt0, kt0 + nbk)], writes=[Bkb])
        P.dma("sp", vb[:, 0:nbk, :], vb_s[ks:ks + nk, :].rearrange("(b p) f -> p b f", p=128),
              reads=tb("vb", kt0, kt0 + nbk), writes=[Bvb])
        rc, Brc = rc_r.next()
        n1 = min(nk, 512)
        Bacc = accC[1]
        yield
        def head_gen(h):
                hp, j = h % 2, h // 2
                sB, BsB = sB_r.next()
                parts = [(0, n1)] + ([(512, 128)] if nk > 512 else [])
                for (k0, kn) in parts:
                    ps, Bp = PSF()
                    P.op("pe", lambda hh, ps=ps, k0=k0, kn=kn, hp=hp, j=j, qb=qb, kb=kb: hh.matmul(
                        ps[:, 0:kn], lhsT=qb[hp * 64:(hp + 1) * 64, j, :], rhs=kb[hp * 64:(hp + 1) * 64, j, k0:k0 + kn],
                        start=True, stop=True), reads=[Bqb, Bkb], writes=[Bp])
                    P.op("dve", lambda hh, ps=ps, k0=k0, kn=kn, sB=sB, h=h: hh.scalar_tensor_tensor(
                        out=sB[:, k0:k0 + kn], in0=ps[:, 0:kn], scalar=0.125, in1=biasB[:, h, c0 + k0:c0 + k0 + kn],
                        op0=ALU.mult, op1=ALU.add), reads=[Bp, B_c2], writes=[BsB])
                yield
                st, Bst = st2_r.next()
                P.op("dve", lambda hh, sB=sB, st=st: hh.tensor_reduce(out=st[:, 0:1], in_=sB[:, 0:nk], axis=AX.X, op=ALU.max, negate=True),
                     reads=[BsB], writes=[Bst])
                pB, BpB = pB_r.next()
                P.op("act", lambda hh, sB=sB, st=st, pB=pB: hh.activation(
                    out=pB[:, 0:nk], in_=sB[:, 0:nk], func=AF.Exp, bias=st[:, 0:1], scale=1.0, accum_out=st[:, 1:2]),
                    reads=[BsB, Bst], writes=[BpB, Bst])
                yield
                P.op("dve", lambda hh, st=st, rc=rc, h=h: hh.reciprocal(out=rc[:, h:h + 1], in_=st[:, 1:2]), reads=[Bst], writes=[Brc])
                yield
                yield 'S'
                ptB, BptB = ptB_r.next()
                pT, BpT = PSB()
                for b in range(nbk):
                    P.op("pe", lambda hh, pT=pT, b=b, pB=pB: hh.transpose(
                        out=pT[:, b * 128:(b + 1) * 128], in_=pB[:, b * 128:(b + 1) * 128], identity=ident_b[:]),
                        reads=[BpB, B_const], writes=[BpT])
                evac(ptB[:, 0:nbk * 128], pT[:, 0:nbk * 128], [BpT], [BptB])
                yield
                for b in range(nbk):
                    P.op("pe", lambda hh, b=b, h=h, ptB=ptB, vb=vb: hh.matmul(
                        accC[0][:, h * 64:(h + 1) * 64], lhsT=ptB[:, b * 128:(b + 1) * 128], rhs=vb[:, b, h * 64:(h + 1) * 64],
                        start=(b == 0), stop=(b == nbk - 1)), reads=[BptB, Bvb], writes=[Bacc])
        yield from run_pipelined_gen(head_gen)
        yb, Byb = yb_r.next()
        for h in range(8):
            P.op("dve", lambda hh, h=h, rc=rc, yb=yb: hh.tensor_scalar(
                out=yb[:, h * 64:(h + 1) * 64], in0=accC[0][:, h * 64:(h + 1) * 64], scalar1=rc[:, h:h + 1],
                scalar2=None, op0=ALU.mult), reads=[Bacc, Brc], writes=[Byb])
        yield
        pT, BpT = PSB()
        for b in range(4):
            P.op("pe", lambda hh, pT=pT, b=b, yb=yb: hh.transpose(
                out=pT[:, b * 128:(b + 1) * 128], in_=yb[:, b * 128:(b + 1) * 128], identity=ident_b[:]),
                reads=[Byb, B_const], writes=[BpT])
        ybs, Bybs = ybs_r.next()
        evac(ybs[:, :], pT[:, 0:512], [BpT], [Bybs])
        P.dma("pool", ybT_s[:, :, tok].rearrange("c p t -> p c t"), ybs[:].rearrange("p (c t) -> p c t", c=4), reads=[Bybs], writes=tb("ybT", tg, tg + 1))

    for seq in range(nseq):
        kidx2, Bk = kidx2_r.next()
        ckvT, BcT = ckvT_r.next()
        ckvtm, Bcm = ckvtm_r.next()
        sl = slice(seq * SEQ, (seq + 1) * SEQ)
        P.dma("sp", kidx2[0:64, :], kidxT_s[:, sl], reads=tb("kidxT", seq * 16, seq * 16 + 16), writes=[Bk])
        P.dma("sp", kidx2[64:128, :], kidxT_s[:, sl], reads=tb("kidxT", seq * 16, seq * 16 + 16), writes=[Bk])
        P.dma("sp", ckvT[:], ckvT_s[:, sl], reads=tb("ckvT", seq * 16, seq * 16 + 16), writes=[BcT])
        P.dma("sp", ckvtm[:], ckvtm_s[sl, :].rearrange("(b p) r -> p b r", p=128),
              reads=tb("ckvtm", seq * 16, seq * 16 + 16), writes=[Bcm])
        if "A" in mixers:
            makers = []
            for t in range(16):
                makers.append(lambda t=t: indexer(seq, t, kidx2, Bk))
                makers.append(lambda t=t: thresh_gen(t))
            bg = BG(makers)
            bgref[0] = bg
        for t in range(16):
            if "A" in mixers:
                bg.force(2 * t + 1)
                bg.limit = 2 * (t + 2)
                if "B" in mixers:
                    bgB[0] = mixerB(seq, t)
                attention(seq, t, ckvT, BcT, ckvtm, Bcm, None)
                while bgB[0] is not None:
                    sp(0)
            elif "B" in mixers:
                for _ in mixerB(seq, t):
                    pass
        bgref[0] = None
    P.barrier()
    if stop_after == "CD":
        P.emit()
        return nc

    S3 = Arena(nc, A0, SBMAX)
    psn[0] = 6
    B_c3 = Buf()
    wa = S3.t("wa", [128, 4, D], BF16)
    wb = S3.t("wb", [128, 4, D], BF16)
    wo = S3.t("wo", [128, 8, D], BF16)
    ln1g = S3.t("ln1g", [128, D], F32)
    ln1b = S3.t("ln1b", [128, D], F32)
    wr = S3.t("wr", [128, 8, NEXP], BF16)
    brt = S3.t("brt", [128, NEXP], F32)
    P.dma("pool", wa[:], wa_d.rearrange("(k p) n -> p k n", p=128), writes=[B_c3])
    P.dma("pool", wb[:], wb_d.rearrange("(k p) n -> p k n", p=128), writes=[B_c3])
    P.dma("pool", wo[:], wo_d.rearrange("(k p) n -> p k n", p=128), writes=[B_c3])
    P.dma("sp", ln1g[:], ln1g_d, writes=[B_c3])
    P.dma("sp", ln1b[:], ln1b_d, writes=[B_c3])
    P.dma("pool", wr[:], wr_d.rearrange("(k p) n -> p k n", p=128), writes=[B_c3])
    P.dma("sp", brt[:], br_d, writes=[B_c3])
    yag_r = Rot(S3, "yag", [128, 4, 512], BF16, 2)
    ybg_r = Rot(S3, "ybg", [128, 4, 512], BF16, 2)
    sga_r = Rot(S3, "sga", [128, 8, 512], BF16, 2)
    sgb_r = Rot(S3, "sgb", [128, 8, 512], BF16, 2)
    t1_r = Rot(S3, "t1", [128, 512], F32, 2)
    t2_r = Rot(S3, "t2", [128, 512], F32, 2)
    mT_r = Rot(S3, "mT", [128, 8, 512], BF16, 2)
    xr_r = Rot(S3, "xr", [128, D], F32, 4)
    z_r = Rot(S3, "z", [128, D], F32, 4)
    x1t_r = Rot(S3, "x1t", [128, D], F32, 4)
    x1b_r = Rot(S3, "x1b", [128, 1024], BF16, 4)
    x1Tb_r = Rot(S3, "x1Tb", [128, 1024], BF16, 4)
    st3_r = Rot(S3, "st3", [128, 16], F32, 8)
    lg_r = Rot(S3, "lg", [128, NEXP], F32, 4)
    ex_r = Rot(S3, "ex", [128, NEXP], F32, 4)
    gs_r = Rot(S3, "gs", [128, NEXP], F32, 4)
    m8b_r = Rot(S3, "m8b", [128, 8], F32, 8)
    selb_r = Rot(S3, "selb", [128, NEXP], BF16, 4)
    rw_r = Rot(S3, "rw", [128, 4 * NEXP], F32, 4)
    sf_r = Rot(S3, "sf", [128, 12], F32, 5)
    su_r = Rot(S3, "su", [128, 1], U32, 16)
    ustr = S3.t("ustr", [128, 128], BF16)
    onesb = S3.t("onesb", [128, 128], BF16)
    ebase = S3.t("ebase", [128, NEXP], F32)
    P.dma("sp", ustr[:], ustr_d, writes=[B_c3])
    P.dma("sp", onesb[:], onesb_d, writes=[B_c3])
    P.dma("sp", ebase[:], ebase_d, writes=[B_c3])
    rt_tiles = [(S3.t("rt0", [128, NEXP], F32), Buf()), (S3.t("rt1", [128, NEXP], F32), Buf())]
    P.op("dve", lambda h: h.memset(rt_tiles[0][0][:], 0.0), writes=[rt_tiles[0][1]])

    def layer_norm_gen(z, Bz, g_t, b_t, Bg, out, Bout, st_rot):
        st, Bst = st_rot.next()
        yield
        P.op("dve", lambda h: h.bn_stats(out=st[:, 0:6], in_=z[:, 0:512]), reads=[Bz], writes=[Bst])
        yield
        P.op("dve", lambda h: h.bn_stats(out=st[:, 6:12], in_=z[:, 512:1024]), reads=[Bz], writes=[Bst])
        yield
        P.op("dve", lambda h: h.bn_aggr(out=st[:, 12:14], in_=st[:, 0:12]), reads=[Bst], writes=[Bst])
        yield
        P.op("act", lambda h: h.activation(out=st[:, 14:15], in_=st[:, 13:14], func=AF.Sqrt, scale=1.0, bias=eps_t[:, 0:1]),
             reads=[Bst, B_const], writes=[Bst])
        yield
        P.op("dve", lambda h: h.reciprocal(out=st[:, 15:16], in_=st[:, 14:15]), reads=[Bst], writes=[Bst])
        yield
        P.op("dve", lambda h: h.tensor_scalar(out=z[:], in0=z[:], scalar1=st[:, 12:13], scalar2=st[:, 15:16],
                                              op0=ALU.subtract, op1=ALU.mult), reads=[Bz, Bst], writes=[Bz])
        yield
        P.op("dve", lambda h: h.tensor_tensor(out=z[:], in0=z[:], in1=g_t[:], op=ALU.mult), reads=[Bz, Bg], writes=[Bz])
        yield
        P.op("dve", lambda h: h.tensor_tensor(out=out[:], in0=z[:], in1=b_t[:], op=ALU.add), reads=[Bz, Bg], writes=[Bout])

    def layer_norm(*a):
        for _ in layer_norm_gen(*a):
            pass

    for g in range(TOK // 512):
        gs_ = slice(g * 512, (g + 1) * 512)
        t0 = g * 4
        yag, Byag = yag_r.next()
        ybg, Bybg = ybg_r.next()
        sga, Bsga = sga_r.next()
        sgb, Bsgb = sgb_r.next()
        P.dma("sp", yag[:], yaT_s[:, :, gs_].rearrange("c p t -> p c t"), reads=tb("yaT", t0, t0 + 4), writes=[Byag])
        P.dma("sp", ybg[:], ybT_s[:, :, gs_].rearrange("c p t -> p c t"), reads=tb("ybT", t0, t0 + 4), writes=[Bybg])
        P.dma("sp", sga[:], featT[16:24, :, gs_].rearrange("c p t -> p c t"),
              reads=[b for c in range(16, 24) for b in tb(("f", c), t0, t0 + 4)], writes=[Bsga])
        P.dma("sp", sgb[:], featT[24:32, :, gs_].rearrange("c p t -> p c t"),
              reads=[b for c in range(24, 32) for b in tb(("f", c), t0, t0 + 4)], writes=[Bsgb])
        mT, BmT = mT_r.next()
        for n in range(8):
            pa, Bpa = PSF()
            for k in range(4):
                P.op("pe", lambda h, pa=pa, k=k, n=n, yag=yag: h.matmul(
                    pa[:, :], lhsT=wa[:, k, n * 128:(n + 1) * 128], rhs=yag[:, k, :], start=(k == 0), stop=(k == 3)),
                    reads=[B_c3, Byag], writes=[Bpa])
            pb_, Bpb_ = PSF()
            for k in range(4):
                P.op("pe", lambda h, pb_=pb_, k=k, n=n, ybg=ybg: h.matmul(
                    pb_[:, :], lhsT=wb[:, k, n * 128:(n + 1) * 128], rhs=ybg[:, k, :], start=(k == 0), stop=(k == 3)),
                    reads=[B_c3, Bybg], writes=[Bpb_])
            t1, Bt1 = t1_r.next()
            t2, Bt2 = t2_r.next()
            P.op("dve", lambda h, t1=t1, pa=pa, n=n, sga=sga: h.tensor_tensor(out=t1[:], in0=pa[:, :], in1=sga[:, n, :], op=ALU.mult),
                 reads=[Bpa, Bsga], writes=[Bt1])
            P.op("dve", lambda h, t2=t2, pb_=pb_, n=n, sgb=sgb: h.tensor_tensor(out=t2[:], in0=pb_[:, :], in1=sgb[:, n, :], op=ALU.mult),
                 reads=[Bpb_, Bsgb], writes=[Bt2])
            P.op("dve", lambda h, t1=t1, t2=t2, n=n, mT=mT: h.tensor_tensor(out=mT[:, n, :], in0=t1[:], in1=t2[:], op=ALU.add),
                 reads=[Bt1, Bt2], writes=[BmT])
        def tile_gen(tt):
                t = t0 + tt
                tok = slice(t * 128, (t + 1) * 128)
                yield
                xr, Bxr = xr_r.next()
                yield
                P.dma("sp", xr[:], x_d[tok, :], writes=[Bxr])
                yield
                z, Bz = z_r.next()
                yield
                for nh in range(2):
                    po, Bpo = PSF()
                    for k in range(8):
                        P.op("pe", lambda h, po=po, k=k, nh=nh, tt=tt, mT=mT: h.matmul(
                            po[:, :], lhsT=mT[:, k, tt * 128:(tt + 1) * 128], rhs=wo[:, k, nh * 512:(nh + 1) * 512],
                            start=(k == 0), stop=(k == 7)), reads=[BmT, B_c3], writes=[Bpo])
                    P.op("dve", lambda h, po=po, nh=nh, xr=xr, z=z: h.scalar_tensor_tensor(
                        out=z[:, nh * 512:(nh + 1) * 512], in0=xr[:, nh * 512:(nh + 1) * 512], scalar=ALPHA, in1=po[:, :],
                        op0=ALU.mult, op1=ALU.add), reads=[Bpo, Bxr], writes=[Bz])
                yield
                x1t, Bx1 = x1t_r.next()
                yield
                yield from layer_norm_gen(z, Bz, ln1g, ln1b, B_c3, x1t, Bx1, st3_r)
                yield
                P.dma("pool", x1_s[tok, :], x1t[:], reads=[Bx1], writes=tb("x1", t, t + 1))
                yield
                x1b, Bx1b = x1b_r.next()
                P.op("act", lambda h, x1b=x1b, x1t=x1t: h.copy(out=x1b[:], in_=x1t[:]), reads=[Bx1], writes=[Bx1b])
                yield
                x1Tb, Bxb = x1Tb_r.next()
                yield
                pt_, Bpt_ = PSB()
                for b in range(8):
                    P.op("pe", lambda h, pt_=pt_, b=b, x1b=x1b: h.transpose(
                        out=pt_[:, b * 128:(b + 1) * 128], in_=x1b[:, b * 128:(b + 1) * 128], identity=ident_b[:]),
                        reads=[Bx1b, B_const], writes=[Bpt_])
                evac(x1Tb[:, :], pt_[:, :], [Bpt_], [Bxb])
                yield
                P.dma("pool", x1T_s[:, :, tok].rearrange("c p t -> p c t"), x1Tb[:].rearrange("p (c t) -> p c t", c=8), reads=[Bxb], writes=tb("x1T", t, t + 1))
                yield
                pr, Bpr = PSF()
                for k in range(8):
                    P.op("pe", lambda h, pr=pr, k=k, x1Tb=x1Tb: h.matmul(
                        pr[:, 0:NEXP], lhsT=x1Tb[:, k * 128:(k + 1) * 128], rhs=wr[:, k, :], start=(k == 0), stop=(k == 7)),
                        reads=[Bxb, B_c3], writes=[Bpr])
                yield
                lg, Blg = lg_r.next()
                P.op("dve", lambda h, lg=lg, pr=pr: h.tensor_tensor(out=lg[:], in0=pr[:, 0:NEXP], in1=brt[:], op=ALU.add),
                     reads=[Bpr, B_c3], writes=[Blg])
                yield
                m8, Bm8 = m8b_r.next()
                P.op("dve", lambda h, lg=lg, m8=m8: h.max(out=m8[:, 0:8], in_=lg[:]), reads=[Blg], writes=[Bm8])
                yield
                st, Bst = st3_r.next()
                P.op("dve", lambda h, m8=m8, st=st: h.tensor_scalar(out=st[:, 0:1], in0=m8[:, 0:1], scalar1=-1.0, scalar2=None, op0=ALU.mult),
                     reads=[Bm8], writes=[Bst])
                yield
                ex, Bex = ex_r.next()
                P.op("act", lambda h, ex=ex, lg=lg, st=st: h.activation(out=ex[:], in_=lg[:], func=AF.Exp, bias=st[:, 0:1], scale=1.0),
                     reads=[Blg, Bst], writes=[Bex])
                yield
                gs, Bgs = gs_r.next()
                P.op("dve", lambda h, gs=gs, lg=lg, m8=m8, ex=ex: h.scalar_tensor_tensor(
                    out=gs[:], in0=lg[:], scalar=m8[:, 3:4], in1=ex[:], op0=ALU.is_ge, op1=ALU.mult),
                    reads=[Blg, Bm8, Bex], writes=[Bgs])
                P.op("dve", lambda h, gs=gs, st=st: h.tensor_reduce(out=st[:, 1:2], in_=gs[:], axis=AX.X, op=ALU.add),
                     reads=[Bgs], writes=[Bst])
                P.op("dve", lambda h, st=st: h.reciprocal(out=st[:, 2:3], in_=st[:, 1:2]), reads=[Bst], writes=[Bst])
                P.op("dve", lambda h, gs=gs, st=st: h.tensor_scalar(out=gs[:], in0=gs[:], scalar1=st[:, 2:3], scalar2=None, op0=ALU.mult),
                     reads=[Bgs, Bst], writes=[Bgs])
                yield
                selb, Bsel = selb_r.next()
                P.op("dve", lambda h, selb=selb, gs=gs: h.tensor_scalar(out=selb[:], in0=gs[:], scalar1=0.0, scalar2=None, op0=ALU.is_gt),
                     reads=[Bgs], writes=[Bsel])
                yield
                pp, Bpp = PSF()
                P.op("pe", lambda h, pp=pp, selb=selb: h.matmul(pp[:, 0:NEXP], lhsT=ustr[:], rhs=selb[:], start=True, stop=True),
                     reads=[Bsel, B_c3], writes=[Bpp])
                P.op("pe", lambda h, pp=pp, selb=selb: h.matmul(pp[:, NEXP:2 * NEXP], lhsT=onesb[:], rhs=selb[:], start=True, stop=True),
                     reads=[Bsel, B_c3], writes=[Bpp])
                yield
                rt_old, Brt_old = rt_tiles[t % 2]
                yield
                rt_new, Brt_new = rt_tiles[(t + 1) % 2]
                yield
                rw, Brw = rw_r.next()
                P.op("dve", lambda h, rw=rw, pp=pp, rt_old=rt_old: h.tensor_tensor(out=rw[:, 0:NEXP], in0=pp[:, 0:NEXP], in1=rt_old[:], op=ALU.add),
                     reads=[Bpp, Brt_old], writes=[Brw])
                P.op("dve", lambda h, pp=pp, rt_old=rt_old, rt_new=rt_new: h.tensor_tensor(out=rt_new[:], in0=pp[:, NEXP:2 * NEXP], in1=rt_old[:], op=ALU.add),
                     reads=[Bpp, Brt_old], writes=[Brt_new])
                P.op("dve", lambda h, rw=rw: h.tensor_scalar(out=rw[:, 2 * NEXP:3 * NEXP], in0=rw[:, 0:NEXP], scalar1=float(CAP), scalar2=100000.0,
                                                             op0=ALU.is_ge, op1=ALU.mult), reads=[Brw], writes=[Brw])
                P.op("dve", lambda h, rw=rw: h.tensor_tensor(out=rw[:, NEXP:2 * NEXP], in0=rw[:, 0:NEXP], in1=ebase[:], op=ALU.add),
                     reads=[Brw, B_c3], writes=[Brw])
                P.op("dve", lambda h, rw=rw: h.tensor_tensor(out=rw[:, NEXP:2 * NEXP], in0=rw[:, NEXP:2 * NEXP], in1=rw[:, 2 * NEXP:3 * NEXP], op=ALU.add),
                     reads=[Brw], writes=[Brw])
                P.op("dve", lambda h, rw=rw: h.tensor_scalar(out=rw[:, 2 * NEXP:3 * NEXP], in0=rw[:, NEXP:2 * NEXP], scalar1=-1.0, scalar2=BIGM,
                                                             op0=ALU.mult, op1=ALU.add), reads=[Brw], writes=[Brw])
                P.op("dve", lambda h, rw=rw, selb=selb: h.tensor_tensor(out=rw[:, 3 * NEXP:4 * NEXP], in0=rw[:, 2 * NEXP:3 * NEXP], in1=selb[:], op=ALU.mult),
                     reads=[Brw, Bsel], writes=[Brw])
                yield
                k8, Bk8 = m8b_r.next()
                P.op("dve", lambda h, rw=rw, k8=k8: h.max(out=k8[:, 0:8], in_=rw[:, 3 * NEXP:4 * NEXP]), reads=[Brw], writes=[Bk8])
                yield
                sf, Bsf = sf_r.next()
                P.op("dve", lambda h, sf=sf, k8=k8: h.tensor_scalar(out=sf[:, 0:4], in0=k8[:, 0:4], scalar1=-1.0, scalar2=BIGM, op0=ALU.mult, op1=ALU.add),
                     reads=[Bk8], writes=[Bsf])
                yield
                for k in range(4):
                    P.op("dve", lambda h, rw=rw, sf=sf, gs=gs, k=k: h.scalar_tensor_tensor(
                        out=rw[:, 2 * NEXP:3 * NEXP], in0=rw[:, NEXP:2 * NEXP], scalar=sf[:, k:k + 1], in1=gs[:],
                        op0=ALU.is_equal, op1=ALU.mult, accum_out=sf[:, 4 + k:5 + k]), reads=[Brw, Bsf, Bgs], writes=[Brw, Bsf])
                P.op("dve", lambda h, sf=sf: h.tensor_scalar(out=sf[:, 8:12], in0=sf[:, 0:4], scalar1=float(NSLOT), scalar2=None, op0=ALU.is_lt),
                     reads=[Bsf], writes=[Bsf])
                P.op("dve", lambda h, sf=sf: h.tensor_tensor(out=sf[:, 4:8], in0=sf[:, 4:8], in1=sf[:, 8:12], op=ALU.mult),
                     reads=[Bsf], writes=[Bsf])
                yield
                P.dma("pool", route_s[tok, :], sf[:, 0:8], reads=[Bsf], writes=tb("route", t, t + 1))
                yield
                for k in range(4):
                    su, Bsu = su_r.next()
                    P.op("dve", lambda h, su=su, sf=sf, k=k: h.tensor_copy(out=su[:], in_=sf[:, k:k + 1]), reads=[Bsf], writes=[Bsu])
                    P.idma(xd_s, bass.IndirectOffsetOnAxis(ap=su[:, 0:1], axis=0), x1b[:], None, reads=[Bx1b, Bsu], writes=[], bc=NSLOT - 1)

        gens_ = [tile_gen(tt) for tt in range(4)]
        live_ = list(gens_)
        while live_:
            for g_ in list(live_):
                try:
                    next(g_)
                except StopIteration:
                    live_.remove(g_)
    P.barrier()
    if stop_after == "E":
        P.emit()
        return nc

    S4 = Arena(nc, A0, SBMAX)
    psn[0] = 6
    B_c4 = Buf()
    bgt = S4.t("bgt", [128, NEXP, 8], F32)
    but = S4.t("but", [128, NEXP, 8], F32)
    ones1 = S4.t("ones1", [1, 128], BF16)
    P.op("dve", lambda h: h.memset(ones1[:], 1.0), writes=[B_c4])
    P.dma("sp", bgt[:], bg_d, writes=[B_c4])
    P.dma("sp", but[:], bu_d, writes=[B_c4])
    P.op("dve", lambda h: h.tensor_scalar(out=but[:], in0=but[:], scalar1=1.0, scalar2=None, op0=ALU.add), reads=[B_c4], writes=[B_c4])
    wg_r = Rot(S4, "wg", [128, 8, DFF], BF16, 2)
    wu_r = Rot(S4, "wu", [128, 8, DFF], BF16, 2)
    wd_r = Rot(S4, "wd", [128, 8, D], BF16, 2)
    bde_r = Rot(S4, "bde", [1, D], BF16, 2)
    xrow_r = Rot(S4, "xrow", [128, D], BF16, 6)
    xT_r = Rot(S4, "xTe", [128, 8, 512], BF16, 2)
    hT_r = Rot(S4, "hT", [128, 8, 512], BF16, 2)
    hg_r = Rot(S4, "hg", [128, 512], F32, 2)
    sg_r = Rot(S4, "sg", [128, 512], F32, 2)
    v_r = Rot(S4, "v", [128, 512], F32, 2)
    hu_r = Rot(S4, "hu", [128, 512], F32, 2)
    g2_r = Rot(S4, "g2", [128, 512], F32, 2)
    ysb_r = Rot(S4, "ysb", [128, D], F32, 3)
    SUB = [(0, 512), (512, 512), (1024, CAP - 1024)]
    def wload(e):
        wg, Bwg = wg_r.next()
        wu, Bwu = wu_r.next()
        wd, Bwd = wd_r.next()
        bde, Bbde = bde_r.next()
        P.dma("pool", wg[:], wg_d[e].rearrange("(k p) f -> p k f", p=128), writes=[Bwg])
        P.dma("pool", wu[:], wu_d[e].rearrange("(k p) f -> p k f", p=128), writes=[Bwu])
        P.dma("pool", wd[:], wd_d[e].rearrange("(k p) f -> p k f", p=128), writes=[Bwd])
        P.dma("pool", bde[:], bd_d[e:e + 1, :], writes=[Bbde])
        return (wg, Bwg, wu, Bwu, wd, Bwd, bde, Bbde)

    pend_down = [None]

    def down_proj(e, s0, nt_, hT, BhT, wd, Bwd, bde, Bbde):
        for tt in range(nt_):
            r0 = e * CAP + s0 + tt * 128
            ysb, Bysb = ysb_r.next()
            for nh in range(2):
                py, Bpy = PSF()
                for fc in range(8):
                    P.op("pe", lambda h, py=py, fc=fc, tt=tt, nh=nh, hT=hT, wd=wd: h.matmul(
                        py[:, :], lhsT=hT[:, fc, tt * 128:(tt + 1) * 128], rhs=wd[:, fc, nh * 512:(nh + 1) * 512],
                        start=(fc == 0), stop=False), reads=[BhT, Bwd], writes=[Bpy])
                P.op("pe", lambda h, py=py, nh=nh, bde=bde: h.matmul(
                    py[:, :], lhsT=ones1[:], rhs=bde[:, nh * 512:(nh + 1) * 512], start=False, stop=True),
                    reads=[Bbde, B_c4], writes=[Bpy])
                evac(ysb[:, nh * 512:(nh + 1) * 512], py[:, :], [Bpy], [Bysb])
            for hf in range(2):
                P.dma("pool", yd_h[hf][r0:r0 + 128, :], ysb[:, hf * 512:(hf + 1) * 512], reads=[Bysb], writes=[])

    wnext = wload(0)
    for e in range(NEXP):
        wg, Bwg, wu, Bwu, wd, Bwd, bde, Bbde = wnext
        for si, (s0, ns) in enumerate(SUB):
            nt_ = ns // 128
            xT, BxT = xT_r.next()
            for tt in range(nt_):
                r0 = e * CAP + s0 + tt * 128
                xrow, Bxrow = xrow_r.next()
                P.dma("sp", xrow[:], xd_s[r0:r0 + 128, :], writes=[Bxrow])
                pt_, Bpt_ = PSB()
                for b in range(8):
                    P.op("pe", lambda h, pt_=pt_, b=b, xrow=xrow: h.transpose(
                        out=pt_[:, b * 128:(b + 1) * 128], in_=xrow[:, b * 128:(b + 1) * 128], identity=ident_b[:]),
                        reads=[Bxrow, B_const], writes=[Bpt_])
                evac(xT[:, :, tt * 128:(tt + 1) * 128], pt_[:, :].rearrange("p (b t) -> p b t", b=8), [Bpt_], [BxT])
            hT, BhT = hT_r.next()
            for fc in range(8):
                pg, Bpg = PSF()
                for k in range(8):
                    P.op("pe", lambda h, pg=pg, k=k, fc=fc, wg=wg, xT=xT, ns=ns: h.matmul(
                        pg[:, 0:ns], lhsT=wg[:, k, fc * 128:(fc + 1) * 128], rhs=xT[:, k, 0:ns], start=(k == 0), stop=(k == 7)),
                        reads=[Bwg, BxT], writes=[Bpg])
                pu, Bpu = PSF()
                for k in range(8):
                    P.op("pe", lambda h, pu=pu, k=k, fc=fc, wu=wu, xT=xT, ns=ns: h.matmul(
                        pu[:, 0:ns], lhsT=wu[:, k, fc * 128:(fc + 1) * 128], rhs=xT[:, k, 0:ns], start=(k == 0), stop=(k == 7)),
                        reads=[Bwu, BxT], writes=[Bpu])
                hg, Bhg = hg_r.next()
                sg, Bsg = sg_r.next()
                v, Bv = v_r.next()
                hu, Bhu = hu_r.next()
                g2, Bg2 = g2_r.next()
                P.op("dve", lambda h, hg=hg, pg=pg, e=e, fc=fc, ns=ns: h.tensor_scalar(
                    out=hg[:, 0:ns], in0=pg[:, 0:ns], scalar1=bgt[:, e, fc:fc + 1], scalar2=7.0, op0=ALU.add, op1=ALU.min),
                    reads=[Bpg, B_c4], writes=[Bhg])
                P.op("act", lambda h, sg=sg, hg=hg, ns=ns: h.activation(out=sg[:, 0:ns], in_=hg[:, 0:ns], func=AF.Sigmoid, scale=1.702),
                     reads=[Bhg], writes=[Bsg])
                P.op("act", lambda h, v=v, pu=pu, e=e, fc=fc, ns=ns: h.activation(
                    out=v[:, 0:ns], in_=pu[:, 0:ns], func=AF.Identity, bias=but[:, e, fc:fc + 1], scale=1.0),
                    reads=[Bpu, B_c4], writes=[Bv])
                P.op("dve", lambda h, hu=hu, v=v, ns=ns: h.tensor_scalar(out=hu[:, 0:ns], in0=v[:, 0:ns], scalar1=8.0, scalar2=-6.0,
                                                                         op0=ALU.min, op1=ALU.max), reads=[Bv], writes=[Bhu])
                P.op("dve", lambda h, g2=g2, hg=hg, sg=sg, ns=ns: h.tensor_tensor(out=g2[:, 0:ns], in0=hg[:, 0:ns], in1=sg[:, 0:ns], op=ALU.mult),
                     reads=[Bhg, Bsg], writes=[Bg2])
                P.op("dve", lambda h, hT=hT, fc=fc, hu=hu, g2=g2, ns=ns: h.tensor_tensor(out=hT[:, fc, 0:ns], in0=hu[:, 0:ns], in1=g2[:, 0:ns], op=ALU.mult),
                     reads=[Bhu, Bg2], writes=[BhT])
            if pend_down[0] is not None:
                pend_down[0]()
            if si == 0 and e + 1 < NEXP:
                wnext = wload(e + 1)
            pend_down[0] = (lambda e=e, s0=s0, nt_=nt_, hT=hT, BhT=BhT, wd=wd, Bwd=Bwd, bde=bde, Bbde=Bbde:
                            down_proj(e, s0, nt_, hT, BhT, wd, Bwd, bde, Bbde))
    pend_down[0]()

    P.barrier()
    if stop_after == "F":
        P.emit()
        return nc

    S5 = Arena(nc, A0, SBMAX)
    B_c5 = Buf()
    ln2g = S5.t("ln2g", [128, D], F32)
    ln2b = S5.t("ln2b", [128, D], F32)
    P.dma("sp", ln2g[:], ln2g_d, writes=[B_c5])
    P.dma("sp", ln2b[:], ln2b_d, writes=[B_c5])
    xo_r = Rot(S5, "xo", [128, D], F32, 5)
    yk_r = Rot(S5, "yk", [128, D], F32, 16)
    for i_ in range(len(yk_r.items)):
        yk_r.items[i_] = (yk_r.items[i_][0], [Buf(), Buf()])
    rtile_r = Rot(S5, "rtile", [128, 8], F32, 6)
    su5_r = Rot(S5, "su5", [128, 1], U32, 16)
    st5_r = Rot(S5, "st5", [128, 16], F32, 4)
    for (ykt, Byk) in yk_r.items:
        P.op("dve", lambda h, ykt=ykt: h.memset(ykt[:], 0.0), writes=Byk)
    def fetch5(t):
        tok = slice(t * 128, (t + 1) * 128)
        xo, Bxo = xo_r.next()
        rtile, Brt = rtile_r.next()
        P.dma("sp", xo[:], x1_s[tok, :], reads=tb("x1", t, t + 1), writes=[Bxo])
        P.dma("sp", rtile[:], route_s[tok, :], reads=tb("route", t, t + 1), writes=[Brt])
        yks = []
        for k in range(4):
            su, Bsu = su5_r.next()
            P.op("dve", lambda h, su=su, rtile=rtile, k=k: h.tensor_copy(out=su[:], in_=rtile[:, k:k + 1]), reads=[Brt], writes=[Bsu])
            ykt, Byk = yk_r.next()
            for hf in range(2):
                P.idma(ykt[:, hf * 512:(hf + 1) * 512], None, yd_h[hf], bass.IndirectOffsetOnAxis(ap=su[:, 0:1], axis=0),
                       reads=[Bsu], writes=[Byk[hf]], bc=NSLOT - 1)
            yks.append((ykt, Byk))
        P.op("act", lambda h, xo=xo: h.mul(out=xo[:], in_=xo[:], mul=ALPHA), reads=[Bxo], writes=[Bxo])
        return (tok, xo, Bxo, rtile, Brt, yks)

    AHEAD = 3
    ctxs = {}
    for t in range(min(AHEAD, NT)):
        ctxs[t] = fetch5(t)
    for t in range(NT):
        if t + AHEAD < NT:
            ctxs[t + AHEAD] = fetch5(t + AHEAD)
        tok, xo, Bxo, rtile, Brt, yks = ctxs.pop(t)
        for k in range(4):
            ykt, Byk = yks[k]
            P.op("dve", lambda h, xo=xo, ykt=ykt, rtile=rtile, k=k: h.scalar_tensor_tensor(
                out=xo[:], in0=ykt[:], scalar=rtile[:, 4 + k:5 + k], in1=xo[:], op0=ALU.mult, op1=ALU.add),
                reads=[Bxo, Brt] + Byk, writes=[Bxo])
        layer_norm(xo, Bxo, ln2g, ln2b, B_c5, xo, Bxo, st5_r)
        P.dma("act", out_d[tok, :], xo[:], reads=[Bxo])
    P.barrier()
    P.emit()
    return nc


def _consts():
    p = np.arange(128)[:, None]
    j = np.arange(2048)[None, :]
    dist = np.abs(p - j + 1920)
    negD = ((-16.0 * (dist // 16)).astype(np.float32), (-(dist % 16)).astype(np.float32))
    jj = np.arange(640)[None, :]
    kp = jj - 512
    cq = p // 64
    valid = (kp >= cq * 64 - 512) & (kp < cq * 64 + 64)
    rel = np.clip(p - kp, -128, 128) + 128
    maskB = np.where(valid, 0.0, NEG).astype(np.float32)
    return negD, rel, maskB


def prep_inputs(inp, nseq=4, ncores=NCORES):
    f = lambda a: np.ascontiguousarray(np.asarray(a, dtype=np.float32))
    negD, rel, maskB = _consts()
    bc = lambda v, n=128: np.ascontiguousarray(np.broadcast_to(np.asarray(v, np.float32).reshape(1, -1), (n, v.size)))
    w_uk = f(inp["w_uk"][0])
    w_uk2 = np.ascontiguousarray(w_uk.reshape(4, 2, 64, 128).transpose(1, 2, 0, 3).reshape(128, 4, 128))
    relb = np.ascontiguousarray(f(inp["rel_bias"][0])[:, rel])
    bgp = np.ascontiguousarray(f(inp["b_gate"][0]).reshape(NEXP, 8, 128).transpose(2, 0, 1))
    bup = np.ascontiguousarray(f(inp["b_up"][0]).reshape(NEXP, 8, 128).transpose(2, 0, 1))
    shared = {
        "w_in": f(inp["w_in"][0]), "kvg_bc": bc(inp["kv_norm_g"][0]), "ikg_bc": bc(inp["idx_k_norm_g"][0]),
        "ikb_bc": bc(inp["idx_k_norm_b"][0]), "w_uk2": w_uk2, "w_uv": f(inp["w_uv"][0]), "relb": relb,
        "maskB": maskB, "negDh": negD[0].astype(ml_dtypes.bfloat16), "negDl": negD[1].astype(ml_dtypes.bfloat16),
        "slopeI": np.ascontiguousarray(np.stack([np.eye(128, dtype=np.float32) * 2.0 ** (-(h + 1)) for h in range(8)], axis=1)).astype(ml_dtypes.bfloat16), "w_branch_a": f(inp["w_branch_a"][0]), "w_branch_b": f(inp["w_branch_b"][0]),
        "w_out": f(inp["w_out"][0]), "ln1g_bc": bc(inp["ln1_g"][0]), "ln1b_bc": bc(inp["ln1_b"][0]),
        "w_router": f(inp["w_router"][0]), "br_bc": bc(inp["b_router"][0]), "w_gate": f(inp["w_gate"][0]),
        "w_up": f(inp["w_up"][0]), "w_down": f(inp["w_down"][0]), "b_gate_p": bgp, "b_up_p": bup,
        "b_down": f(inp["b_down"][0]), "ln2g_bc": bc(inp["ln2_g"][0]), "ln2b_bc": bc(inp["ln2_b"][0]),
        "ident_bf": np.eye(128, dtype=np.float32).astype(ml_dtypes.bfloat16), "ident_f": np.eye(128, dtype=np.float32),
        "ustrict": np.triu(np.ones((128, 128), np.float32), 1).astype(ml_dtypes.bfloat16),
        "pow2": np.ascontiguousarray(np.broadcast_to((2.0 ** -(np.arange(21, dtype=np.float64) + 1)).astype(np.float32)[None, :], (128, 21))),
        "ones_bf": np.ones((128, 128), np.float32).astype(ml_dtypes.bfloat16),
        "ebase": np.ascontiguousarray(np.broadcast_to((np.arange(NEXP, dtype=np.float32) * CAP)[None, :], (128, NEXP))),
    }
    x = np.asarray(inp["x"], dtype=np.float32)
    maps = []
    for c in range(ncores):
        xs = x[c * nseq:(c + 1) * nseq].reshape(nseq * SEQ, D)
        m = dict(shared)
        m["x"] = np.ascontiguousarray(xs)
        m["xT"] = np.ascontiguousarray(xs.T)
        maps.append(m)
    return maps


def kernel(**inputs):
    nseq = inputs["x"].shape[0] // NCORES
    nc = build(nseq)
    maps = prep_inputs(inputs, nseq)
    res = run_bass_kernel_spmd(nc, maps, core_ids=list(range(NCORES)))
    out = np.concatenate([np.asarray(r["out"]).reshape(nseq, SEQ, D) for r in res.results], axis=0)
    return out.astype(np.float32)
```
